# Optimizing a Trainium2 kernel written in Bass

```python
import math
import jax, jax.numpy as jnp
from jax import lax
import numpy as np

D_MODEL = 1024
BATCH = 2
SEQ = 8192
DEPTH = 2

GRID_W = 64
CTX_LEN = 256

MIX_W = D_MODEL
POOL_W = MIX_W // 2
POOL_GROUPS = 4
POOL_WINDOWS = (2, 4, 8, 16)
POOL_GC = POOL_W // POOL_GROUPS
MLA_HEADS = 8
QK_NOPE = 64
QK_ROPE = 32
V_HEAD = 64
Q_LORA = D_MODEL // 4
KV_LORA = D_MODEL // 4
IN_W = POOL_W + Q_LORA + KV_LORA + QK_ROPE
ROPE_AXIS = QK_ROPE // 2
ROPE_BASE = 10000.0
Q_BLOCK = 128
D_FF = 2816
N_EXPERTS = 8
TOP_K = 2
D_FF_EXPERT = 3584
HY_WIDTH = D_MODEL
HY_SHORT = 3
HY_BANDS = 16
HY_EMB = 1 + 2 * HY_BANDS
HY_FILTER_HIDDEN = 64
HY_FAST_DECAY = 0.3
HY_SLOW_DECAY = 1.5
HY_TARGET = 1e-2
HY_MIN_DECAY = math.log(HY_TARGET) / HY_SLOW_DECAY
HY_MAX_DECAY = math.log(HY_TARGET) / HY_FAST_DECAY
LN_EPS = 1e-5
RMS_EPS = 1e-6

kernel_name = 'hybrid_pool_mla_hyena_moe_diffusion_block'


def layer_norm(x, g, b):
    xf = x.astype(jnp.float32)
    mu = jnp.mean(xf, axis=-1, keepdims=True)
    var = jnp.mean(jnp.square(xf - mu), axis=-1, keepdims=True)
    return ((xf - mu) * lax.rsqrt(var + LN_EPS) * g.astype(jnp.float32) + b.astype(jnp.float32)).astype(x.dtype)


def rms_norm(x, g):
    xf = x.astype(jnp.float32)
    return (xf * lax.rsqrt(jnp.mean(jnp.square(xf), axis=-1, keepdims=True) + RMS_EPS) * g.astype(jnp.float32)).astype(x.dtype)


def ada_chunks(s, w, b, k):
    mod = s @ w[:, :k * D_MODEL] + b[:k * D_MODEL]
    return [m[:, None, :] for m in jnp.split(mod, k, axis=-1)]


def axial_angles(n):
    rows = n // GRID_W
    r = jnp.repeat(jnp.arange(rows, dtype=jnp.float32), GRID_W)
    col = jnp.tile(jnp.arange(GRID_W, dtype=jnp.float32), rows)
    inv = ROPE_BASE ** (-jnp.arange(0, ROPE_AXIS, 2, dtype=jnp.float32) / ROPE_AXIS)
    return r[:, None] * inv, col[:, None] * inv


def rotate_pairs(x, ang):
    half = x.shape[-1] // 2
    x1, x2 = x[..., :half], x[..., half:]
    cos = jnp.cos(ang).astype(x.dtype)
    sin = jnp.sin(ang).astype(x.dtype)
    return jnp.concatenate([x1 * cos - x2 * sin, x2 * cos + x1 * sin], axis=-1)


def axial_rope(x, ang_r, ang_c):
    return jnp.concatenate([rotate_pairs(x[..., :ROPE_AXIS], ang_r), rotate_pairs(x[..., ROPE_AXIS:], ang_c)], axis=-1)


def multiscale_pool(u, pool_w, pool_scale):
    b, n, _ = u.shape
    ug = u.reshape(b, n, POOL_GROUPS, POOL_GC)
    cs = jnp.cumsum(ug.astype(jnp.float32), axis=1)
    cs = jnp.concatenate([jnp.zeros((b, 1, POOL_GROUPS, POOL_GC), jnp.float32), cs], axis=1)
    t = jnp.arange(n)[:, None]
    half = jnp.array([w // 2 for w in POOL_WINDOWS], dtype=jnp.int32)[None, :]
    lo = jnp.clip(t - half, 0, n)
    hi = jnp.clip(t + half, 0, n)
    grp = jnp.arange(POOL_GROUPS)[None, :]
    win_sum = cs[:, hi, grp] - cs[:, lo, grp]
    mean = win_sum / (hi - lo).astype(jnp.float32)[None, :, :, None]
    d = (mean - ug.astype(jnp.float32)).astype(u.dtype)
    y = jnp.einsum('bngc,gcd->bngd', d, pool_w)
    return y.reshape(b, n, POOL_W) * pool_scale


def mla_queries(q_lat, q_norm, q_up, ang):
    b, n, _ = q_lat.shape
    q = (rms_norm(q_lat, q_norm) @ q_up).reshape(b, n, MLA_HEADS, QK_NOPE + QK_ROPE)
    q_nope, q_rope = q[..., :QK_NOPE], q[..., QK_NOPE:]
    if ang is not None:
        q_rope = axial_rope(q_rope, ang[0][:, None, :], ang[1][:, None, :])
    return jnp.concatenate([q_nope, q_rope], axis=-1) * (QK_NOPE + QK_ROPE) ** -0.5


def mla_keys_values(kv_in, kv_norm, kv_up, ang):
    b, n, _ = kv_in.shape
    kv_lat, k_rope = kv_in[..., :KV_LORA], kv_in[..., KV_LORA:]
    kv = (rms_norm(kv_lat, kv_norm) @ kv_up).reshape(b, n, MLA_HEADS, QK_NOPE + V_HEAD)
    k_nope, v = kv[..., :QK_NOPE], kv[..., QK_NOPE:]
    if ang is not None:
        k_rope = axial_rope(k_rope, ang[0], ang[1])
    k = jnp.concatenate([k_nope, jnp.broadcast_to(k_rope[:, :, None, :], (b, n, MLA_HEADS, QK_ROPE))], axis=-1)
    return k, v


def attend(q, k, v):
    s = jnp.einsum('bqhd,bkhd->bhqk', q, k).astype(jnp.float32)
    p = jax.nn.softmax(s, axis=-1).astype(v.dtype)
    return jnp.einsum('bhqk,bkhd->bqhd', p, v)


def blocked_attend(q, k, v):
    b, n, h, d = q.shape
    qb = q.reshape(b, n // Q_BLOCK, Q_BLOCK, h, d).transpose(1, 0, 2, 3, 4)
    o = lax.map(lambda qq: attend(qq, k, v), qb)
    return o.transpose(1, 0, 2, 3, 4).reshape(b, n, h, V_HEAD)


def pool_mla_mixer(u, u_ctx, ctx_out, ang, in_w, pool_w, pool_scale, q_norm, q_up, kv_norm, kv_up, out_w):
    b, n, _ = u.shape
    kv0 = POOL_W + Q_LORA
    proj = u @ in_w
    if ctx_out:
        proj_c = u_ctx @ in_w
        kv_c_in = proj_c[..., kv0:]
    else:
        kv_c_in = u_ctx @ in_w[:, kv0:]
    k_c, v_c = mla_keys_values(kv_c_in, kv_norm, kv_up, None)
    k_l, v_l = mla_keys_values(proj[..., kv0:], kv_norm, kv_up, ang)
    q_l = mla_queries(proj[..., POOL_W:kv0], q_norm, q_up, ang)
    o_l = blocked_attend(q_l, jnp.concatenate([k_c, k_l], axis=1), jnp.concatenate([v_c, v_l], axis=1))
    y_l = jnp.concatenate([multiscale_pool(proj[..., :POOL_W], pool_w, pool_scale), o_l.reshape(b, n, MLA_HEADS * V_HEAD)], axis=-1) @ out_w
    y_c = None
    if ctx_out:
        m = u_ctx.shape[1]
        q_c = mla_queries(proj_c[..., POOL_W:kv0], q_norm, q_up, None)
        o_c = attend(q_c, k_c, v_c)
        y_c = jnp.concatenate([multiscale_pool(proj_c[..., :POOL_W], pool_w, pool_scale), o_c.reshape(b, m, MLA_HEADS * V_HEAD)], axis=-1) @ out_w
    return y_l, y_c


def short_conv(z, w, bias):
    y = lax.conv_general_dilated(z, w[:, None, :], window_strides=(1,), padding=[(HY_SHORT // 2, HY_SHORT // 2)],
                                 dimension_numbers=('NWC', 'WIO', 'NWC'), feature_group_count=z.shape[-1])
    return y + bias


def hyena_filters(n, fw1, fb1, fw2, fb2, fw3, fb3, fout, freq):
    t = jnp.linspace(0.0, 1.0, n, dtype=jnp.float32)[:, None]
    w_ang = (2.0 * math.pi / n) * jnp.arange(n, dtype=jnp.float32)[:, None]
    bands = jnp.linspace(1e-4, HY_BANDS - 1, HY_BANDS, dtype=jnp.float32)[None, :]
    z = jnp.concatenate([t, jnp.cos(bands * w_ang), -jnp.sin(bands * w_ang)], axis=-1)
    freq = freq.astype(jnp.float32)
    h = jnp.sin(freq * (z @ fw1.astype(jnp.float32) + fb1.astype(jnp.float32)))
    h = jnp.sin(freq * (h @ fw2.astype(jnp.float32) + fb2.astype(jnp.float32)))
    h = jnp.sin(freq * (h @ fw3.astype(jnp.float32) + fb3.astype(jnp.float32)))
    h = (h @ fout.astype(jnp.float32)).reshape(n, 2, HY_WIDTH)
    deltas = jnp.abs(jnp.linspace(HY_MIN_DECAY, HY_MAX_DECAY, HY_WIDTH, dtype=jnp.float32))
    h = h * jnp.exp(-t * deltas)[:, None, :]
    return h[:, 0], h[:, 1]


def bidir_long_conv(v, h_f, h_b, skip):
    b, n, ch = v.shape
    vf = v.astype(jnp.float32)
    k = jnp.concatenate([h_f, jnp.zeros((1, ch), jnp.float32), jnp.flip(h_b[1:], axis=0)], axis=0)
    spec = jnp.fft.rfft(vf, n=2 * n, axis=1) * jnp.fft.rfft(k, n=2 * n, axis=0)[None]
    y = jnp.fft.irfft(spec, n=2 * n, axis=1)[:, :n]
    return (y + vf * skip.astype(jnp.float32)).astype(v.dtype)


def hyena_mixer(u, in_w, conv_w, conv_b, fw1, fb1, fw2, fb2, fw3, fb3, fout, freq, skip, out_w):
    n = u.shape[1]
    z = short_conv(u @ in_w, conv_w, conv_b)
    x0, x1, v = jnp.split(z, 3, axis=-1)
    h_f, h_b = hyena_filters(n, fw1, fb1, fw2, fb2, fw3, fb3, fout, freq)
    y = x0 * bidir_long_conv(v * x1, h_f, h_b, skip)
    return y @ out_w


def swiglu(h, wg, wu, wd):
    return (jax.nn.silu(h @ wg) * (h @ wu)) @ wd


def moe_swiglu(h, router_w, wg, wu, wd):
    shp = h.shape
    t = h.reshape(-1, shp[-1])
    logits = (t @ router_w).astype(jnp.float32)
    top_v, top_i = lax.top_k(logits, TOP_K)
    gates = jax.nn.softmax(top_v, axis=-1)
    combine = jnp.sum(jax.nn.one_hot(top_i, N_EXPERTS, dtype=jnp.float32) * gates[..., None], axis=1).astype(h.dtype)
    out = jnp.zeros_like(t)
    for e in range(N_EXPERTS):
        out = out + combine[:, e:e + 1] * swiglu(t, wg[e], wu[e], wd[e])
    return out.reshape(shp)


def setup_inputs(seed: int = 0) -> dict:
    key = jax.random.key(seed)
    keys = jax.random.split(key, 48)
    counter = [0]

    def nrm(shape, scale):
        k = keys[counter[0]]
        counter[0] += 1
        return jax.random.normal(k, shape, jnp.float32) * scale

    D = D_MODEL
    ne = (DEPTH + 1) // 2
    no = DEPTH // 2
    beta = (8.0 * DEPTH) ** -0.25
    fh = HY_FILTER_HIDDEN
    return {
        'x': nrm((BATCH, SEQ, D), 1.0),
        'c': nrm((BATCH, D), 1.0),
        'ctx': nrm((BATCH, CTX_LEN, D), 1.0),
        'c_ctx': nrm((D,), 1.0),
        'ada_w': nrm((DEPTH, D, 6 * D), 0.5 * D ** -0.5),
        'ada_b': nrm((DEPTH, 6 * D), 0.02),
        'ln_g': 1.0 + nrm((DEPTH, 2, D), 0.02),
        'ln_b': nrm((DEPTH, 2, D), 0.02),
        'mix_in_w': nrm((ne, D, IN_W), D ** -0.5),
        'pool_w': nrm((ne, POOL_GROUPS, POOL_GC, POOL_GC), POOL_GC ** -0.5),
        'pool_scale': 1.0 + nrm((ne, POOL_W), 0.02),
        'q_norm': 1.0 + nrm((ne, Q_LORA), 0.02),
        'q_up': nrm((ne, Q_LORA, MLA_HEADS * (QK_NOPE + QK_ROPE)), Q_LORA ** -0.5),
        'kv_norm': 1.0 + nrm((ne, KV_LORA), 0.02),
        'kv_up': nrm((ne, KV_LORA, MLA_HEADS * (QK_NOPE + V_HEAD)), KV_LORA ** -0.5),
        'mix_out_w': nrm((ne, MIX_W, D), beta * MIX_W ** -0.5),
        'ffn_gate': nrm((ne, D, D_FF), D ** -0.5),
        'ffn_up': nrm((ne, D, D_FF), D ** -0.5),
        'ffn_down': nrm((ne, D_FF, D), beta * D_FF ** -0.5),
        'hy_in_w': nrm((no, D, 3 * HY_WIDTH), D ** -0.5),
        'hy_conv_w': nrm((no, HY_SHORT, 3 * HY_WIDTH), HY_SHORT ** -0.5),
        'hy_conv_b': nrm((no, 3 * HY_WIDTH), 0.02),
        'hy_fw1': nrm((no, HY_EMB, fh), HY_EMB ** -0.5),
        'hy_fb1': nrm((no, fh), 0.1),
        'hy_fw2': nrm((no, fh, fh), fh ** -0.5),
        'hy_fb2': nrm((no, fh), 0.1),
        'hy_fw3': nrm((no, fh, fh), fh ** -0.5),
        'hy_fb3': nrm((no, fh), 0.1),
        'hy_fout': nrm((no, fh, 2 * HY_WIDTH), 0.1 * fh ** -0.5),
        'hy_freq': 1.0 + nrm((no, fh), 0.02),
        'hy_skip': nrm((no, HY_WIDTH), 1.0),
        'hy_out_w': nrm((no, HY_WIDTH, D), beta * HY_WIDTH ** -0.5),
        'router_w': nrm((no, D, N_EXPERTS), D ** -0.5),
        'moe_gate': nrm((no, N_EXPERTS, D, D_FF_EXPERT), D ** -0.5),
        'moe_up': nrm((no, N_EXPERTS, D, D_FF_EXPERT), D ** -0.5),
        'moe_down': nrm((no, N_EXPERTS, D_FF_EXPERT, D), beta * D_FF_EXPERT ** -0.5),
    }


def reference(x, c, ctx, c_ctx, ada_w, ada_b, ln_g, ln_b, mix_in_w, pool_w, pool_scale, q_norm, q_up, kv_norm, kv_up,
              mix_out_w, ffn_gate, ffn_up, ffn_down, hy_in_w, hy_conv_w, hy_conv_b, hy_fw1, hy_fb1, hy_fw2, hy_fb2,
              hy_fw3, hy_fb3, hy_fout, hy_freq, hy_skip, hy_out_w, router_w, moe_gate, moe_up, moe_down):
    alpha = (2.0 * DEPTH) ** 0.25
    n = x.shape[1]
    ang = axial_angles(n)
    s_lat = jax.nn.silu(c)
    s_ctx = jax.nn.silu(c_ctx)[None, :]
    last_reader = (DEPTH - 1) - ((DEPTH - 1) % 2)
    h, hc = x, ctx
    for l in range(DEPTH):
        i = l // 2
        even = (l % 2 == 0)
        ctx_live = l <= last_reader
        ctx_out = l < last_reader
        sh1, sc1, g1, sh2, sc2, g2 = ada_chunks(s_lat, ada_w[l], ada_b[l], 6)
        u = h * (1 + sc1) + sh1
        uc, mc = None, None
        if ctx_live:
            mc = ada_chunks(s_ctx, ada_w[l], ada_b[l], 6 if ctx_out else 2)
            uc = hc * (1 + mc[1]) + mc[0]
        if even:
            y, yc = pool_mla_mixer(u, uc, ctx_out, ang, mix_in_w[i], pool_w[i], pool_scale[i], q_norm[i], q_up[i],
                                   kv_norm[i], kv_up[i], mix_out_w[i])
        else:
            hy_args = (hy_in_w[i], hy_conv_w[i], hy_conv_b[i], hy_fw1[i], hy_fb1[i], hy_fw2[i], hy_fb2[i],
                       hy_fw3[i], hy_fb3[i], hy_fout[i], hy_freq[i], hy_skip[i], hy_out_w[i])
            y = hyena_mixer(u, *hy_args)
            yc = hyena_mixer(uc, *hy_args) if ctx_out else None

        def channel_mix(z):
            if even:
                return swiglu(z, ffn_gate[i], ffn_up[i], ffn_down[i])
            return moe_swiglu(z, router_w[i], moe_gate[i], moe_up[i], moe_down[i])

        h = layer_norm(alpha * h + g1 * y, ln_g[l, 0], ln_b[l, 0])
        h = layer_norm(alpha * h + g2 * channel_mix(h * (1 + sc2) + sh2), ln_g[l, 1], ln_b[l, 1])
        if ctx_out:
            hc = layer_norm(alpha * hc + mc[2] * yc, ln_g[l, 0], ln_b[l, 0])
            hc = layer_norm(alpha * hc + mc[5] * channel_mix(hc * (1 + mc[4]) + mc[3]), ln_g[l, 1], ln_b[l, 1])
    return h
```

```python
import math
import numpy as np
from contextlib import ExitStack
import concourse.bass as bass
import concourse.mybir as mybir
from concourse.bass_utils import run_bass_kernel_spmd

F32 = mybir.dt.float32
BF16 = mybir.dt.bfloat16
AF = mybir.ActivationFunctionType
ALU = mybir.AluOpType

SEM_CHUNK = 30000
NCORES = 8
ALPHA = 4.0 ** 0.25
LN_EPS = 1e-5
RMS_EPS = 1e-6
POOL_WINDOWS = (2, 4, 8, 16)


class Buf:
    __slots__ = ("name", "w", "r")

    def __init__(self, name=""):
        self.name = name
        self.w = None
        self.r = []


class Sched:
    ENG = ("pe", "act", "dve", "pool", "sp")

    def __init__(self, nc, stack):
        self.nc = nc
        self.stack = stack
        self.streams = {e: [] for e in self.ENG}
        self.seq = {e: 0 for e in self.ENG}
        self.esems = {e: [] for e in self.ENG}
        self.waited = {e: {} for e in self.ENG}
        self.dsem = {}
        self.free_d = []
        self.nsem = 0

    def _newsem(self, name):
        s = self.stack.enter_context(self.nc.semaphore(name))
        self.nsem += 1
        return s

    def _etoken(self, eng):
        n = self.seq[eng]
        ci = n // SEM_CHUNK
        while len(self.esems[eng]) <= ci:
            self.esems[eng].append(self._newsem(f"e_{eng}_{len(self.esems[eng])}"))
        self.seq[eng] = n + 1
        return (self.esems[eng][ci], n % SEM_CHUNK + 1, eng)

    def _need_wait(self, eng, tok):
        if tok is None:
            return False
        sem, val, src = tok
        if src == eng and eng == "pe":
            return False
        cur = self.waited[eng].get(id(sem), 0)
        if cur >= val:
            return False
        self.waited[eng][id(sem)] = val
        return True

    def _deps(self, eng, r, w):
        toks = []
        for b in r:
            if b.w is not None:
                toks.append(b.w)
        for b in w:
            if b.w is not None:
                toks.append(b.w)
            toks.extend(b.r)
        return [t for t in toks if self._need_wait(eng, t)]

    def _commit(self, tok, r, w):
        for b in r:
            b.r.append(tok)
            if len(b.r) > 96:
                b.r = b.r[-96:]
        for b in w:
            b.w = tok
            b.r = []

    def op(self, eng, fn, r=(), w=()):
        waits = self._deps(eng, r, w)
        tok = self._etoken(eng)
        self.streams[eng].append(("op", fn, waits, tok))
        self._commit(tok, r, w)
        return tok

    def dma(self, eng, out, in_, r=(), w=(), key=None):
        waits = self._deps(eng, r, w)
        kb = key if key is not None else (w[0] if len(w) else r[0])
        ent = self._dsem_for(kb)
        ent[1] += 16
        tok = (ent[0], ent[1], "dma")
        self.streams[eng].append(("dma", (out, in_), waits, tok))
        self._commit(tok, r, w)
        return tok

    def _dsem_for(self, kb):
        if kb not in self.dsem:
            if self.free_d:
                self.dsem[kb] = self.free_d.pop()
            else:
                self.dsem[kb] = [self._newsem(f"d{self.nsem}"), 0]
        return self.dsem[kb]

    def coll(self, kind, groups, src_t, dst_t, r=(), w=()):
        waits = self._deps("pool", r, w)
        ent = self._dsem_for(("cc", len(self.streams["pool"])))
        ent[1] += 1
        tok = (ent[0], ent[1], "dma")
        self.streams["pool"].append(("cc", (kind, groups, src_t, dst_t), waits, tok))
        self._commit(tok, r, w)
        return tok

    def wait(self, eng, tok):
        if self._need_wait(eng, tok):
            self.streams[eng].append(("wait", None, [tok], None))

    def barrier(self):
        toks = []
        for e in self.ENG:
            n = self.seq[e]
            if n > 0:
                ci = (n - 1) // SEM_CHUNK
                toks.append((self.esems[e][ci], (n - 1) % SEM_CHUNK + 1, e))
        for kb, ent in self.dsem.items():
            if ent[1] > 0:
                toks.append((ent[0], ent[1], "dma"))
        for e in self.ENG:
            for t in toks:
                if t[2] == e:
                    continue
                self.wait(e, t)
        for kb, ent in self.dsem.items():
            self.free_d.append(ent)
        self.dsem = {}

    def emit(self):
        nc = self.nc
        engmap = {"pe": "tensor", "act": "scalar", "dve": "vector", "pool": "gpsimd", "sp": "sync"}
        with nc.Block() as block:
            for e in self.ENG:
                stream = self.streams[e]

                def body(engine, stream=stream):
                    for kind, payload, waits, tok in stream:
                        for (sem, val, _src) in waits:
                            engine.wait_ge(sem, val)
                        if kind == "op":
                            payload(engine).then_inc(tok[0], 1)
                        elif kind == "dma":
                            out, in_ = payload
                            if callable(in_):
                                in_ = in_(engine)
                            engine.dma_start(out=out, in_=in_).then_inc(tok[0], 16)
                        elif kind == "cc":
                            ckind, groups, src_t, dst_t = payload
                            engine.collective_compute(ckind, ALU.bypass, replica_groups=groups, ins=[src_t.ap().opt()],
                                                      outs=[dst_t.ap().opt()]).then_inc(tok[0])
                getattr(block, engmap[e])(body)


class Arena:
    def __init__(self, t, lo, hi):
        self.t = t
        self.lo = lo
        self.hi = hi
        self.off = lo

    def alloc(self, free_shape, dt, parts=128):
        n = 1
        for s in free_shape:
            n *= s
        esz = 4 if dt == F32 else 2
        words = (n * esz + 3) // 4
        words = (words + 7) // 8 * 8
        assert self.off + words <= self.hi, f"arena overflow {self.off}+{words}>{self.hi}"
        ap = self.t[0:parts, self.off:self.off + words]
        self.off += words
        if dt != F32:
            ap = ap.bitcast(dt)
        ap = ap[:, 0:n]
        if len(free_shape) == 2:
            ap = ap.rearrange("p (a b) -> p a b", a=free_shape[0])
        elif len(free_shape) == 3:
            ap = ap.rearrange("p (a b c) -> p a b c", a=free_shape[0], b=free_shape[1])
        return ap

    def mark(self):
        return self.off

    def reset(self, m):
        self.off = m


class K:
    def __init__(self, S, identb):
        self.S = S
        self.identb = identb
        self.bident = Buf("ident")

    def mm(self, out, lhsT, rhs, start, stop, r, w):
        return self.S.op("pe", lambda e: e.matmul(out=out, lhsT=lhsT, rhs=rhs, start=start, stop=stop), r=r, w=w)

    def tr(self, out, in_, r, w):
        n = in_.shape[0]
        idn = self.identb[0:n, 0:n]
        return self.S.op("pe", lambda e: e.transpose(out=out, in_=in_, identity=idn), r=list(r) + [self.bident], w=w)

    def act(self, out, in_, func, r, w, bias=None, scale=None, accum=None):
        kw = {}
        if bias is not None:
            kw["bias"] = bias
        if scale is not None:
            kw["scale"] = scale
        if accum is not None:
            kw["accum_out"] = accum
        return self.S.op("act", lambda e: e.activation(out=out, in_=in_, func=func, **kw), r=r, w=w)

    def tt(self, eng, out, in0, in1, op, r, w):
        return self.S.op(eng, lambda e: e.tensor_tensor(out=out, in0=in0, in1=in1, op=op), r=r, w=w)

    def ts(self, eng, out, in0, s1, s2, op0, op1, r, w):
        if op1 is None:
            return self.S.op(eng, lambda e: e.tensor_scalar(out=out, in0=in0, scalar1=s1, scalar2=None, op0=op0), r=r, w=w)
        return self.S.op(eng, lambda e: e.tensor_scalar(out=out, in0=in0, scalar1=s1, scalar2=s2, op0=op0, op1=op1), r=r, w=w)

    def stt(self, eng, out, in0, scalar, in1, op0, op1, r, w):
        return self.S.op(eng, lambda e: e.scalar_tensor_tensor(out=out, in0=in0, scalar=scalar, in1=in1, op0=op0, op1=op1), r=r, w=w)

    def cp(self, eng, out, in_, r, w):
        if eng == "act":
            return self.S.op("act", lambda e: e.activation(out=out, in_=in_, func=AF.Copy), r=r, w=w)
        return self.S.op(eng, lambda e: e.tensor_copy(out=out, in_=in_), r=r, w=w)

    def memset(self, eng, out, val, w):
        return self.S.op(eng, lambda e: e.memset(out, val), r=(), w=w)

    def recip(self, out, in_, r, w):
        return self.S.op("dve", lambda e: e.reciprocal(out=out, in_=in_), r=r, w=w)

    def bn_stats(self, out, in_, r, w):
        return self.S.op("dve", lambda e: e.bn_stats(out=out, in_=in_), r=r, w=w)

    def bn_aggr(self, out, in_, r, w):
        return self.S.op("dve", lambda e: e.bn_aggr(out=out, in_=in_), r=r, w=w)


def bcast_mid(ap2, n):
    return ap2.unsqueeze(2).to_broadcast([ap2.shape[0], ap2.shape[1], n])


class Ctx:
    pass


def new_prog():
    nc = bass.Bass("TRN2", target_bir_lowering=False)
    st = ExitStack()
    S = Sched(nc, st)
    arena_t = st.enter_context(nc.sbuf_tensor("arena", [128, 52000], F32))
    psum_t = st.enter_context(nc.psum_tensor("psum", [128, 4096], F32))
    c = Ctx()
    c.nc, c.st, c.S, c.arena_t, c.psum_t = nc, st, S, arena_t, psum_t
    c.A = Arena(arena_t, 0, 52000)
    c.dyn = {}
    return c


def psbank(c, b, n=512, dt=F32, parts=128, nb=1):
    ap = c.psum_t[0:parts, b * 512:(b + nb) * 512]
    if dt != F32:
        ap = ap.bitcast(dt)
    return ap[:, 0:n]


def ln_tile(c, kk, ps_y, bps, res, bres, gate_b, lng_b, lnb_b, bconst, tmp, btmp, out, bout, small, bsmall):
    S = c.S
    for hh in range(2):
        kk.tt("dve", tmp[:, hh * 512:(hh + 1) * 512], ps_y[:, hh * 512:(hh + 1) * 512], gate_b[:, hh * 512:(hh + 1) * 512],
              ALU.mult, r=(list(bps) if isinstance(bps, (list, tuple)) else [bps]) + [bconst], w=[btmp])
    kk.stt("dve", tmp, res, ALPHA, tmp, ALU.mult, ALU.add, r=[bres, btmp], w=[btmp])
    st6 = small[:, 0:12].rearrange("p (a b) -> p a b", a=2)
    for h in range(2):
        kk.bn_stats(st6[:, h, :], tmp[:, h * 512:(h + 1) * 512], r=[btmp], w=[bsmall])
    kk.bn_aggr(small[:, 12:14], st6, r=[bsmall], w=[bsmall])
    kk.act(small[:, 14:15], small[:, 13:14], AF.Sqrt, r=[bsmall], w=[bsmall], bias=c.eps_ln, scale=1.0)
    kk.recip(small[:, 15:16], small[:, 14:15], r=[bsmall], w=[bsmall])
    kk.ts("dve", small[:, 14:15], small[:, 12:13], small[:, 15:16], -1.0, ALU.mult, ALU.mult, r=[bsmall], w=[bsmall])
    kk.act(tmp, tmp, AF.Identity, r=[btmp, bsmall], w=[btmp], bias=small[:, 14:15], scale=small[:, 15:16])
    kk.tt("dve", tmp, tmp, lng_b, ALU.mult, r=[btmp, bconst], w=[btmp])
    kk.tt("pool", out, tmp, lnb_b, ALU.add, r=[btmp, bconst], w=[bout])


def build_l1(dbg=False, stop_after=None, c=None, pre="", link=None):
    own = c is None
    if own:
        c = new_prog()
    nc, S, A = c.nc, c.S, c.A
    A.reset(0)

    def D(name, shape, dt=F32, kind="ExternalInput"):
        return nc.dram_tensor(pre + name, shape, dt, kind=kind).ap()

    xkv = D("xkv", [66, 128, 1024]); xhalo = D("xhalo", [2, 128, 1024]); ropet = D("ropet", [66, 128, 32]); ropeq = D("ropeq", [16, 128, 512])
    cs = D("cs", [128, 16])
    adaw = D("adaw", [1024, 6144]); adabcol = D("adabcol", [128, 32]); adabrow = D("adabrow", [1, 6144])
    lng = D("lng", [2, 1024]); lnb = D("lnb", [2, 1024])
    win = D("win", [1024, 1056]); poolw = D("poolw", [4, 128, 128]); pscale = D("pscale", [128, 4])
    qnorm = D("qnorm", [128, 2]); kvnorm = D("kvnorm", [128, 2])
    qup = D("qup", [256, 768]); kvupk = D("kvupk", [256, 512]); kvupv = D("kvupv", [256, 512])
    wout = D("wout", [1024, 1024]); wg = D("wg", [1024, 2816]); wu = D("wu", [1024, 2816]); wd = D("wd", [2816, 1024])
    bands = D("bands", [7, 4, 128, 128]); ident = D("ident", [128, 128])
    h1 = D("h1", [16, 128, 1024], kind="ExternalOutput") if link is None else None
    hscr = D("hscr", [16, 128, 1024], kind="Internal")
    dbgo = {}
    if dbg:
        dbgo["d_colmod"] = D("d_colmod", [128, 64], kind="ExternalOutput")
        dbgo["d_g1"] = D("d_g1", [128, 1024], kind="ExternalOutput")
        dbgo["d_kvnT"] = D("d_kvnT", [128, 2 * 8448], BF16, kind="ExternalOutput")
        dbgo["d_KT"] = D("d_KT", [96, 8448], BF16, kind="ExternalOutput")
        dbgo["d_qT"] = D("d_qT", [96, 8 * 2048], BF16, kind="ExternalOutput")
        dbgo["d_OnT"] = D("d_OnT", [64, 8 * 2048], BF16, kind="ExternalOutput")
        dbgo["d_h0a"] = D("d_h0a", [16, 128, 1024], kind="ExternalOutput")

    identb = A.alloc([128], BF16)
    kk = K(S, identb)
    ones_f = A.alloc([128], F32)
    colmod = A.alloc([32, 2], F32)
    g1b = A.alloc([1024], F32); g2b = A.alloc([1024], F32)
    lngb = A.alloc([2, 1024], F32); lnbb = A.alloc([2, 1024], F32)
    epsc = A.alloc([4], F32)
    c.eps_ln = epsc[:, 0:1]
    eps_rms = epsc[:, 1:2]
    bconst = Buf("const")
    S.dma("pool", identb, ident[:, :], w=[kk.bident])
    kk.memset("dve", ones_f, 1.0, w=[bconst])
    kk.memset("dve", epsc[:, 0:1], LN_EPS, w=[bconst])
    kk.memset("dve", epsc[:, 1:2], RMS_EPS, w=[bconst])
    for i in range(2):
        S.dma("sp", lngb[:, i, :], lng[i, :].partition_broadcast(128), w=[bconst], key=f"c{i}a")
        S.dma("sp", lnbb[:, i, :], lnb[i, :].partition_broadcast(128), w=[bconst], key=f"c{i}b")
    mP = A.mark()

    s2 = A.alloc([8, 2], F32); bs2 = Buf()
    s_bc = A.alloc([8, 128], F32); bsbc = Buf()
    wblk = [A.alloc([8, 512], F32) for _ in range(2)]; bwblk = [Buf(), Buf()]
    brow = [A.alloc([1024], F32) for _ in range(2)]; bbrow = [Buf(), Buf()]
    abc = A.alloc([32], F32); babc = Buf()
    S.dma("sp", s2.rearrange("p a b -> p (a b)"), cs[:, :], w=[bs2])
    S.dma("sp", abc, adabcol[:, :], w=[babc])
    S.dma("sp", brow[0], adabrow[0, 2048:3072].partition_broadcast(128), w=[bbrow[0]])
    S.dma("sp", brow[1], adabrow[0, 5120:6144].partition_broadcast(128), w=[bbrow[1]])
    kk.act(s2, s2, AF.Silu, r=[bs2], w=[bs2])
    kk.cp("dve", s_bc, bcast_mid(s2[:, :, 0], 128), r=[bs2], w=[bsbc])
    ps_col = psbank(c, 0, 64).rearrange("p (a b) -> p a b", a=32); bpscol = Buf()
    ps_row = [psbank(c, 1), psbank(c, 2)]; bpsrow = [Buf(), Buf()]
    adaw_v = adaw.rearrange("(kc p) n -> p kc n", p=128)
    colidx = {0: 0, 1: 1, 3: 2, 4: 3}
    gdst = {2: (g1b, brow[0], bbrow[0]), 5: (g2b, brow[1], bbrow[1])}
    for bi in range(12):
        v, half = bi // 2, bi % 2
        wb, bwb = wblk[bi % 2], bwblk[bi % 2]
        S.dma("sp", wb, adaw_v[:, :, bi * 512:(bi + 1) * 512], w=[bwb])
        if v in colidx:
            for fcl in range(4):
                slot = colidx[v] * 8 + half * 4 + fcl
                for kc in range(8):
                    kk.mm(ps_col[:, slot, :], wb[:, kc, fcl * 128:(fcl + 1) * 128], s2[:, kc, :], kc == 0, kc == 7,
                          r=[bwb, bs2], w=[bpscol])
        else:
            pr, bpr = ps_row[half], bpsrow[half]
            for kc in range(8):
                kk.mm(pr, s_bc[:, kc, :], wb[:, kc, :], kc == 0, kc == 7, r=[bwb, bsbc], w=[bpr])
            gt, brt, bbrt = gdst[v]
            kk.tt("dve", gt[:, half * 512:(half + 1) * 512], pr, brt[:, half * 512:(half + 1) * 512], ALU.add,
                  r=[bpr, bbrt], w=[bconst])
    kk.tt("dve", colmod, ps_col, bcast_mid(abc, 2), ALU.add, r=[bpscol, babc], w=[bconst])
    for ci in (1, 3):
        kk.ts("dve", colmod[:, ci * 8:(ci + 1) * 8, :], colmod[:, ci * 8:(ci + 1) * 8, :], 1.0, None, ALU.add, None,
              r=[bconst], w=[bconst])
    if dbg:
        S.dma("sp", dbgo["d_colmod"][:, :], colmod.rearrange("p a b -> p (a b)"), r=[bconst], key="dbg")
        S.dma("sp", dbgo["d_g1"][:, :], g1b, r=[bconst], key="dbg")
    S.barrier()
    if stop_after == "A":
        S.emit(); c.st.close(); return nc
    A.reset(mP)

    upool = A.alloc([18, 512], BF16); bup = [Buf() for _ in range(18)]
    mM = A.mark()
    kvnT = A.alloc([2, 8448], BF16); bkvnT = Buf()
    KT = [A.alloc([8448], BF16, parts=96) for _ in range(2)]; bKT = [Buf(), Buf()]
    mX = A.mark()
    qT = A.alloc([8, 2048], BF16, parts=96); bqT = Buf()
    mD = A.mark()
    qnT = A.alloc([2, 2048], BF16); bqnT = Buf()
    mX1 = A.mark()
    winb = A.alloc([8, 1056], BF16); bwin = Buf()
    xin = [A.alloc([1024], F32) for _ in range(3)]; bxin = [Buf() for _ in range(3)]
    xbf = [A.alloc([1024], BF16) for _ in range(2)]; bxbf = [Buf(), Buf()]
    tmpT = [A.alloc([8, 128], F32) for _ in range(2)]; btmpT = [Buf(), Buf()]
    uT = [A.alloc([8, 128], BF16) for _ in range(2)]; buT = [Buf(), Buf()]
    rt = [A.alloc([32], F32) for _ in range(2)]; brt_ = [Buf(), Buf()]
    kvrow = [A.alloc([352], BF16) for _ in range(2)]; bkvrow = [Buf(), Buf()]
    ropA = [A.alloc([32], F32) for _ in range(2)]; ropB = [A.alloc([32], F32) for _ in range(2)]; brop = [Buf(), Buf()]
    junk = A.alloc([256], F32); bjunk = Buf()
    sm = [A.alloc([8], F32) for _ in range(2)]; bsm = [Buf(), Buf()]
    qn = [A.alloc([256], BF16) for _ in range(2)]; bqn = [Buf(), Buf()]
    S.dma("pool", winb, win.rearrange("(kc p) n -> p kc n", p=128), w=[bwin])
    for i in range(2):
        kk.memset("pool", kvrow[i], 0.0, w=[bkvrow[i]])
    psT = [psbank(c, 0, 1024, BF16).rearrange("p (a b) -> p a b", a=8),
           psbank(c, 1, 1024, BF16).rearrange("p (a b) -> p a b", a=8)]
    bpsT = [Buf(), Buf()]
    ps_kv = [psbank(c, 2, 288), psbank(c, 3, 288)]; bpskv = [Buf(), Buf()]
    ps_p = psbank(c, 4); bpsp = Buf()
    ps_q = psbank(c, 5, 256); bpsq = Buf()
    psT2 = psbank(c, 6, 640, BF16).rearrange("p (a b) -> p a b", a=5); bpsT2 = Buf()
    psT3 = psbank(c, 7, 256, BF16).rearrange("p (a b) -> p a b", a=2); bpsT3 = Buf()

    tiles = [("halo", 0), ("halo", 1)] + [("kv", i) for i in range(66)]
    NT = len(tiles)

    def stageA(n):
        kind, i = tiles[n]
        src_ap = xhalo[i] if kind == "halo" else xkv[i]
        nmod = 1 if (kind == "kv" and i < 2) else 0
        xi, bxi = xin[n % 3], bxin[n % 3]
        S.dma("sp", xi, src_ap, w=[bxi])
        xb_, bxb_ = xbf[n % 2], bxbf[n % 2]
        kk.cp("act", xb_, xi, r=[bxi], w=[bxb_])
        pT, bpT = psT[n % 2], bpsT[n % 2]
        for kc in range(8):
            kk.tr(pT[:, kc, :], xb_[:, kc * 128:(kc + 1) * 128], r=[bxb_], w=[bpT])
        tT, btT = tmpT[n % 2], btmpT[n % 2]
        u, bu = uT[n % 2], buT[n % 2]
        kk.tt("dve", tT, pT, bcast_mid(colmod[:, 8:16, nmod], 128), ALU.mult, r=[bpT, bconst], w=[btT])
        kk.tt("dve", u, tT, bcast_mid(colmod[:, 0:8, nmod], 128), ALU.add, r=[btT, bconst], w=[bu])

    def stageB(n):
        kind, i = tiles[n]
        u, bu = uT[n % 2], buT[n % 2]
        if kind == "halo":
            for kc in range(8):
                kk.mm(ps_p, u[:, kc, :], winb[:, kc, 0:512], kc == 0, kc == 7, r=[bu, bwin], w=[bpsp])
            ui = 0 if i == 0 else 17
            kk.cp("act", upool[:, ui, :], ps_p, r=[bpsp], w=[bup[ui]])
            return
        is_own = 2 <= i < 18
        il = i - 2
        r_, br_ = rt[n % 2], brt_[n % 2]
        S.dma("sp", r_, ropet[i], w=[br_])
        pkv, bpkv = ps_kv[n % 2], bpskv[n % 2]
        for kc in range(8):
            kk.mm(pkv, u[:, kc, :], winb[:, kc, 768:1056], kc == 0, kc == 7, r=[bu, bwin], w=[bpkv])
        if is_own:
            for kc in range(8):
                kk.mm(ps_p, u[:, kc, :], winb[:, kc, 0:512], kc == 0, kc == 7, r=[bu, bwin], w=[bpsp])
            for kc in range(8):
                kk.mm(ps_q, u[:, kc, :], winb[:, kc, 512:768], kc == 0, kc == 7, r=[bu, bwin], w=[bpsq])
        s_, bs_ = sm[n % 2], bsm[n % 2]
        row, brow_ = kvrow[n % 2], bkvrow[n % 2]
        kk.act(junk, pkv[:, 0:256], AF.Square, r=[bpkv], w=[bjunk, bs_], accum=s_[:, 0:1])
        kk.act(s_[:, 1:2], s_[:, 0:1], AF.Sqrt, r=[bs_], w=[bs_], bias=eps_rms, scale=1.0 / 256)
        kk.recip(s_[:, 2:3], s_[:, 1:2], r=[bs_], w=[bs_])
        kk.ts("dve", row[:, 0:256], pkv[:, 0:256], s_[:, 2:3], None, ALU.mult, None, r=[bpkv, bs_], w=[brow_])
        xr = pkv[:, 256:288].rearrange("p (a h e) -> p a h e", a=2, h=2)
        cosb = r_[:, 0:16].rearrange("p (a e) -> p a e", a=2).unsqueeze(2).to_broadcast([128, 2, 2, 8])
        sinb = r_[:, 16:32].rearrange("p (a e) -> p a e", a=2).unsqueeze(2).to_broadcast([128, 2, 2, 8])
        rA = ropA[n % 2].rearrange("p (a h e) -> p a h e", a=2, h=2)
        rB = ropB[n % 2].rearrange("p (a h e) -> p a h e", a=2, h=2)
        brp = brop[n % 2]
        kk.tt("dve", rA, xr, cosb, ALU.mult, r=[bpkv, br_], w=[brp])
        kk.tt("dve", rB, xr, sinb, ALU.mult, r=[bpkv, br_], w=[brp])
        ro = row[:, 320:352].rearrange("p (a h e) -> p a h e", a=2, h=2)
        kk.tt("pool", ro[:, :, 0, :], rA[:, :, 0, :], rB[:, :, 1, :], ALU.subtract, r=[brp], w=[brow_])
        kk.tt("pool", ro[:, :, 1, :], rA[:, :, 1, :], rB[:, :, 0, :], ALU.add, r=[brp], w=[brow_])
        if is_own:
            kk.cp("act", upool[:, il + 1, :], ps_p, r=[bpsp], w=[bup[il + 1]])
            qn_, bqn_ = qn[n % 2], bqn[n % 2]
            kk.act(junk, ps_q, AF.Square, r=[bpsq], w=[bjunk, bs_], accum=s_[:, 4:5])
            kk.act(s_[:, 5:6], s_[:, 4:5], AF.Sqrt, r=[bs_], w=[bs_], bias=eps_rms, scale=1.0 / 256)
            kk.recip(s_[:, 6:7], s_[:, 5:6], r=[bs_], w=[bs_])
            kk.ts("dve", qn_, ps_q, s_[:, 6:7], 96.0 ** -0.5, ALU.mult, ALU.mult, r=[bpsq, bs_], w=[bqn_])

    def stageC(n):
        kind, i = tiles[n]
        if kind == "halo":
            return
        is_own = 2 <= i < 18
        il = i - 2
        row, brow_ = kvrow[n % 2], bkvrow[n % 2]
        kk.tr(psT2[:, 0, :], row[:, 0:128], r=[brow_], w=[bpsT2])
        kk.tr(psT2[:, 1, :], row[:, 128:256], r=[brow_], w=[bpsT2])
        kk.tr(psT2[0:96, 2, :], row[:, 256:352], r=[brow_], w=[bpsT2])
        kk.cp("act", kvnT[:, :, i * 128:(i + 1) * 128], psT2[:, 0:2, :], r=[bpsT2], w=[bkvnT])
        kk.cp("act", KT[0][64:96, i * 128:(i + 1) * 128], psT2[64:96, 2, :], r=[bpsT2], w=[bKT[0]])
        kk.cp("act", KT[1][64:96, i * 128:(i + 1) * 128], psT2[64:96, 2, :], r=[bpsT2], w=[bKT[1]])
        if is_own:
            qn_, bqn_ = qn[n % 2], bqn[n % 2]
            kk.tr(psT3[:, 0, :], qn_[:, 0:128], r=[bqn_], w=[bpsT3])
            kk.tr(psT3[:, 1, :], qn_[:, 128:256], r=[bqn_], w=[bpsT3])
            kk.cp("dve", qnT[:, :, il * 128:(il + 1) * 128], psT3, r=[bpsT3], w=[bqnT])

    SKB, SKC = 1, 2
    for s_i in range(NT + SKC):
        if s_i < NT:
            stageA(s_i)
        if 0 <= s_i - SKB < NT:
            stageB(s_i - SKB)
        if 0 <= s_i - SKC < NT:
            stageC(s_i - SKC)
    if dbg:
        S.dma("sp", dbgo["d_kvnT"][:, :], kvnT.rearrange("p a b -> p (a b)"), r=[bkvnT], key="dbg")
    S.barrier()
    if stop_after == "B":
        S.emit(); c.st.close(); return nc
    A.reset(mX1)

    qupb = A.alloc([2, 768], BF16); bqup = Buf()
    qnc = A.alloc([2], F32); bqnc = Buf()
    qrow = [A.alloc([8, 96], BF16) for _ in range(2)]; bqrow = [Buf(), Buf()]
    rtq = [A.alloc([512], F32) for _ in range(2)]; brtq = [Buf(), Buf()]
    xs = [A.alloc([8, 32], F32) for _ in range(2)]; bxs = [Buf(), Buf()]
    qA = [A.alloc([256], F32) for _ in range(2)]; qB = [A.alloc([256], F32) for _ in range(2)]; bqAB = [Buf(), Buf()]
    qo = [A.alloc([8, 32], BF16) for _ in range(2)]; bqo = [Buf(), Buf()]
    S.dma("pool", qupb, qup.rearrange("(kc p) n -> p kc n", p=128), w=[bqup])
    S.dma("sp", qnc, qnorm[:, :], w=[bqnc])
    for kc in range(2):
        kk.ts("dve", qupb[:, kc, :], qupb[:, kc, :], qnc[:, kc:kc + 1], None, ALU.mult, None, r=[bqup, bqnc], w=[bqup])
    ps_qa = [psbank(c, 0, 1024, nb=2), psbank(c, 2, 1024, nb=2)]; bpsqa = [Buf(), Buf()]
    ps_qt = [psbank(c, 4, 1024, BF16, parts=96).rearrange("p (a b) -> p a b", a=8),
             psbank(c, 5, 1024, BF16, parts=96).rearrange("p (a b) -> p a b", a=8)]
    bpsqt = [Buf(), Buf()]
    for il in range(16):
        p = il % 2
        S.dma("sp", rtq[p], ropeq[il], w=[brtq[p]])
        for (h0, nh) in ((0, 5), (5, 3)):
            pq = ps_qa[p][:, (0 if h0 == 0 else 512):(0 if h0 == 0 else 512) + nh * 96]
            for kc in range(2):
                kk.mm(pq, qnT[:, kc, il * 128:(il + 1) * 128], qupb[:, kc, h0 * 96:(h0 + nh) * 96], kc == 0, kc == 1,
                      r=[bqnT, bqup], w=[bpsqa[p]])
            qv = pq.rearrange("p (h d) -> p h d", h=nh)
            kk.cp("act", qrow[p][:, h0:h0 + nh, 0:64], qv[:, :, 0:64], r=[bpsqa[p]], w=[bqrow[p]])
            kk.cp("act", xs[p][:, h0:h0 + nh, :], qv[:, :, 64:96], r=[bpsqa[p]], w=[bxs[p]])
        xs2 = xs[p].rearrange("p h d -> p (h d)")
        kk.tt("dve", qA[p], xs2, rtq[p][:, 0:256], ALU.mult, r=[bxs[p], brtq[p]], w=[bqAB[p]])
        kk.tt("dve", qB[p], xs2, rtq[p][:, 256:512], ALU.mult, r=[bxs[p], brtq[p]], w=[bqAB[p]])
        rA = qA[p].rearrange("p (m g e) -> p m g e", g=2, e=8)
        rB = qB[p].rearrange("p (m g e) -> p m g e", g=2, e=8)
        ro = qo[p].rearrange("p h (a g e) -> p (h a) g e", a=2, g=2)
        kk.tt("pool", ro[:, :, 0, :], rA[:, :, 0, :], rB[:, :, 1, :], ALU.subtract, r=[bqAB[p]], w=[bqo[p]])
        kk.tt("pool", ro[:, :, 1, :], rA[:, :, 1, :], rB[:, :, 0, :], ALU.add, r=[bqAB[p]], w=[bqo[p]])
        kk.cp("pool", qrow[p][:, :, 64:96], qo[p], r=[bqo[p]], w=[bqrow[p]])
        for h in range(8):
            kk.tr(ps_qt[p][:, h, :], qrow[p][:, h, :], r=[bqrow[p]], w=[bpsqt[p]])
        kk.cp("dve" if il % 2 else "act", qT[:, :, il * 128:(il + 1) * 128], ps_qt[p], r=[bpsqt[p]], w=[bqT])
    if dbg:
        S.dma("sp", dbgo["d_qT"][:, :], qT.rearrange("p a b -> p (a b)"), r=[bqT], key="dbg")
    S.barrier()
    if stop_after == "C":
        S.emit(); c.st.close(); return nc
    A.reset(mD)

    OnT = A.alloc([8, 2048], BF16, parts=64); bOnT = Buf()
    mE = A.mark()
    kvkb = A.alloc([2, 512], BF16); kvvb = A.alloc([2, 512], BF16); bkvw = Buf()
    kvnc = A.alloc([2], F32); bkvnc = Buf()
    Vb = [A.alloc([66, 65], BF16) for _ in range(2)]; bVb = [Buf(), Buf()]
    PT = [A.alloc([512], BF16) for _ in range(3)]; bPT = [Buf() for _ in range(3)]
    rrow = A.alloc([512], F32, parts=65); brrow = Buf()
    osb = A.alloc([512], F32, parts=64); bosb = Buf()
    S.dma("pool", kvkb, kvupk.rearrange("(kc p) n -> p kc n", p=128), w=[bkvw], key="kvk")
    S.dma("pool", kvvb, kvupv.rearrange("(kc p) n -> p kc n", p=128), w=[bkvw], key="kvv")
    S.dma("sp", kvnc, kvnorm[:, :], w=[bkvnc])
    for kc in range(2):
        kk.ts("dve", kvkb[:, kc, :], kvkb[:, kc, :], kvnc[:, kc:kc + 1], None, ALU.mult, None, r=[bkvw, bkvnc], w=[bkvw])
        kk.ts("dve", kvvb[:, kc, :], kvvb[:, kc, :], kvnc[:, kc:kc + 1], None, ALU.mult, None, r=[bkvw, bkvnc], w=[bkvw])
    for i in range(2):
        kk.memset("pool", Vb[i][:, :, 64:65], 1.0, w=[bVb[i]])
    ps_s = [psbank(c, b) for b in range(3)]; bpss = [Buf() for _ in range(3)]
    ps_o = [psbank(c, 3, parts=65), psbank(c, 4, parts=65)]; bpso = [Buf(), Buf()]
    ps_k = [psbank(c, 5), psbank(c, 6)]; bpsk = [Buf(), Buf()]
    ps_b = psbank(c, 7, parts=64); bpsb = Buf()
    nkb = 66
    ei = 0
    def kv_for_head(h):
        KTb, bKTb = KT[h % 2], bKT[h % 2]
        V, bV = Vb[h % 2], bVb[h % 2]
        for ch in range(17):
            c0 = ch * 512
            c1 = min(c0 + 512, 8448)
            pk, bpk = ps_k[ch % 2], bpsk[ch % 2]
            for kc in range(2):
                kk.mm(pk[0:64, 0:c1 - c0], kvkb[:, kc, h * 64:(h + 1) * 64], kvnT[:, kc, c0:c1], kc == 0, kc == 1,
                      r=[bkvw, bkvnT], w=[bpk])
            kk.cp("dve", KTb[0:64, c0:c1], pk[0:64, 0:c1 - c0], r=[bpk], w=[bKTb])
        for g0 in range(0, 66, 8):
            g1 = min(g0 + 8, 66)
            pk, bpk = ps_k[(g0 // 8 + 1) % 2], bpsk[(g0 // 8 + 1) % 2]
            pv = pk.rearrange("p (a b) -> p a b", a=8)
            for t in range(g0, g1):
                for kc in range(2):
                    kk.mm(pv[:, t - g0, :], kvnT[:, kc, t * 128:(t + 1) * 128], kvvb[:, kc, h * 64:(h + 1) * 64],
                          kc == 0, kc == 1, r=[bkvnT, bkvw], w=[bpk])
            kk.cp("dve", V[:, g0:g1, 0:64], pv[:, 0:g1 - g0, :], r=[bpk], w=[bV])

    kv_for_head(0)
    for h in range(8):
        KTb, bKTb = KT[h % 2], bKT[h % 2]
        V, bV = Vb[h % 2], bVb[h % 2]
        for qc in range(4):
            if qc == 1 and h + 1 < 8:
                kv_for_head(h + 1)
            po, bpo = ps_o[ei % 2], bpso[ei % 2]
            ei += 1
            LOOK = 2

            def qk_exp(kb):
                j = kb % 3
                kk.mm(ps_s[j], KTb[0:96, kb * 128:(kb + 1) * 128], qT[0:96, h, qc * 512:(qc + 1) * 512], True, True,
                      r=[bKTb, bqT], w=[bpss[j]])
                kk.act(PT[j], ps_s[j], AF.Exp, r=[bpss[j]], w=[bPT[j]])

            for kb in range(min(LOOK, nkb)):
                qk_exp(kb)
            for kb in range(nkb):
                if kb + LOOK < nkb:
                    qk_exp(kb + LOOK)
                j = kb % 3
                kk.mm(po[0:65, :], V[:, kb, 0:65], PT[j], kb == 0, kb == nkb - 1, r=[bV, bPT[j]], w=[bpo])
            kk.recip(rrow[64:65, :], po[64:65, :], r=[bpo], w=[brrow])
            kk.mm(ps_b, ones_f[64:65, 0:64], rrow[64:65, :], True, True, r=[brrow, bconst], w=[bpsb])
            kk.cp("dve", osb, po[0:64, :], r=[bpo], w=[bosb])
            kk.tt("dve", OnT[:, h, qc * 512:(qc + 1) * 512], ps_b, osb, ALU.mult, r=[bpsb, bosb], w=[bOnT])
    if dbg:
        S.dma("sp", dbgo["d_KT"][:, :], KT[1], r=[bKT[1]], key="dbg")
        S.dma("sp", dbgo["d_OnT"][:, :], OnT.rearrange("p a b -> p (a b)"), r=[bOnT], key="dbg")
    S.barrier()
    if stop_after == "D":
        S.emit(); c.st.close(); return nc

    A2 = Arena(c.arena_t, mM, mX)
    woutp = A2.alloc([4, 1024], BF16); wouta = A2.alloc([8, 1024], BF16, parts=64); bwo = Buf()
    bandb = A2.alloc([28, 128], BF16); bband = Buf()
    poolwb = A2.alloc([4, 128], BF16); bpw = Buf()
    psc = A2.alloc([4], F32); bpsc = Buf()
    dT = A2.alloc([4, 512], BF16); bdT = Buf()
    poolT = A2.alloc([4, 512], BF16); bpoolT = Buf()
    xres = [A2.alloc([1024], F32) for _ in range(2)]; bxres = [Buf(), Buf()]
    tmpE = [A2.alloc([1024], F32) for _ in range(2)]; btmpE = [Buf(), Buf()]
    outE = [A2.alloc([1024], F32) for _ in range(2)]; boutE = [Buf(), Buf()]
    smE = [A2.alloc([16], F32) for _ in range(2)]; bsmE = [Buf(), Buf()]
    S.dma("pool", woutp, wout[0:512, :].rearrange("(g p) n -> p g n", p=128), w=[bwo], key="wo1")
    S.dma("pool", wouta, wout[512:1024, :].rearrange("(h p) n -> p h n", p=64), w=[bwo], key="wo2")
    S.dma("pool", bandb, bands.rearrange("k g s t -> s (k g) t"), w=[bband])
    S.dma("pool", poolwb, poolw.rearrange("g i o -> i g o"), w=[bpw])
    S.dma("sp", psc, pscale[:, :], w=[bpsc])
    ps_d = [psbank(c, b) for b in range(4)]; bpsd = [Buf() for _ in range(4)]
    ps_y = [psbank(c, 4), psbank(c, 5)]; bpsy = [Buf(), Buf()]
    ps_out = psbank(c, 6, 1024, nb=2); bpsout = Buf()
    for qc in range(4):
        for g in range(4):
            for tl in range(4):
                il = qc * 4 + tl
                if il == 0:
                    kinds = (3, 4, 2)
                elif il == 15:
                    kinds = (0, 5, 6)
                else:
                    kinds = (0, 1, 2)
                for o in range(3):
                    kk.mm(ps_d[g][:, tl * 128:(tl + 1) * 128], upool[:, il + o, g * 128:(g + 1) * 128],
                          bandb[:, kinds[o] * 4 + g, :], o == 0, o == 2, r=[bup[il + o], bband], w=[bpsd[g]])
            kk.cp("act", dT[:, g, :], ps_d[g], r=[bpsd[g]], w=[bdT])
            kk.mm(ps_y[g % 2], poolwb[:, g, :], dT[:, g, :], True, True, r=[bpw, bdT], w=[bpsy[g % 2]])
            kk.ts("dve", poolT[:, g, :], ps_y[g % 2], psc[:, g:g + 1], None, ALU.mult, None, r=[bpsy[g % 2], bpsc], w=[bpoolT])
        for tl in range(4):
            il = qc * 4 + tl
            p = il % 2
            S.dma("sp", xres[p], xkv[il + 2], w=[bxres[p]])
            for half in range(2):
                n0 = half * 512
                for g in range(4):
                    kk.mm(ps_out[:, n0:n0 + 512], poolT[:, g, tl * 128:(tl + 1) * 128], woutp[:, g, n0:n0 + 512],
                          g == 0, False, r=[bpoolT, bwo], w=[bpsout])
                for h in range(8):
                    kk.mm(ps_out[:, n0:n0 + 512], OnT[0:64, h, il * 128:(il + 1) * 128], wouta[0:64, h, n0:n0 + 512],
                          False, h == 7, r=[bOnT, bwo], w=[bpsout])
            ln_tile(c, kk, ps_out, bpsout, xres[p], bxres[p], g1b, lngb[:, 0, :], lnbb[:, 0, :], bconst,
                    tmpE[p], btmpE[p], outE[p], boutE[p], smE[p], bsmE[p])
            S.dma("sp", hscr[il], outE[p], r=[boutE[p]], key=f"hs{p}")
            if dbg:
                S.dma("sp", dbgo["d_h0a"][il], outE[p], r=[boutE[p]], key=f"dbgh{p}")
    S.barrier()
    if stop_after == "E":
        S.emit(); c.st.close(); return nc
    A.reset(mP)

    h1T = A.alloc([8, 2048], BF16); bh1T = Buf()
    hidT = A.alloc([22, 1024], BF16); bhid = Buf()
    wdb = A.alloc([22, 1024], BF16); bwd = Buf()
    wgb = [A.alloc([8, 256], BF16) for _ in range(2)]; wub = [A.alloc([8, 256], BF16) for _ in range(2)]
    bwgu = [Buf(), Buf()]
    xinF = [A.alloc([1024], F32) for _ in range(2)]; bxinF = [Buf(), Buf()]
    xbfF = [A.alloc([1024], BF16) for _ in range(2)]; bxbfF = [Buf(), Buf()]
    tmpTF = [A.alloc([8, 128], F32) for _ in range(2)]; btmpTF = [Buf(), Buf()]
    sg = [A.alloc([512], BF16) for _ in range(2)]; bsg = [Buf(), Buf()]
    tmpF = [A.alloc([1024], F32) for _ in range(2)]; btmpF = [Buf(), Buf()]
    outF = [A.alloc([1024], F32) for _ in range(2)]; boutF = [Buf(), Buf()]
    smF = [A.alloc([16], F32) for _ in range(2)]; bsmF = [Buf(), Buf()]
    ob16, bob16 = xbfF, bxbfF
    wd_v = wd.rearrange("(c p) n -> p c n", p=128)
    for q4 in range(2):
        S.dma("pool", wdb[:, q4 * 11:(q4 + 1) * 11, :], wd_v[:, q4 * 11:(q4 + 1) * 11, :], w=[bwd], key=f"wd{q4}")
    psTF = [psbank(c, 0, 1024, BF16).rearrange("p (a b) -> p a b", a=8),
            psbank(c, 1, 1024, BF16).rearrange("p (a b) -> p a b", a=8)]
    bpsTF = [Buf(), Buf()]
    for il in range(16):
        p = il % 2
        S.dma("sp", xinF[p], hscr[il], w=[bxinF[p]])
        kk.cp("act", xbfF[p], xinF[p], r=[bxinF[p]], w=[bxbfF[p]])
        for kc in range(8):
            kk.tr(psTF[p][:, kc, :], xbfF[p][:, kc * 128:(kc + 1) * 128], r=[bxbfF[p]], w=[bpsTF[p]])
        kk.tt("dve", tmpTF[p], psTF[p], bcast_mid(colmod[:, 24:32, 0], 128), ALU.mult, r=[bpsTF[p], bconst], w=[btmpTF[p]])
        kk.tt("dve", h1T[:, :, il * 128:(il + 1) * 128], tmpTF[p], bcast_mid(colmod[:, 16:24, 0], 128), ALU.add,
              r=[btmpTF[p], bconst], w=[bh1T])
    ps_g = [psbank(c, 2), psbank(c, 3)]; bpsg = [Buf(), Buf()]
    ps_u = [psbank(c, 4), psbank(c, 5)]; bpsu = [Buf(), Buf()]
    ps_o2s = [psbank(c, 6, 1024, nb=2), psbank(c, 0, 1024, nb=2)]
    bpso2s = [[Buf()], [bpsTF[0], bpsTF[1]]]
    wg_v = wg.rearrange("(kc p) n -> p kc n", p=128)
    wu_v = wu.rearrange("(kc p) n -> p kc n", p=128)
    wi = 0
    ci = 0
    for tb in range(2):
        for fb in range(11):
            p = wi % 2
            wi += 1
            S.dma("pool", wgb[p], wg_v[:, :, fb * 256:(fb + 1) * 256], w=[bwgu[p]], key=f"wg{p}")
            S.dma("pool", wub[p], wu_v[:, :, fb * 256:(fb + 1) * 256], w=[bwgu[p]], key=f"wu{p}")
            for fc in range(2):
                ffc = fb * 2 + fc
                for ts_ in range(2):
                    q = ci % 2
                    ci += 1
                    t0 = tb * 1024 + ts_ * 512
                    for kc in range(8):
                        kk.mm(ps_g[q], wgb[p][:, kc, fc * 128:(fc + 1) * 128], h1T[:, kc, t0:t0 + 512], kc == 0, kc == 7,
                              r=[bwgu[p], bh1T], w=[bpsg[q]])
                    for kc in range(8):
                        kk.mm(ps_u[q], wub[p][:, kc, fc * 128:(fc + 1) * 128], h1T[:, kc, t0:t0 + 512], kc == 0, kc == 7,
                              r=[bwgu[p], bh1T], w=[bpsu[q]])
                    kk.act(sg[q], ps_g[q], AF.Silu, r=[bpsg[q]], w=[bsg[q]])
                    kk.tt("dve", hidT[:, ffc, ts_ * 512:(ts_ + 1) * 512], ps_u[q], sg[q], ALU.mult, r=[bpsu[q], bsg[q]], w=[bhid])
        for tl in range(8):
            il = tb * 8 + tl
            p = il % 2
            S.dma("sp", xinF[p], hscr[il], w=[bxinF[p]])
            ps_o2, bpso2 = ps_o2s[il % 2], bpso2s[il % 2]
            for half in range(2):
                n0 = half * 512
                for ffc in range(22):
                    kk.mm(ps_o2[:, n0:n0 + 512], hidT[:, ffc, tl * 128:(tl + 1) * 128], wdb[:, ffc, n0:n0 + 512],
                          ffc == 0, ffc == 21, r=[bhid, bwd], w=bpso2)
            ln_tile(c, kk, ps_o2, bpso2, xinF[p], bxinF[p], g2b, lngb[:, 1, :], lnbb[:, 1, :], bconst,
                    tmpF[p], btmpF[p], outF[p], boutF[p], smF[p], bsmF[p])
            if link is None:
                S.dma("sp", h1[il], outF[p], r=[boutF[p]], key=f"ho{p}")
            else:
                S.dma("sp", link["h1res"][il], outF[p], r=[boutF[p]], w=[link["bh1res"][il]], key=f"ho{p}")
                kk.cp("act", ob16[p], outF[p], r=[boutF[p]], w=[bob16[p]])
                S.dma("sp", link["xsrc"][il // 4][(il % 4) * 128:(il % 4 + 1) * 128, :], ob16[p], r=[bob16[p]],
                      w=[link["bxsrc"][il // 4]], key=f"hx{p}")
    S.barrier()
    if not own:
        return None
    S.emit()
    c.st.close()
    return nc


def mod_phase(c, kk, cs, adaw, adabcol, adabrow, colmod, g1b, g2b, bconst, need=(0, 1, 2, 3, 4, 5)):
    S, A = c.S, c.A
    m0 = A.mark()
    s2 = A.alloc([8, 2], F32); bs2 = Buf()
    s_bc = A.alloc([8, 128], F32); bsbc = Buf()
    wblk = [A.alloc([8, 512], F32) for _ in range(2)]; bwblk = [Buf(), Buf()]
    brow = [A.alloc([1024], F32) for _ in range(2)]; bbrow = [Buf(), Buf()]
    abc = A.alloc([32], F32); babc = Buf()
    S.dma("sp", s2.rearrange("p a b -> p (a b)"), cs[:, :], w=[bs2])
    S.dma("sp", abc, adabcol[:, :], w=[babc])
    S.dma("sp", brow[0], adabrow[0, 2048:3072].partition_broadcast(128), w=[bbrow[0]])
    S.dma("sp", brow[1], adabrow[0, 5120:6144].partition_broadcast(128), w=[bbrow[1]])
    kk.act(s2, s2, AF.Silu, r=[bs2], w=[bs2])
    kk.cp("dve", s_bc, bcast_mid(s2[:, :, 0], 128), r=[bs2], w=[bsbc])
    ps_col = psbank(c, 0, 64).rearrange("p (a b) -> p a b", a=32); bpscol = Buf()
    ps_row = [psbank(c, 1), psbank(c, 2)]; bpsrow = [Buf(), Buf()]
    adaw_v = adaw.rearrange("(kc p) n -> p kc n", p=128)
    colidx = {0: 0, 1: 1, 3: 2, 4: 3}
    gdst = {2: (g1b, brow[0], bbrow[0]), 5: (g2b, brow[1], bbrow[1])}
    for bi in range(12):
        v, half = bi // 2, bi % 2
        if v not in need or (v not in colidx and gdst[v][0] is None):
            continue
        wb, bwb = wblk[bi % 2], bwblk[bi % 2]
        S.dma("sp", wb, adaw_v[:, :, bi * 512:(bi + 1) * 512], w=[bwb])
        if v in colidx:
            for fcl in range(4):
                slot = colidx[v] * 8 + half * 4 + fcl
                for kc in range(8):
                    kk.mm(ps_col[:, slot, :], wb[:, kc, fcl * 128:(fcl + 1) * 128], s2[:, kc, :], kc == 0, kc == 7,
                          r=[bwb, bs2], w=[bpscol])
        else:
            pr, bpr = ps_row[half], bpsrow[half]
            for kc in range(8):
                kk.mm(pr, s_bc[:, kc, :], wb[:, kc, :], kc == 0, kc == 7, r=[bwb, bsbc], w=[bpr])
            gt, brt, bbrt = gdst[v]
            kk.tt("dve", gt[:, half * 512:(half + 1) * 512], pr, brt[:, half * 512:(half + 1) * 512], ALU.add,
                  r=[bpr, bbrt], w=[bconst])
    for v in need:
        if v not in colidx:
            continue
        ci = colidx[v]
        kk.tt("dve", colmod[:, ci * 8:(ci + 1) * 8, :], ps_col[:, ci * 8:(ci + 1) * 8, :], bcast_mid(abc[:, ci * 8:(ci + 1) * 8], 2),
              ALU.add, r=[bpscol, babc], w=[bconst])
        if ci in (1, 3):
            kk.ts("dve", colmod[:, ci * 8:(ci + 1) * 8, :], colmod[:, ci * 8:(ci + 1) * 8, :], 1.0, None, ALU.add, None,
                  r=[bconst], w=[bconst])
    S.barrier()
    A.reset(m0)


CG = 32


def build_l2(dbg=False, stop_after=None, c=None, pre="", link=None):
    own = c is None
    if own:
        c = new_prog()
    nc, S, A = c.nc, c.S, c.A
    A.reset(0)

    def D(name, shape, dt=F32, kind="ExternalInput"):
        return nc.dram_tensor(pre + name, shape, dt, kind=kind).ap()

    hin = D("hin", [64, 128, 1024]) if link is None else None
    cs = D("cs", [128, 16])
    adaw = D("adaw", [1024, 6144]); adabcol = D("adabcol", [128, 32]); adabrow = D("adabrow", [1, 6144])
    win = D("win", [1024, 768]); convw = D("convw", [128, 18]); convb = D("convb", [128, 6])
    zemb = D("zemb", [33, 8192]); fw1 = D("fw1", [33, 64]); fw2 = D("fw2", [64, 64]); fw3 = D("fw3", [64, 64])
    fout = D("fout", [64, 512]); fbs = D("fbs", [64, 4])
    skipc = D("skipc", [128, 2]); deltac = D("deltac", [128, 2]); tvals = D("tvals", [1, 8192])
    f1cat = D("f1cat", [64, 1024]); f1inv = D("f1inv", [128, 512]); gmat = D("gmat", [128, 128 * 2 * 128])
    ident = D("ident", [128, 128])
    yout = D("yout", [256, 8192], BF16, kind="ExternalOutput") if link is None else None
    pscr = D("pscr", [256, 8192], BF16, kind="Internal")
    x0scr = D("x0scr", [256, 8192], BF16, kind="Internal")
    hfscr = D("hfscr", [256, 8192], BF16, kind="Internal")
    hbscr = D("hbscr", [256, 8192], BF16, kind="Internal")
    yscr = D("yscr", [256, 8192], F32, kind="Internal")
    dbgo = {}
    if dbg:
        dbgo["d_p"] = D("d_p", [256, 8192], BF16, kind="ExternalOutput")
        dbgo["d_x0"] = D("d_x0", [256, 8192], BF16, kind="ExternalOutput")
        dbgo["d_hf"] = D("d_hf", [256, 8192], BF16, kind="ExternalOutput")
        dbgo["d_hb"] = D("d_hb", [256, 8192], BF16, kind="ExternalOutput")
        dbgo["d_y"] = D("d_y", [256, 8192], F32, kind="ExternalOutput")

    identb = A.alloc([128], BF16)
    kk = K(S, identb)
    colmod = A.alloc([32, 2], F32)
    bconst = Buf("const")
    S.dma("pool", identb, ident[:, :], w=[kk.bident])
    mP = A.mark()
    mod_phase(c, kk, cs, adaw, adabcol, adabrow, colmod, None, None, bconst, need=(0, 1))

    zT = A.alloc([6, 8194], BF16); bz = [Buf() for _ in range(6)]
    winb = A.alloc([8, 768], BF16); bwin = Buf()
    cw = A.alloc([18], F32); cb = A.alloc([6], F32); bcw = Buf()
    uTb = [A.alloc([8, 512], BF16) for _ in range(2)]; buTb = [Buf(), Buf()]
    xin = [A.alloc([1024], F32) for _ in range(3)]; bxin = [Buf() for _ in range(3)]
    xbf = [A.alloc([1024], BF16) for _ in range(2)]; bxbf = [Buf(), Buf()]
    tmpT = [A.alloc([8, 128], F32) for _ in range(2)]; btmpT = [Buf(), Buf()]
    ct = [A.alloc([2048], F32) for _ in range(3)]; bct = [Buf() for _ in range(3)]
    ob = [A.alloc([2048], BF16) for _ in range(2)]; bob = [Buf(), Buf()]
    S.dma("pool", winb, win.rearrange("(kc p) n -> p kc n", p=128), w=[bwin])
    S.dma("sp", cw, convw[:, :], w=[bcw], key="cw")
    S.dma("sp", cb, convb[:, :], w=[bcw], key="cb")
    for ch in range(6):
        kk.memset("pool", zT[:, ch, 0:1], 0.0, w=[bz[ch]])
        kk.memset("pool", zT[:, ch, 8193:8194], 0.0, w=[bz[ch]])
    psT = [psbank(c, 0, 1024, BF16).rearrange("p (a b) -> p a b", a=8),
           psbank(c, 1, 1024, BF16).rearrange("p (a b) -> p a b", a=8)]
    bpsT = [Buf(), Buf()]
    ps_z = [psbank(c, 2), psbank(c, 3), psbank(c, 4)]; bpsz = [Buf() for _ in range(3)]
    zi = 0

    def p1_prep(tb):
        u, bu = uTb[tb % 2], buTb[tb % 2]
        for tl in range(4):
            i = tb * 4 + tl
            xi, bxi = xin[i % 3], bxin[i % 3]
            if link is None:
                S.dma("sp", xi, hin[i], w=[bxi])
                kk.cp("act", xbf[i % 2], xi, r=[bxi], w=[bxbf[i % 2]])
            else:
                r_, k_, m_ = i // 16, (i % 16) // 4, i % 4
                S.dma("sp", xbf[i % 2], link["xdst"][k_][512 * r_ + 128 * m_:512 * r_ + 128 * m_ + 128, :],
                      r=[link["bxdst"][k_]], w=[bxbf[i % 2]], key=f"xb{i % 2}")
            for kc in range(8):
                kk.tr(psT[i % 2][:, kc, :], xbf[i % 2][:, kc * 128:(kc + 1) * 128], r=[bxbf[i % 2]], w=[bpsT[i % 2]])
            kk.tt("dve", tmpT[i % 2], psT[i % 2], bcast_mid(colmod[:, 8:16, 0], 128), ALU.mult, r=[bpsT[i % 2], bconst], w=[btmpT[i % 2]])
            kk.tt("dve", u[:, :, tl * 128:(tl + 1) * 128], tmpT[i % 2], bcast_mid(colmod[:, 0:8, 0], 128), ALU.add,
                  r=[btmpT[i % 2], bconst], w=[bu])

    def p1_proj(tb):
        nonlocal zi
        u, bu = uTb[tb % 2], buTb[tb % 2]
        for ch in range(6):
            pz, bpz = ps_z[zi % 3], bpsz[zi % 3]
            zi += 1
            for kc in range(8):
                kk.mm(pz, winb[:, kc, ch * 128:(ch + 1) * 128], u[:, kc, :], kc == 0, kc == 7, r=[bwin, bu], w=[bpz])
            kk.cp("act" if ch % 2 else "dve", zT[:, ch, 1 + tb * 512:1 + (tb + 1) * 512], pz, r=[bpz], w=[bz[ch]])

    p1_prep(0)
    for tb in range(16):
        if tb + 1 < 16:
            p1_prep(tb + 1)
        p1_proj(tb)
    ci = 0

    def sconv(ch, t0, n, dst, bdst, tmp, btmp):
        kk.act(tmp, zT[:, ch, 1 + t0:1 + t0 + n], AF.Identity, r=[bz[ch], bcw], w=[btmp],
               bias=cb[:, ch:ch + 1], scale=cw[:, ch * 3 + 1:ch * 3 + 2])
        kk.stt("dve", tmp, zT[:, ch, t0:t0 + n], cw[:, ch * 3:ch * 3 + 1], tmp, ALU.mult, ALU.add, r=[bz[ch], bcw, btmp], w=[btmp])
        kk.stt("dve", dst, zT[:, ch, 2 + t0:2 + t0 + n], cw[:, ch * 3 + 2:ch * 3 + 3], tmp, ALU.mult, ALU.add,
               r=[bz[ch], bcw, btmp], w=[bdst])

    oi = 0
    for q in range(2):
        for sb in range(4):
            t0 = sb * 2048
            o, bo = ob[oi % 2], bob[oi % 2]; oi += 1
            sconv(q, t0, 2048, o, bo, ct[0], bct[0])
            S.dma("sp", x0scr[q * 128:(q + 1) * 128, t0:t0 + 2048], o, r=[bo], key=f"x0s{oi % 2}")
            if dbg:
                S.dma("sp", dbgo["d_x0"][q * 128:(q + 1) * 128, t0:t0 + 2048], o, r=[bo], key=f"dbg{oi % 2}")
            sconv(2 + q, t0, 2048, ct[1], bct[1], ct[1], bct[1])
            sconv(4 + q, t0, 2048, ct[2], bct[2], ct[2], bct[2])
            o, bo = ob[oi % 2], bob[oi % 2]; oi += 1
            kk.tt("pool", o, ct[1], ct[2], ALU.mult, r=[bct[1], bct[2]], w=[bo])
            S.dma("sp", pscr[q * 128:(q + 1) * 128, t0:t0 + 2048], o, r=[bo], key=f"ps{oi % 2}")
            if dbg:
                S.dma("sp", dbgo["d_p"][q * 128:(q + 1) * 128, t0:t0 + 2048], o, r=[bo], key=f"dbg{oi % 2}")
    S.barrier()
    if stop_after == "P1":
        S.emit(); c.st.close(); return nc
    A.reset(mP)

    PI = math.pi
    ze = A.alloc([8192], F32, parts=33); bze = Buf()
    hA = A.alloc([8192], F32, parts=64); bhA = Buf()
    hB = A.alloc([8192], F32, parts=64); bhB = Buf()
    mt = A.alloc([8192], F32, parts=64); bmt = Buf()
    w1 = A.alloc([64], F32, parts=33); w2 = A.alloc([64], F32, parts=64); w3 = A.alloc([64], F32, parts=64)
    fo = A.alloc([512], F32, parts=64); fb = A.alloc([8], F32, parts=64); bfw = Buf()
    skc = A.alloc([2], F32); dlc = A.alloc([2], F32); bsk = Buf()
    tv = [A.alloc([512], F32) for _ in range(2)]; btv = [Buf(), Buf()]
    dec = [A.alloc([512], F32) for _ in range(2)]; bdec = [Buf(), Buf()]
    o32 = [A.alloc([512], F32) for _ in range(2)]; bo32 = [Buf(), Buf()]
    o16 = [A.alloc([512], BF16) for _ in range(4)]; bo16 = [Buf() for _ in range(4)]
    S.dma("sp", ze, zemb[:, :], w=[bze])
    S.dma("sp", w1, fw1[:, :], w=[bfw], key="fw1")
    S.dma("sp", w2, fw2[:, :], w=[bfw], key="fw2")
    S.dma("sp", w3, fw3[:, :], w=[bfw], key="fw3")
    S.dma("sp", fo, fout[:, :], w=[bfw], key="fo")
    S.dma("sp", fb[:, 0:4], fbs[:, :], w=[bfw], key="fbs")
    S.dma("sp", skc, skipc[:, :], w=[bsk], key="skc")
    S.dma("sp", dlc, deltac[:, :], w=[bsk], key="dlc")
    for l in range(3):
        kk.tt("dve", fb[:, 4 + l:5 + l], fb[:, l:l + 1], fb[:, 3:4], ALU.mult, r=[bfw], w=[bfw])
    kk.ts("dve", dlc, dlc, -1.0, None, ALU.mult, None, r=[bsk], w=[bsk])
    ps_f = [psbank(c, b) for b in range(4)]; bpsf = [Buf() for _ in range(4)]
    src, bsrc, K0 = ze, bze, 33
    layers = [(w1, hA, bhA), (w2, hB, bhB), (w3, hA, bhA)]
    for l, (wl, dst, bdst) in enumerate(layers):
        for blk in range(16):
            pf, bpf = ps_f[blk % 4], bpsf[blk % 4]
            kk.mm(pf[0:64, :], wl[0:K0, :], src[0:K0, blk * 512:(blk + 1) * 512], True, True, r=[bfw, bsrc], w=[bpf])
            kk.ts("dve", dst[:, blk * 512:(blk + 1) * 512], pf[0:64, :], fb[:, 3:4], fb[:, 4 + l:5 + l], ALU.mult, ALU.add,
                  r=[bpf, bfw], w=[bdst])
        for sb in range(4):
            xs_ = dst[:, sb * 2048:(sb + 1) * 2048]
            ms_ = mt[:, sb * 2048:(sb + 1) * 2048]
            for rep in range(1):
                kk.ts("dve", ms_, xs_, PI, -2 * PI, ALU.is_gt, ALU.mult, r=[bdst], w=[bmt])
                kk.tt("dve", xs_, xs_, ms_, ALU.add, r=[bdst, bmt], w=[bdst])
                kk.ts("dve", ms_, xs_, -PI, 2 * PI, ALU.is_lt, ALU.mult, r=[bdst], w=[bmt])
                kk.tt("dve", xs_, xs_, ms_, ALU.add, r=[bdst, bmt], w=[bdst])
            kk.act(xs_, xs_, AF.Sin, r=[bdst], w=[bdst])
        src, bsrc, K0 = dst, bdst, 64
    h3, bh3 = src, bsrc
    ps_o = [psbank(c, 4 + b) for b in range(4)]; bpso = [Buf() for _ in range(4)]
    oi = 0
    for blk in range(16):
        S.dma("sp", tv[blk % 2], tvals[0, blk * 512:(blk + 1) * 512].partition_broadcast(128), w=[btv[blk % 2]])
        for q in range(2):
            d_, bd_ = dec[q], bdec[q]
            kk.act(d_, tv[blk % 2], AF.Exp, r=[btv[blk % 2], bsk], w=[bd_], scale=dlc[:, q:q + 1])
            for kind in range(2):
                cc = kind * 2 + q
                po, bpo = ps_o[cc], bpso[cc]
                kk.mm(po, fo[0:64, cc * 128:(cc + 1) * 128], h3[0:64, blk * 512:(blk + 1) * 512], True, True, r=[bfw, bh3], w=[bpo])
                o3, bo3 = o32[kind], bo32[kind]
                kk.tt("dve", o3, po, d_, ALU.mult, r=[bpo, bd_], w=[bo3])
                if blk == 0:
                    if kind == 0:
                        kk.ts("dve", o3[:, 0:1], o3[:, 0:1], skc[:, q:q + 1], None, ALU.add, None, r=[bo3, bsk], w=[bo3])
                    else:
                        kk.memset("dve", o3[:, 0:1], 0.0, w=[bo3])
                o6, bo6 = o16[oi % 4], bo16[oi % 4]
                kk.cp("pool", o6, o3, r=[bo3], w=[bo6])
                dstd = hfscr if kind == 0 else hbscr
                S.dma("sp", dstd[q * 128:(q + 1) * 128, blk * 512:(blk + 1) * 512], o6, r=[bo6], key=f"hs{oi % 4}")
                if dbg:
                    dd = dbgo["d_hf"] if kind == 0 else dbgo["d_hb"]
                    S.dma("sp", dd[q * 128:(q + 1) * 128, blk * 512:(blk + 1) * 512], o6, r=[bo6], key=f"dbgh{oi % 4}")
                oi += 1
    S.barrier()
    if stop_after == "P2":
        S.emit(); c.st.close(); return nc
    A.reset(mP)

    G = A.alloc([128, 2, 128], BF16); bG = Buf()
    f1c = A.alloc([1024], BF16, parts=64); f1i = A.alloc([512], BF16); bf1 = Buf()
    gv = gmat.rearrange("p (j r b) -> p j r b", j=128, r=2)
    for q4 in range(4):
        S.dma("pool", G[:, q4 * 32:(q4 + 1) * 32, :, :], gv[:, q4 * 32:(q4 + 1) * 32, :, :], w=[bG], key=f"G{q4}")
    S.dma("pool", f1c, f1cat[:, :], w=[bf1], key="f1c")
    S.dma("pool", f1i, f1inv[:, :], w=[bf1], key="f1i")
    xg = A.alloc([CG, 128], BF16, parts=64); bxg = Buf()
    hfg = A.alloc([CG, 128], BF16, parts=64); bhfg = Buf()
    hbg = A.alloc([CG, 128], BF16, parts=64); bhbg = Buf()
    A1f = A.alloc([128, CG, 2], BF16); A2f = A.alloc([128, CG, 2], BF16); bAf = Buf()
    A1b = A.alloc([128, CG, 2], BF16); A2b = A.alloc([128, CG, 2], BF16); bAb = Buf()
    Kf = A.alloc([128, CG * 2], F32); bKf = Buf()
    ysb = A2b.rearrange("p a b c -> p (a b c)")[0:64, :].bitcast(F32).rearrange("p (c n) -> p c n", c=CG); bysb = bAb
    tq = [A.alloc([256], F32) for _ in range(4)]; btq = [Buf() for _ in range(4)]
    Yt = A1b.rearrange("p a b c -> p (a b c)").rearrange("p (c r k) -> p c r k", c=CG, r=2)
    Ap1 = A2b
    ps_a = [psbank(c, b) for b in range(3)]; bpsa = [Buf() for _ in range(3)]
    ps_x = [psbank(c, 3 + b) for b in range(3)]; bpsx = [Buf() for _ in range(3)]
    ps_y = [psbank(c, 6 + b, parts=64) for b in range(2)]; bpsy = [Buf() for _ in range(2)]
    NB = 512 // (2 * CG)
    NBI = 512 // CG
    ai = 0
    xi_ = 0
    yi_ = 0

    def s1(xsrc, bxs, cl, rhs, A1, A2, bA):
        nonlocal ai
        pa, bpa = ps_a[ai % 3], bpsa[ai % 3]
        kk.mm(pa, xsrc[0:64, cl, :], rhs, True, True, r=[bxs, bf1], w=[bpa])
        e1, e2 = ("act", "dve") if ai % 2 == 0 else ("dve", "act")
        ai += 1
        kk.cp(e1, A1[:, :, cl, :], pa[:, 0:256].rearrange("p (r k) -> p k r", r=2), r=[bpa], w=[bA])
        kk.cp(e2, A2[:, :, cl, :], pa[:, 256:512].rearrange("p (r k) -> p k r", r=2), r=[bpa], w=[bA])

    for g in range(256 // CG):
        c0 = g * CG
        S.dma("sp", hfg, hfscr[c0:c0 + CG, :].rearrange("c (a b) -> a c b", b=128), w=[bhfg])
        S.dma("sp", hbg, hbscr[c0:c0 + CG, :].rearrange("c (a b) -> a c b", b=128), w=[bhbg])
        S.dma("sp", xg, pscr[c0:c0 + CG, :].rearrange("c (a b) -> a c b", b=128), w=[bxg])
        for cl in range(CG):
            s1(hfg, bhfg, cl, f1c[0:64, 0:512], A1f, A2f, bAf)
            s1(hbg, bhbg, cl, f1c[0:64, 512:1024], A1b, A2b, bAb)
        for k0 in range(0, 128, NB):
            px, bpx = ps_x[xi_ % 3], bpsx[xi_ % 3]; xi_ += 1
            pxv = px.rearrange("p (k n) -> p k n", k=NB)
            for kq in range(NB):
                k2 = k0 + kq
                kk.mm(pxv[:, kq, :], G[:, k2, 0, :], A1f[:, k2, :, :].rearrange("p c r -> p (c r)"), True, False, r=[bG, bAf], w=[bpx])
                kk.mm(pxv[:, kq, :], G[:, k2, 1, :], A2f[:, k2, :, :].rearrange("p c r -> p (c r)"), False, False, r=[bG, bAf], w=[bpx])
                kk.mm(pxv[:, kq, :], G[:, k2, 0, :], A1b[:, k2, :, :].rearrange("p c r -> p (c r)"), False, False, r=[bG, bAb], w=[bpx])
                kk.mm(pxv[:, kq, :], G[:, k2, 1, :], A2b[:, k2, :, :].rearrange("p c r -> p (c r)"), False, True, r=[bG, bAb], w=[bpx])
            kk.cp("act", Kf[:, k0:k0 + NB, :], pxv, r=[bpx], w=[bKf])
        for cl in range(CG):
            s1(xg, bxg, cl, f1c[0:64, 0:512], A1f, A2f, bAf)
        for k0 in range(0, 128, NB):
            px, bpx = ps_x[xi_ % 3], bpsx[xi_ % 3]; xi_ += 1
            pxv = px.rearrange("p (k n) -> p k n", k=NB)
            for kq in range(NB):
                k2 = k0 + kq
                kk.mm(pxv[:, kq, :], G[:, k2, 0, :], A1f[:, k2, :, :].rearrange("p c r -> p (c r)"), True, False, r=[bG, bAf], w=[bpx])
                kk.mm(pxv[:, kq, :], G[:, k2, 1, :], A2f[:, k2, :, :].rearrange("p c r -> p (c r)"), False, True, r=[bG, bAf], w=[bpx])
            Xr = px.rearrange("p (m r) -> p m r", r=2)[:, :, 0]
            Xi = px.rearrange("p (m r) -> p m r", r=2)[:, :, 1]
            Kv = Kf[:, k0:k0 + NB, :].rearrange("p k n -> p (k n)").rearrange("p (m r) -> p m r", r=2)
            kk.tt("dve", tq[0], Xr, Kv[:, :, 0], ALU.mult, r=[bpx, bKf], w=[btq[0]])
            kk.tt("dve", tq[1], Xi, Kv[:, :, 1], ALU.mult, r=[bpx, bKf], w=[btq[1]])
            kk.tt("dve", tq[2], Xr, Kv[:, :, 1], ALU.mult, r=[bpx, bKf], w=[btq[2]])
            kk.tt("dve", tq[3], Xi, Kv[:, :, 0], ALU.mult, r=[bpx, bKf], w=[btq[3]])
            t3 = [t.rearrange("p (k c) -> p k c", k=NB) for t in tq]
            kk.tt("pool", Yt[:, :, 0, k0:k0 + NB].rearrange("p c k -> p k c"), t3[0], t3[1], ALU.subtract, r=[btq[0], btq[1]], w=[bAb])
            kk.tt("pool", Yt[:, :, 1, k0:k0 + NB].rearrange("p c k -> p k c"), t3[2], t3[3], ALU.add, r=[btq[2], btq[3]], w=[bAb])
        for cl in range(CG):
            pa, bpa = ps_a[ai % 3], bpsa[ai % 3]
            kk.mm(pa[:, 0:256], Yt[:, cl, 0, :], f1i[:, 0:256], True, False, r=[bAb, bf1], w=[bpa])
            kk.mm(pa[:, 0:256], Yt[:, cl, 1, :], f1i[:, 256:512], False, True, r=[bAb, bf1], w=[bpa])
            kk.cp("act" if ai % 2 else "dve", A2f[:, :, cl, :], pa[:, 0:256].rearrange("p (r k) -> p k r", r=2), r=[bpa], w=[bAf])
            ai += 1
        for j0 in range(0, 128, NBI):
            py, bpy = ps_y[yi_ % 2], bpsy[yi_ % 2]; yi_ += 1
            pyv = py.rearrange("p (j c) -> p j c", j=NBI)
            for jq in range(NBI):
                j = j0 + jq
                kk.mm(pyv[:, jq, :], G[:, j, 0, 0:64], A2f[:, j, :, 0], True, False, r=[bG, bAf], w=[bpy])
                kk.mm(pyv[:, jq, :], G[:, j, 1, 0:64], A2f[:, j, :, 1], False, True, r=[bG, bAf], w=[bpy])
            kk.act(ysb[:, :, j0:j0 + NBI].rearrange("p c j -> p j c"), pyv, AF.Copy, r=[bpy], w=[bysb], scale=1.0 / 16384)
        S.dma("sp", yscr[c0:c0 + CG, :].rearrange("c (a b) -> a c b", b=128), ysb, r=[bysb], key="ysc")
        if dbg:
            S.dma("sp", dbgo["d_y"][c0:c0 + CG, :].rearrange("c (a b) -> a c b", b=128), ysb, r=[bysb], key="dbgy")
    S.barrier()
    if stop_after == "P3":
        S.emit(); c.st.close(); return nc
    A.reset(mP)

    yl = [A.alloc([2048], F32) for _ in range(2)]; byl = [Buf(), Buf()]
    xl = [A.alloc([2048], BF16) for _ in range(2)]; bxl = [Buf(), Buf()]
    ol = [A.alloc([2048], BF16) for _ in range(2)]; bol = [Buf(), Buf()]
    i = 0
    for q in range(2):
        for sb in range(4):
            p = i % 2; i += 1
            S.dma("sp", yl[p], yscr[q * 128:(q + 1) * 128, sb * 2048:(sb + 1) * 2048], w=[byl[p]])
            S.dma("sp", xl[p], x0scr[q * 128:(q + 1) * 128, sb * 2048:(sb + 1) * 2048], w=[bxl[p]])
            kk.tt("dve", ol[p], yl[p], xl[p], ALU.mult, r=[byl[p], bxl[p]], w=[bol[p]])
            if link is None:
                S.dma("sp", yout[q * 128:(q + 1) * 128, sb * 2048:(sb + 1) * 2048], ol[p], r=[bol[p]], key=f"yo{p}")
            else:
                for hh in range(2):
                    S.dma("sp", link["ysrc"][2 * q + hh][:, sb * 2048:(sb + 1) * 2048], ol[p][hh * 64:(hh + 1) * 64, :],
                          r=[bol[p]], w=[link["bysrc"][2 * q + hh]], key=f"yo{p}{hh}")
    S.barrier()
    if not own:
        return None
    S.emit()
    c.st.close()
    return nc


def build_l3(dbg=False, stop_after=None, c=None, pre="", link=None):
    own = c is None
    if own:
        c = new_prog()
    nc, S, A = c.nc, c.S, c.A
    A.reset(0)

    def D(name, shape, dt=F32, kind="ExternalInput"):
        return nc.dram_tensor(pre + name, shape, dt, kind=kind).ap()

    yT = D("yT", [128, 8 * 2048], BF16) if link is None else None
    hres = D("hres", [16, 128, 1024]) if link is None else link["h1res"]
    cs = D("cs", [128, 16])
    adaw = D("adaw", [1024, 6144]); adabcol = D("adabcol", [128, 32]); adabrow = D("adabrow", [1, 6144])
    lng = D("lng", [2, 1024]); lnb = D("lnb", [2, 1024])
    wout = D("wout", [1024, 1024]); rw = D("rw", [128, 64])
    mg = D("mg", [8, 1024, 3584]); mu = D("mu", [8, 1024, 3584]); md = D("md", [8, 3584, 1024])
    ident = D("ident", [128, 128])
    out = D("out", [16, 128, 1024], kind="ExternalOutput")
    hscr = D("hscr", [16, 128, 1024], kind="Internal")
    dbgo = {}
    if dbg:
        dbgo["d_h1a"] = D("d_h1a", [16, 128, 1024], kind="ExternalOutput")
        dbgo["d_comb"] = D("d_comb", [128, 128], kind="ExternalOutput")
        dbgo["d_acc"] = D("d_acc", [128, 16 * 1024], kind="ExternalOutput")

    identb = A.alloc([128], BF16)
    kk = K(S, identb)
    identf = A.alloc([128], F32)
    colmod = A.alloc([32, 2], F32)
    g1b = A.alloc([1024], F32); g2b = A.alloc([1024], F32)
    lngb = A.alloc([2, 1024], F32); lnbb = A.alloc([2, 1024], F32)
    epsc = A.alloc([4], F32)
    comb = A.alloc([16, 8], F32); bcomb = Buf()
    c.eps_ln = epsc[:, 0:1]
    bconst = Buf("const")
    S.dma("pool", identb, ident[:, :], w=[kk.bident], key="idb")
    S.dma("sp", identf, ident[:, :], w=[kk.bident], key="idf")
    kk.memset("dve", epsc[:, 0:1], LN_EPS, w=[bconst])
    for i in range(2):
        S.dma("sp", lngb[:, i, :], lng[i, :].partition_broadcast(128), w=[bconst], key=f"c{i}a")
        S.dma("sp", lnbb[:, i, :], lnb[i, :].partition_broadcast(128), w=[bconst], key=f"c{i}b")
    mP = A.mark()
    mod_phase(c, kk, cs, adaw, adabcol, adabrow, colmod, g1b, g2b, bconst, need=(2, 3, 4, 5))

    tT = A.alloc([8, 2048], BF16); btT = Buf()
    mP2 = A.mark()
    yTb = A.alloc([8, 2048], BF16); byT = Buf()
    woutb = A.alloc([8, 1024], BF16); bwo = Buf()
    rwf = A.alloc([8, 8], F32); brw = Buf()
    xres = [A.alloc([1024], F32) for _ in range(2)]; bxres = [Buf(), Buf()]
    tmpE = [A.alloc([1024], F32) for _ in range(2)]; btmpE = [Buf(), Buf()]
    outE = [A.alloc([1024], F32) for _ in range(2)]; boutE = [Buf(), Buf()]
    smE = [A.alloc([16], F32) for _ in range(2)]; bsmE = [Buf(), Buf()]
    tmpT = [A.alloc([8, 128], F32) for _ in range(2)]; btmpT = [Buf(), Buf()]
    t32 = [A.alloc([8, 128], F32) for _ in range(2)]; bt32 = [Buf(), Buf()]
    rs = [A.alloc([48], F32) for _ in range(2)]; brs = [Buf(), Buf()]
    if link is None:
        S.dma("sp", yTb.rearrange("p a b -> p (a b)"), yT[:, :], w=[byT])
    else:
        for r_ in range(4):
            for k_ in range(4):
                src = link["ydst"][k_]

                def lazy(eng, src=src, r_=r_):
                    if "off" not in c.dyn:
                        c.dyn["off"] = (eng.partition_id() % 4) * 1024
                    return src[r_ * 64:(r_ + 1) * 64, bass.ds(c.dyn["off"], 1024)]
                S.dma("sp", yTb[(k_ % 2) * 64:(k_ % 2) * 64 + 64, 2 * r_ + k_ // 2, :].bitcast(F32), lazy,
                      r=[link["bydst"][k_]], w=[byT], key=f"yl{(r_ * 4 + k_) % 4}")
    S.dma("pool", woutb, wout.rearrange("(kc p) n -> p kc n", p=128), w=[bwo])
    S.dma("sp", rwf.rearrange("p a b -> p (a b)"), rw[:, :], w=[brw])
    ps_out = psbank(c, 0, 1024, nb=2); bpsout = Buf()
    psTf = [psbank(c, 2, 1024, nb=2).rearrange("p (a b) -> p a b", a=8),
            psbank(c, 4, 1024, nb=2).rearrange("p (a b) -> p a b", a=8)]
    bpsTf = [Buf(), Buf()]
    ps_l = psbank(c, 6, 8); bpsl = Buf()
    def ph2_a(il):
        p = il % 2
        S.dma("sp", xres[p], hres[il], r=([] if link is None else [link["bh1res"][il]]), w=[bxres[p]])
        for half in range(2):
            n0 = half * 512
            for kc in range(8):
                kk.mm(ps_out[:, n0:n0 + 512], yTb[:, kc, il * 128:(il + 1) * 128], woutb[:, kc, n0:n0 + 512], kc == 0, kc == 7,
                      r=[byT, bwo], w=[bpsout])
        ln_tile(c, kk, ps_out, bpsout, xres[p], bxres[p], g1b, lngb[:, 0, :], lnbb[:, 0, :], bconst,
                tmpE[p], btmpE[p], outE[p], boutE[p], smE[p], bsmE[p])
        S.dma("sp", hscr[il], outE[p], r=[boutE[p]], key=f"hs{p}")
        if dbg:
            S.dma("sp", dbgo["d_h1a"][il], outE[p], r=[boutE[p]], key=f"dbgh{p}")

    def ph2_b(il):
        p = il % 2
        for kc in range(8):
            S.op("pe", (lambda o, i_: (lambda e: e.transpose(out=o, in_=i_, identity=identf)))(psTf[p][:, kc, :], outE[p][:, kc * 128:(kc + 1) * 128]),
                 r=[boutE[p], kk.bident], w=[bpsTf[p]])
        for hb_ in range(2):
            kk.tt("dve", tmpT[p][:, hb_ * 4:(hb_ + 1) * 4, :], psTf[p][:, hb_ * 4:(hb_ + 1) * 4, :],
                  bcast_mid(colmod[:, 24 + hb_ * 4:28 + hb_ * 4, 0], 128), ALU.mult, r=[bpsTf[p], bconst], w=[btmpT[p]])
        kk.tt("dve", t32[p], tmpT[p], bcast_mid(colmod[:, 16:24, 0], 128), ALU.add, r=[btmpT[p], bconst], w=[bt32[p]])
        kk.cp("act", tT[:, :, il * 128:(il + 1) * 128], t32[p], r=[bt32[p]], w=[btT])
        for kc in range(8):
            kk.mm(ps_l, t32[p][:, kc, :], rwf[:, kc, :], kc == 0, kc == 7, r=[bt32[p], brw], w=[bpsl])
        r_ = rs[p]; br_ = brs[p]
        lg = r_[:, 0:8]; eq1 = r_[:, 8:16]; l2 = r_[:, 16:24]; eq2 = r_[:, 24:32]
        m1 = r_[:, 32:33]; m2 = r_[:, 33:34]; dg = r_[:, 34:35]; ga = r_[:, 35:36]; gb = r_[:, 36:37]
        kk.cp("dve", lg, ps_l, r=[bpsl], w=[br_])
        S.op("dve", (lambda o, i_: (lambda e: e.tensor_reduce(out=o, in_=i_, axis=mybir.AxisListType.X, op=ALU.max)))(m1, lg), r=[br_], w=[br_])
        kk.ts("dve", eq1, lg, m1, None, ALU.is_equal, None, r=[br_], w=[br_])
        kk.stt("dve", l2, eq1, -1e30, lg, ALU.mult, ALU.add, r=[br_], w=[br_])
        S.op("dve", (lambda o, i_: (lambda e: e.tensor_reduce(out=o, in_=i_, axis=mybir.AxisListType.X, op=ALU.max)))(m2, l2), r=[br_], w=[br_])
        kk.ts("dve", eq2, l2, m2, None, ALU.is_equal, None, r=[br_], w=[br_])
        kk.tt("dve", dg, m1, m2, ALU.subtract, r=[br_], w=[br_])
        kk.act(ga, dg, AF.Sigmoid, r=[br_], w=[br_])
        kk.ts("dve", gb, ga, -1.0, 1.0, ALU.mult, ALU.add, r=[br_], w=[br_])
        kk.ts("dve", comb[:, il, :], eq1, ga, None, ALU.mult, None, r=[br_], w=[bcomb])
        kk.stt("dve", comb[:, il, :], eq2, gb, comb[:, il, :], ALU.mult, ALU.add, r=[br_, bcomb], w=[bcomb])

    ph2_a(0)
    for il in range(16):
        if il + 1 < 16:
            ph2_a(il + 1)
        ph2_b(il)
    if dbg:
        S.dma("sp", dbgo["d_comb"][:, :], comb.rearrange("p a b -> p (a b)"), r=[bcomb], key="dbgc")
    S.barrier()
    if stop_after == "P2":
        S.emit(); c.st.close(); return nc
    A.reset(mP2)

    acc = A.alloc([16, 1024], F32); bacc = [Buf() for _ in range(16)]
    mAcc = A.mark()
    hidT = A.alloc([4, 2048], BF16); bhid = Buf()
    wgq = [A.alloc([8, 512], BF16) for _ in range(2)]; wuq = [A.alloc([8, 512], BF16) for _ in range(2)]
    wdq = [A.alloc([4, 1024], BF16) for _ in range(2)]; bwq = [Buf(), Buf()]; bwdq = [Buf(), Buf()]
    sg = [A.alloc([512], BF16) for _ in range(2)]; bsg = [Buf(), Buf()]
    ps_g = [psbank(c, 0), psbank(c, 1)]; bpsg = [Buf(), Buf()]
    ps_u = [psbank(c, 2), psbank(c, 3)]; bpsu = [Buf(), Buf()]
    ps_d = [psbank(c, 4, 1024, nb=2), psbank(c, 6, 1024, nb=2)]; bpsd = [Buf(), Buf()]
    gi = 0
    ci = 0
    di = 0
    for e in range(8):
        mg_v = mg[e].rearrange("(kc p) n -> p kc n", p=128)
        mu_v = mu[e].rearrange("(kc p) n -> p kc n", p=128)
        md_v = md[e].rearrange("(c p) n -> p c n", p=128)
        for fg in range(7):
            p = gi % 2
            first = (gi == 0)
            gi += 1
            S.dma("pool", wgq[p], mg_v[:, :, fg * 512:(fg + 1) * 512], w=[bwq[p]], key=f"wg{p}")
            S.dma("pool", wuq[p], mu_v[:, :, fg * 512:(fg + 1) * 512], w=[bwq[p]], key=f"wu{p}")
            S.dma("pool", wdq[p], md_v[:, fg * 4:(fg + 1) * 4, :], w=[bwdq[p]], key=f"wd{p}")
            for fc in range(4):
                for ts_ in range(4):
                    q = ci % 2
                    ci += 1
                    t0 = ts_ * 512
                    for kc in range(8):
                        kk.mm(ps_g[q], wgq[p][:, kc, fc * 128:(fc + 1) * 128], tT[:, kc, t0:t0 + 512], kc == 0, kc == 7,
                              r=[bwq[p], btT], w=[bpsg[q]])
                    for kc in range(8):
                        kk.mm(ps_u[q], wuq[p][:, kc, fc * 128:(fc + 1) * 128], tT[:, kc, t0:t0 + 512], kc == 0, kc == 7,
                              r=[bwq[p], btT], w=[bpsu[q]])
                    kk.act(sg[q], ps_g[q], AF.Silu, r=[bpsg[q]], w=[bsg[q]])
                    kk.tt("dve", hidT[:, fc, t0:t0 + 512], ps_u[q], sg[q], ALU.mult, r=[bpsu[q], bsg[q]], w=[bhid])
            for il in range(16):
                d = di % 2
                di += 1
                for half in range(2):
                    n0 = half * 512
                    for fc in range(4):
                        kk.mm(ps_d[d][:, n0:n0 + 512], hidT[:, fc, il * 128:(il + 1) * 128], wdq[p][:, fc, n0:n0 + 512], fc == 0, fc == 3,
                              r=[bhid, bwdq[p]], w=[bpsd[d]])
                for half in range(2):
                    n0 = half * 512
                    if first:
                        kk.ts("dve", acc[:, il, n0:n0 + 512], ps_d[d][:, n0:n0 + 512], comb[:, il, e:e + 1], None, ALU.mult, None,
                              r=[bpsd[d], bcomb], w=[bacc[il]])
                    else:
                        kk.stt("dve", acc[:, il, n0:n0 + 512], ps_d[d][:, n0:n0 + 512], comb[:, il, e:e + 1], acc[:, il, n0:n0 + 512],
                               ALU.mult, ALU.add, r=[bpsd[d], bcomb, bacc[il]], w=[bacc[il]])
    if dbg:
        S.dma("sp", dbgo["d_acc"][:, :], acc.rearrange("p a b -> p (a b)"), r=bacc, key="dbga")
    S.barrier()
    A.reset(mAcc)
    xr2 = [A.alloc([1024], F32) for _ in range(2)]; bxr2 = [Buf(), Buf()]
    tmpF = [A.alloc([1024], F32) for _ in range(2)]; btmpF = [Buf(), Buf()]
    outF = [A.alloc([1024], F32) for _ in range(2)]; boutF = [Buf(), Buf()]
    smF = [A.alloc([16], F32) for _ in range(2)]; bsmF = [Buf(), Buf()]
    for il in range(16):
        p = il % 2
        S.dma("sp", xr2[p], hscr[il], w=[bxr2[p]])
        ln_tile(c, kk, acc[:, il, :], bacc[il], xr2[p], bxr2[p], g2b, lngb[:, 1, :], lnbb[:, 1, :], bconst,
                tmpF[p], btmpF[p], outF[p], boutF[p], smF[p], bsmF[p])
        S.dma("sp", out[il], outF[p], r=[boutF[p]], key=f"ho{p}")
    S.barrier()
    if not own:
        return None
    S.emit()
    c.st.close()
    return nc


def rope_table():
    n = 8192
    rows = n // 64
    r = np.repeat(np.arange(rows, dtype=np.float32), 64)
    col = np.tile(np.arange(64, dtype=np.float32), rows)
    inv = (np.float32(10000.0) ** (-np.arange(0, 16, 2, dtype=np.float32) / np.float32(16))).astype(np.float32)
    ar = r[:, None] * inv
    ac = col[:, None] * inv
    return np.concatenate([np.cos(ar), np.cos(ac), np.sin(ar), np.sin(ac)], axis=1).astype(np.float32)


def band_mats(j):
    n = 8192

    def A(G, o, g):
        m = np.zeros((128, 128), np.float32)
        Gs = G + o
        if Gs < 0 or Gs >= 64:
            return m
        half = POOL_WINDOWS[g] // 2
        for t in range(128):
            T = G * 128 + t
            lo = max(T - half, 0)
            hi = min(T + half, n)
            for Sx in range(lo, hi):
                s = Sx - Gs * 128
                if 0 <= s < 128:
                    m[s, t] += 1.0 / (hi - lo)
            s = T - Gs * 128
            if 0 <= s < 128:
                m[s, t] -= 1.0
        return m
    G0 = 16 * j
    Gm = 1
    out = np.zeros((7, 4, 128, 128), np.float32)
    for g in range(4):
        out[0, g] = A(Gm, -1, g); out[1, g] = A(Gm, 0, g); out[2, g] = A(Gm, 1, g)
        out[3, g] = A(G0, -1, g); out[4, g] = A(G0, 0, g)
        out[5, g] = A(G0 + 15, 0, g); out[6, g] = A(G0 + 15, 1, g)
    return out


def col_form(v, nchunk):
    return np.ascontiguousarray(v.reshape(nchunk, 128).T)


def l1_inputs(inp):
    x = inp["x"]; ctx = inp["ctx"]
    rt = rope_table()
    ident = np.eye(128, dtype=np.float32)
    ada_b0 = inp["ada_b"][0]
    adabcol = np.concatenate([col_form(ada_b0[v * 1024:(v + 1) * 1024], 8) for v in (0, 1, 3, 4)], axis=1)
    kv_up = inp["kv_up"][0].reshape(256, 8, 128)
    kvupk = np.ascontiguousarray(kv_up[:, :, :64].reshape(256, 512))
    kvupv = np.ascontiguousarray(kv_up[:, :, 64:].reshape(256, 512))
    maps = []
    for core in range(NCORES):
        b, j = core // 4, core % 4
        xt = x[b].reshape(64, 128, 1024)
        ct = ctx[b].reshape(2, 128, 1024)
        own = list(range(16 * j, 16 * j + 16))
        others = [t for t in range(64) if t not in own]
        order = own + others
        xkv = np.concatenate([ct, xt[order]], axis=0)
        rtt = rt.reshape(64, 128, 32)[order]
        r0 = np.zeros((2, 128, 32), np.float32); r0[:, :, 0:16] = 1.0
        ropet = np.concatenate([r0, rtt], axis=0)
        ro = rt.reshape(64, 128, 32)[own]
        cosq = np.broadcast_to(ro[:, :, None, 0:16].reshape(16, 128, 1, 2, 1, 8), (16, 128, 8, 2, 2, 8)).reshape(16, 128, 256)
        sinq = np.broadcast_to(ro[:, :, None, 16:32].reshape(16, 128, 1, 2, 1, 8), (16, 128, 8, 2, 2, 8)).reshape(16, 128, 256)
        ropeq = np.ascontiguousarray(np.concatenate([cosq, sinq], axis=2))
        xhalo = np.zeros((2, 128, 1024), np.float32)
        if 16 * j - 1 >= 0:
            xhalo[0] = xt[16 * j - 1]
        if 16 * j + 16 < 64:
            xhalo[1] = xt[16 * j + 16]
        cs = np.stack([col_form(inp["c"][b], 8), col_form(inp["c_ctx"], 8)], axis=2).reshape(128, 16)
        maps.append({
            "xkv": np.ascontiguousarray(xkv), "xhalo": xhalo, "ropeq": ropeq, "ropet": np.ascontiguousarray(ropet),
            "cs": np.ascontiguousarray(cs), "adaw": inp["ada_w"][0], "adabcol": np.ascontiguousarray(adabcol),
            "adabrow": ada_b0.reshape(1, 6144), "lng": inp["ln_g"][0], "lnb": inp["ln_b"][0],
            "win": inp["mix_in_w"][0], "poolw": inp["pool_w"][0], "pscale": col_form(inp["pool_scale"][0], 4),
            "qnorm": col_form(inp["q_norm"][0], 2), "kvnorm": col_form(inp["kv_norm"][0], 2),
            "qup": inp["q_up"][0], "kvupk": kvupk, "kvupv": kvupv, "wout": inp["mix_out_w"][0],
            "wg": inp["ffn_gate"][0], "wu": inp["ffn_up"][0], "wd": inp["ffn_down"][0],
            "bands": band_mats(j), "ident": ident,
        })
    return maps


HY_MIN_DECAY = math.log(1e-2) / 1.5
HY_MAX_DECAY = math.log(1e-2) / 0.3


def fft_consts():
    n1 = np.arange(64)[:, None]; k2 = np.arange(128)[None, :]
    th = 2 * np.pi * n1 * k2 / 128
    fr, fi = np.cos(th), -np.sin(th)
    f1cat = np.concatenate([fr, fi, -fi, fr, fr, -fi, -fi, -fr], axis=1).astype(np.float32)
    k1 = np.arange(128)[:, None]; n2 = np.arange(128)[None, :]
    th = 2 * np.pi * k1 * n2 / 128
    pr, pi_ = np.cos(th), np.sin(th)
    f1inv = np.concatenate([pr, pi_, -pi_, pr], axis=1).astype(np.float32)
    a = np.arange(128)[:, None, None]; j = np.arange(128)[None, :, None]; b = np.arange(128)[None, None, :]
    th = 2 * np.pi * ((a * (j + 128 * b)) % 16384) / 16384
    g = np.stack([np.cos(th), -np.sin(th)], axis=2).astype(np.float32)
    return f1cat, f1inv, np.ascontiguousarray(g.reshape(128, 128 * 2 * 128))


def filt_consts():
    n = 8192
    t = np.linspace(0.0, 1.0, n, dtype=np.float32)
    w_ang = (np.float32(2.0 * math.pi / n) * np.arange(n, dtype=np.float32))[:, None]
    bands = np.linspace(1e-4, 15, 16, dtype=np.float32)[None, :]
    z = np.concatenate([t[:, None], np.cos(bands * w_ang), -np.sin(bands * w_ang)], axis=-1).astype(np.float32)
    deltas = np.abs(np.linspace(HY_MIN_DECAY, HY_MAX_DECAY, 1024, dtype=np.float32))
    return np.ascontiguousarray(z.T), t.reshape(1, n).copy(), deltas


def mod_inputs(inp, layer, b):
    ada_b = inp["ada_b"][layer]
    adabcol = np.concatenate([col_form(ada_b[v * 1024:(v + 1) * 1024], 8) for v in (0, 1, 3, 4)], axis=1)
    cs = np.stack([col_form(inp["c"][b], 8), col_form(inp["c_ctx"], 8)], axis=2).reshape(128, 16)
    return {"cs": np.ascontiguousarray(cs), "adaw": inp["ada_w"][layer], "adabcol": np.ascontiguousarray(adabcol),
            "adabrow": ada_b.reshape(1, 6144)}


def l2_inputs(inp, h1):
    f1cat, f1inv, gmat = fft_consts()
    zembT, tvals, deltas = filt_consts()
    ident = np.eye(128, dtype=np.float32)
    w_in = inp["hy_in_w"][0]; cw = inp["hy_conv_w"][0]; cb = inp["hy_conv_b"][0]
    fout = inp["hy_fout"][0]
    fbs = np.stack([inp["hy_fb1"][0], inp["hy_fb2"][0], inp["hy_fb3"][0], inp["hy_freq"][0]], axis=1)
    maps = []
    for core in range(NCORES):
        b, j = core // 4, core % 4
        cols = np.concatenate([part * 1024 + 256 * j + np.arange(256) for part in range(3)])
        convw = np.zeros((128, 18), np.float32); convb = np.zeros((128, 6), np.float32)
        for ch in range(6):
            cc = cols[ch * 128:(ch + 1) * 128]
            convw[:, ch * 3:(ch + 1) * 3] = cw[:, cc].T
            convb[:, ch] = cb[cc]
        fcols = np.concatenate([kind * 1024 + 256 * j + np.arange(256) for kind in range(2)])
        m = {"win": np.ascontiguousarray(w_in[:, cols]),
             "convw": convw, "convb": convb, "zemb": zembT, "fw1": inp["hy_fw1"][0], "fw2": inp["hy_fw2"][0],
             "fw3": inp["hy_fw3"][0], "fout": np.ascontiguousarray(fout[:, fcols]), "fbs": np.ascontiguousarray(fbs),
             "skipc": col_form(inp["hy_skip"][0][256 * j:256 * j + 256], 2), "deltac": col_form(deltas[256 * j:256 * j + 256], 2),
             "tvals": tvals, "f1cat": f1cat, "f1inv": f1inv, "gmat": gmat, "ident": ident}
        if h1 is not None:
            m["hin"] = np.ascontiguousarray(h1[b].reshape(64, 128, 1024))
        m.update(mod_inputs(inp, 1, b))
        maps.append(m)
    return maps


def l3_inputs(inp, h1, yfull):
    ident = np.eye(128, dtype=np.float32)
    rw = np.ascontiguousarray(inp["router_w"][0].reshape(8, 128, 8).transpose(1, 0, 2).reshape(128, 64))
    maps = []
    for core in range(NCORES):
        b, j = core // 4, core % 4
        m = {"lng": inp["ln_g"][1], "lnb": inp["ln_b"][1], "wout": inp["hy_out_w"][0], "rw": rw,
             "mg": inp["moe_gate"][0], "mu": inp["moe_up"][0], "md": inp["moe_down"][0], "ident": ident}
        if yfull is not None:
            yt = yfull[b][:, 2048 * j:2048 * (j + 1)].reshape(8, 128, 2048).transpose(1, 0, 2).reshape(128, 8 * 2048)
            m["yT"] = np.ascontiguousarray(yt)
            m["hres"] = np.ascontiguousarray(h1[b, 2048 * j:2048 * (j + 1)].reshape(16, 128, 1024))
        m.update(mod_inputs(inp, 1, b))
        maps.append(m)
    return maps


G4 = [[0, 1, 2, 3], [4, 5, 6, 7]]


def build_fused():
    c = new_prog()
    nc, S = c.nc, c.S
    h1res = nc.dram_tensor("x_h1res", [16, 128, 1024], F32).ap()
    bh1res = [Buf() for _ in range(16)]
    xs_t = [nc.dram_tensor(f"x_xs{k}", [2048, 128], F32) for k in range(4)]
    xd_t = [nc.dram_tensor(f"x_xd{k}", [8192, 128], F32) for k in range(4)]
    ys_t = [nc.dram_tensor(f"x_ys{k}", [2048, 128], F32) for k in range(4)]
    yd_t = [nc.dram_tensor(f"x_yd{k}", [8192, 128], F32) for k in range(4)]
    xsrc = [t.ap().bitcast(BF16).rearrange("(t a) b -> t (a b)", a=4) for t in xs_t]
    xdst = [t.ap().bitcast(BF16).rearrange("(t a) b -> t (a b)", a=4) for t in xd_t]
    ysrc = [t.ap().bitcast(BF16).rearrange("(c a) b -> c (a b)", a=32) for t in ys_t]
    ydst = [t.ap().rearrange("(c a) b -> c (a b)", a=32) for t in yd_t]
    bxsrc = [Buf() for _ in range(4)]; bxdst = [Buf() for _ in range(4)]
    bysrc = [Buf() for _ in range(4)]; bydst = [Buf() for _ in range(4)]
    build_l1(c=c, pre="a_", link={"h1res": h1res, "bh1res": bh1res, "xsrc": xsrc, "bxsrc": bxsrc})
    for k in range(4):
        S.coll("AllGather", G4, xs_t[k], xd_t[k], r=[bxsrc[k]], w=[bxdst[k]])
    build_l2(c=c, pre="b_", link={"xdst": xdst, "bxdst": bxdst, "ysrc": ysrc, "bysrc": bysrc})
    for k in range(4):
        S.coll("AllGather", G4, ys_t[k], yd_t[k], r=[bysrc[k]], w=[bydst[k]])
    build_l3(c=c, pre="c_", link={"h1res": h1res, "bh1res": bh1res, "ydst": ydst, "bydst": bydst})
    S.emit()
    c.st.close()
    return nc


def fused_inputs(inp):
    m1 = l1_inputs(inp)
    m2 = l2_inputs(inp, None)
    m3 = l3_inputs(inp, None, None)
    maps = []
    for core in range(NCORES):
        m = {}
        for pre, mm_ in (("a_", m1[core]), ("b_", m2[core]), ("c_", m3[core])):
            for k, v in mm_.items():
                m[pre + k] = v
        maps.append(m)
    return maps


_CACHE = {}


def kernel(**inputs):
    inp = {k: np.asarray(v) for k, v in inputs.items()}
    if "fused" not in _CACHE:
        _CACHE["fused"] = build_fused()
    res = run_bass_kernel_spmd(_CACHE["fused"], fused_inputs(inp), core_ids=list(range(NCORES)))
    out = np.stack([np.asarray(r["c_out"]).reshape(2048, 1024) for r in res.results]).reshape(2, 8192, 1024)
    return out.astype(np.float32)
```

```python
import math
import numpy as np
from contextlib import ExitStack
import concourse.bass as bass
import concourse.mybir as mybir
from concourse.bass_utils import run_bass_kernel_spmd

F32 = mybir.dt.float32
BF16 = mybir.dt.bfloat16
AF = mybir.ActivationFunctionType
ALU = mybir.AluOpType

SEM_CHUNK = 30000
NCORES = 8
ALPHA = 4.0 ** 0.25
LN_EPS = 1e-5
RMS_EPS = 1e-6
POOL_WINDOWS = (2, 4, 8, 16)


class Buf:
    __slots__ = ("name", "w", "r")

    def __init__(self, name=""):
        self.name = name
        self.w = None
        self.r = []


class Sched:
    ENG = ("pe", "act", "dve", "pool", "sp")

    def __init__(self, nc, stack):
        self.nc = nc
        self.stack = stack
        self.streams = {e: [] for e in self.ENG}
        self.seq = {e: 0 for e in self.ENG}
        self.esems = {e: [] for e in self.ENG}
        self.waited = {e: {} for e in self.ENG}
        self.dsem = {}
        self.free_d = []
        self.nsem = 0

    def _newsem(self, name):
        s = self.stack.enter_context(self.nc.semaphore(name))
        self.nsem += 1
        return s

    def _etoken(self, eng):
        n = self.seq[eng]
        ci = n // SEM_CHUNK
        while len(self.esems[eng]) <= ci:
            self.esems[eng].append(self._newsem(f"e_{eng}_{len(self.esems[eng])}"))
        self.seq[eng] = n + 1
        return (self.esems[eng][ci], n % SEM_CHUNK + 1, eng)

    def _need_wait(self, eng, tok):
        if tok is None:
            return False
        sem, val, src = tok
        if src == eng and eng == "pe":
            return False
        cur = self.waited[eng].get(id(sem), 0)
        if cur >= val:
            return False
        self.waited[eng][id(sem)] = val
        return True

    def _deps(self, eng, r, w):
        toks = []
        for b in r:
            if b.w is not None:
                toks.append(b.w)
        for b in w:
            if b.w is not None:
                toks.append(b.w)
            toks.extend(b.r)
        return [t for t in toks if self._need_wait(eng, t)]

    def _commit(self, tok, r, w):
        for b in r:
            b.r.append(tok)
            if len(b.r) > 96:
                b.r = b.r[-96:]
        for b in w:
            b.w = tok
            b.r = []

    def op(self, eng, fn, r=(), w=()):
        waits = self._deps(eng, r, w)
        tok = self._etoken(eng)
        self.streams[eng].append(("op", fn, waits, tok))
        self._commit(tok, r, w)
        return tok

    def dma(self, eng, out, in_, r=(), w=(), key=None):
        waits = self._deps(eng, r, w)
        kb = key if key is not None else (w[0] if len(w) else r[0])
        ent = self._dsem_for(kb)
        ent[1] += 16
        tok = (ent[0], ent[1], "dma")
        self.streams[eng].append(("dma", (out, in_), waits, tok))
        self._commit(tok, r, w)
        return tok

    def _dsem_for(self, kb):
        if kb not in self.dsem:
            if self.free_d:
                self.dsem[kb] = self.free_d.pop()
            else:
                self.dsem[kb] = [self._newsem(f"d{self.nsem}"), 0]
        return self.dsem[kb]

    def coll(self, kind, groups, src_t, dst_t, r=(), w=()):
        waits = self._deps("pool", r, w)
        ent = self._dsem_for(("cc", len(self.streams["pool"])))
        ent[1] += 1
        tok = (ent[0], ent[1], "dma")
        self.streams["pool"].append(("cc", (kind, groups, src_t, dst_t), waits, tok))
        self._commit(tok, r, w)
        return tok

    def wait(self, eng, tok):
        if self._need_wait(eng, tok):
            self.streams[eng].append(("wait", None, [tok], None))

    def barrier(self):
        toks = []
        for e in self.ENG:
            n = self.seq[e]
            if n > 0:
                ci = (n - 1) // SEM_CHUNK
                toks.append((self.esems[e][ci], (n - 1) % SEM_CHUNK + 1, e))
        for kb, ent in self.dsem.items():
            if ent[1] > 0:
                toks.append((ent[0], ent[1], "dma"))
        for e in self.ENG:
            for t in toks:
                if t[2] == e:
                    continue
                self.wait(e, t)
        for kb, ent in self.dsem.items():
            self.free_d.append(ent)
        self.dsem = {}

    def emit(self):
        nc = self.nc
        engmap = {"pe": "tensor", "act": "scalar", "dve": "vector", "pool": "gpsimd", "sp": "sync"}
        with nc.Block() as block:
            for e in self.ENG:
                stream = self.streams[e]

                def body(engine, stream=stream):
                    for kind, payload, waits, tok in stream:
                        for (sem, val, _src) in waits:
                            engine.wait_ge(sem, val)
                        if kind == "op":
                            payload(engine).then_inc(tok[0], 1)
                        elif kind == "dma":
                            out, in_ = payload
                            if callable(in_):
                                in_ = in_(engine)
                            engine.dma_start(out=out, in_=in_).then_inc(tok[0], 16)
                        elif kind == "cc":
                            ckind, groups, src_t, dst_t = payload
                            engine.collective_compute(ckind, ALU.bypass, replica_groups=groups, ins=[src_t.ap().opt()],
                                                      outs=[dst_t.ap().opt()]).then_inc(tok[0])
                getattr(block, engmap[e])(body)


class Arena:
    def __init__(self, t, lo, hi):
        self.t = t
        self.lo = lo
        self.hi = hi
        self.off = lo

    def alloc(self, free_shape, dt, parts=128):
        n = 1
        for s in free_shape:
            n *= s
        esz = 4 if dt == F32 else 2
        words = (n * esz + 3) // 4
        words = (words + 7) // 8 * 8
        assert self.off + words <= self.hi, f"arena overflow {self.off}+{words}>{self.hi}"
        ap = self.t[0:parts, self.off:self.off + words]
        self.off += words
        if dt != F32:
            ap = ap.bitcast(dt)
        ap = ap[:, 0:n]
        if len(free_shape) == 2:
            ap = ap.rearrange("p (a b) -> p a b", a=free_shape[0])
        elif len(free_shape) == 3:
            ap = ap.rearrange("p (a b c) -> p a b c", a=free_shape[0], b=free_shape[1])
        return ap

    def mark(self):
        return self.off

    def reset(self, m):
        self.off = m


class K:
    def __init__(self, S, identb):
        self.S = S
        self.identb = identb
        self.bident = Buf("ident")

    def mm(self, out, lhsT, rhs, start, stop, r, w):
        return self.S.op("pe", lambda e: e.matmul(out=out, lhsT=lhsT, rhs=rhs, start=start, stop=stop), r=r, w=w)

    def tr(self, out, in_, r, w):
        n = in_.shape[0]
        idn = self.identb[0:n, 0:n]
        return self.S.op("pe", lambda e: e.transpose(out=out, in_=in_, identity=idn), r=list(r) + [self.bident], w=w)

    def act(self, out, in_, func, r, w, bias=None, scale=None, accum=None):
        kw = {}
        if bias is not None:
            kw["bias"] = bias
        if scale is not None:
            kw["scale"] = scale
        if accum is not None:
            kw["accum_out"] = accum
        return self.S.op("act", lambda e: e.activation(out=out, in_=in_, func=func, **kw), r=r, w=w)

    def tt(self, eng, out, in0, in1, op, r, w):
        return self.S.op(eng, lambda e: e.tensor_tensor(out=out, in0=in0, in1=in1, op=op), r=r, w=w)

    def ts(self, eng, out, in0, s1, s2, op0, op1, r, w):
        if op1 is None:
            return self.S.op(eng, lambda e: e.tensor_scalar(out=out, in0=in0, scalar1=s1, scalar2=None, op0=op0), r=r, w=w)
        return self.S.op(eng, lambda e: e.tensor_scalar(out=out, in0=in0, scalar1=s1, scalar2=s2, op0=op0, op1=op1), r=r, w=w)

    def stt(self, eng, out, in0, scalar, in1, op0, op1, r, w):
        return self.S.op(eng, lambda e: e.scalar_tensor_tensor(out=out, in0=in0, scalar=scalar, in1=in1, op0=op0, op1=op1), r=r, w=w)

    def cp(self, eng, out, in_, r, w):
        if eng == "act":
            return self.S.op("act", lambda e: e.activation(out=out, in_=in_, func=AF.Copy), r=r, w=w)
        return self.S.op(eng, lambda e: e.tensor_copy(out=out, in_=in_), r=r, w=w)

    def memset(self, eng, out, val, w):
        return self.S.op(eng, lambda e: e.memset(out, val), r=(), w=w)

    def recip(self, out, in_, r, w):
        return self.S.op("dve", lambda e: e.reciprocal(out=out, in_=in_), r=r, w=w)

    def bn_stats(self, out, in_, r, w):
        return self.S.op("dve", lambda e: e.bn_stats(out=out, in_=in_), r=r, w=w)

    def bn_aggr(self, out, in_, r, w):
        return self.S.op("dve", lambda e: e.bn_aggr(out=out, in_=in_), r=r, w=w)


def bcast_mid(ap2, n):
    return ap2.unsqueeze(2).to_broadcast([ap2.shape[0], ap2.shape[1], n])


class Ctx:
    pass


def new_prog():
    nc = bass.Bass("TRN2", target_bir_lowering=False)
    st = ExitStack()
    S = Sched(nc, st)
    arena_t = st.enter_context(nc.sbuf_tensor("arena", [128, 52000], F32))
    psum_t = st.enter_context(nc.psum_tensor("psum", [128, 4096], F32))
    c = Ctx()
    c.nc, c.st, c.S, c.arena_t, c.psum_t = nc, st, S, arena_t, psum_t
    c.A = Arena(arena_t, 0, 52000)
    c.dyn = {}
    return c


def psbank(c, b, n=512, dt=F32, parts=128, nb=1):
    ap = c.psum_t[0:parts, b * 512:(b + nb) * 512]
    if dt != F32:
        ap = ap.bitcast(dt)
    return ap[:, 0:n]


def ln_tile(c, kk, ps_y, bps, res, bres, gate_b, lng_b, lnb_b, bconst, tmp, btmp, out, bout, small, bsmall):
    S = c.S
    for hh in range(2):
        kk.tt("dve", tmp[:, hh * 512:(hh + 1) * 512], ps_y[:, hh * 512:(hh + 1) * 512], gate_b[:, hh * 512:(hh + 1) * 512],
              ALU.mult, r=[bps, bconst], w=[btmp])
    kk.stt("dve", tmp, res, ALPHA, tmp, ALU.mult, ALU.add, r=[bres, btmp], w=[btmp])
    st6 = small[:, 0:12].rearrange("p (a b) -> p a b", a=2)
    for h in range(2):
        kk.bn_stats(st6[:, h, :], tmp[:, h * 512:(h + 1) * 512], r=[btmp], w=[bsmall])
    kk.bn_aggr(small[:, 12:14], st6, r=[bsmall], w=[bsmall])
    kk.act(small[:, 14:15], small[:, 13:14], AF.Sqrt, r=[bsmall], w=[bsmall], bias=c.eps_ln, scale=1.0)
    kk.recip(small[:, 15:16], small[:, 14:15], r=[bsmall], w=[bsmall])
    kk.ts("dve", small[:, 14:15], small[:, 12:13], small[:, 15:16], -1.0, ALU.mult, ALU.mult, r=[bsmall], w=[bsmall])
    kk.act(tmp, tmp, AF.Identity, r=[btmp, bsmall], w=[btmp], bias=small[:, 14:15], scale=small[:, 15:16])
    kk.tt("dve", tmp, tmp, lng_b, ALU.mult, r=[btmp, bconst], w=[btmp])
    kk.tt("pool", out, tmp, lnb_b, ALU.add, r=[btmp, bconst], w=[bout])


def build_l1(dbg=False, stop_after=None, c=None, pre="", link=None):
    own = c is None
    if own:
        c = new_prog()
    nc, S, A = c.nc, c.S, c.A
    A.reset(0)

    def D(name, shape, dt=F32, kind="ExternalInput"):
        return nc.dram_tensor(pre + name, shape, dt, kind=kind).ap()

    xkv = D("xkv", [66, 128, 1024]); xhalo = D("xhalo", [2, 128, 1024]); ropet = D("ropet", [66, 128, 32]); ropeq = D("ropeq", [16, 128, 512])
    cs = D("cs", [128, 16])
    adaw = D("adaw", [1024, 6144]); adabcol = D("adabcol", [128, 32]); adabrow = D("adabrow", [1, 6144])
    lng = D("lng", [2, 1024]); lnb = D("lnb", [2, 1024])
    win = D("win", [1024, 1056]); poolw = D("poolw", [4, 128, 128]); pscale = D("pscale", [128, 4])
    qnorm = D("qnorm", [128, 2]); kvnorm = D("kvnorm", [128, 2])
    qup = D("qup", [256, 768]); kvupk = D("kvupk", [256, 512]); kvupv = D("kvupv", [256, 512])
    wout = D("wout", [1024, 1024]); wg = D("wg", [1024, 2816]); wu = D("wu", [1024, 2816]); wd = D("wd", [2816, 1024])
    bands = D("bands", [7, 4, 128, 128]); ident = D("ident", [128, 128])
    h1 = D("h1", [16, 128, 1024], kind="ExternalOutput") if link is None else None
    hscr = D("hscr", [16, 128, 1024], kind="Internal")
    dbgo = {}
    if dbg:
        dbgo["d_colmod"] = D("d_colmod", [128, 64], kind="ExternalOutput")
        dbgo["d_g1"] = D("d_g1", [128, 1024], kind="ExternalOutput")
        dbgo["d_kvnT"] = D("d_kvnT", [128, 2 * 8448], BF16, kind="ExternalOutput")
        dbgo["d_KT"] = D("d_KT", [96, 8448], BF16, kind="ExternalOutput")
        dbgo["d_qT"] = D("d_qT", [96, 8 * 2048], BF16, kind="ExternalOutput")
        dbgo["d_OnT"] = D("d_OnT", [64, 8 * 2048], BF16, kind="ExternalOutput")
        dbgo["d_h0a"] = D("d_h0a", [16, 128, 1024], kind="ExternalOutput")

    identb = A.alloc([128], BF16)
    kk = K(S, identb)
    ones_f = A.alloc([128], F32)
    colmod = A.alloc([32, 2], F32)
    g1b = A.alloc([1024], F32); g2b = A.alloc([1024], F32)
    lngb = A.alloc([2, 1024], F32); lnbb = A.alloc([2, 1024], F32)
    epsc = A.alloc([4], F32)
    c.eps_ln = epsc[:, 0:1]
    eps_rms = epsc[:, 1:2]
    bconst = Buf("const")
    S.dma("pool", identb, ident[:, :], w=[kk.bident])
    kk.memset("dve", ones_f, 1.0, w=[bconst])
    kk.memset("dve", epsc[:, 0:1], LN_EPS, w=[bconst])
    kk.memset("dve", epsc[:, 1:2], RMS_EPS, w=[bconst])
    for i in range(2):
        S.dma("sp", lngb[:, i, :], lng[i, :].partition_broadcast(128), w=[bconst], key=f"c{i}a")
        S.dma("sp", lnbb[:, i, :], lnb[i, :].partition_broadcast(128), w=[bconst], key=f"c{i}b")
    mP = A.mark()

    s2 = A.alloc([8, 2], F32); bs2 = Buf()
    s_bc = A.alloc([8, 128], F32); bsbc = Buf()
    wblk = [A.alloc([8, 512], F32) for _ in range(2)]; bwblk = [Buf(), Buf()]
    brow = [A.alloc([1024], F32) for _ in range(2)]; bbrow = [Buf(), Buf()]
    abc = A.alloc([32], F32); babc = Buf()
    S.dma("sp", s2.rearrange("p a b -> p (a b)"), cs[:, :], w=[bs2])
    S.dma("sp", abc, adabcol[:, :], w=[babc])
    S.dma("sp", brow[0], adabrow[0, 2048:3072].partition_broadcast(128), w=[bbrow[0]])
    S.dma("sp", brow[1], adabrow[0, 5120:6144].partition_broadcast(128), w=[bbrow[1]])
    kk.act(s2, s2, AF.Silu, r=[bs2], w=[bs2])
    kk.cp("dve", s_bc, bcast_mid(s2[:, :, 0], 128), r=[bs2], w=[bsbc])
    ps_col = psbank(c, 0, 64).rearrange("p (a b) -> p a b", a=32); bpscol = Buf()
    ps_row = [psbank(c, 1), psbank(c, 2)]; bpsrow = [Buf(), Buf()]
    adaw_v = adaw.rearrange("(kc p) n -> p kc n", p=128)
    colidx = {0: 0, 1: 1, 3: 2, 4: 3}
    gdst = {2: (g1b, brow[0], bbrow[0]), 5: (g2b, brow[1], bbrow[1])}
    for bi in range(12):
        v, half = bi // 2, bi % 2
        wb, bwb = wblk[bi % 2], bwblk[bi % 2]
        S.dma("sp", wb, adaw_v[:, :, bi * 512:(bi + 1) * 512], w=[bwb])
        if v in colidx:
            for fcl in range(4):
                slot = colidx[v] * 8 + half * 4 + fcl
                for kc in range(8):
                    kk.mm(ps_col[:, slot, :], wb[:, kc, fcl * 128:(fcl + 1) * 128], s2[:, kc, :], kc == 0, kc == 7,
                          r=[bwb, bs2], w=[bpscol])
        else:
            pr, bpr = ps_row[half], bpsrow[half]
            for kc in range(8):
                kk.mm(pr, s_bc[:, kc, :], wb[:, kc, :], kc == 0, kc == 7, r=[bwb, bsbc], w=[bpr])
            gt, brt, bbrt = gdst[v]
            kk.tt("dve", gt[:, half * 512:(half + 1) * 512], pr, brt[:, half * 512:(half + 1) * 512], ALU.add,
                  r=[bpr, bbrt], w=[bconst])
    kk.tt("dve", colmod, ps_col, bcast_mid(abc, 2), ALU.add, r=[bpscol, babc], w=[bconst])
    for ci in (1, 3):
        kk.ts("dve", colmod[:, ci * 8:(ci + 1) * 8, :], colmod[:, ci * 8:(ci + 1) * 8, :], 1.0, None, ALU.add, None,
              r=[bconst], w=[bconst])
    if dbg:
        S.dma("sp", dbgo["d_colmod"][:, :], colmod.rearrange("p a b -> p (a b)"), r=[bconst], key="dbg")
        S.dma("sp", dbgo["d_g1"][:, :], g1b, r=[bconst], key="dbg")
    S.barrier()
    if stop_after == "A":
        S.emit(); c.st.close(); return nc
    A.reset(mP)

    upool = A.alloc([18, 512], BF16); bup = [Buf() for _ in range(18)]
    mM = A.mark()
    kvnT = A.alloc([2, 8448], BF16); bkvnT = Buf()
    KT = [A.alloc([8448], BF16, parts=96) for _ in range(2)]; bKT = [Buf(), Buf()]
    mX = A.mark()
    qT = A.alloc([8, 2048], BF16, parts=96); bqT = Buf()
    mD = A.mark()
    qnT = A.alloc([2, 2048], BF16); bqnT = Buf()
    mX1 = A.mark()
    winb = A.alloc([8, 1056], BF16); bwin = Buf()
    xin = [A.alloc([1024], F32) for _ in range(3)]; bxin = [Buf() for _ in range(3)]
    xbf = [A.alloc([1024], BF16) for _ in range(2)]; bxbf = [Buf(), Buf()]
    tmpT = [A.alloc([8, 128], F32) for _ in range(2)]; btmpT = [Buf(), Buf()]
    uT = [A.alloc([8, 128], BF16) for _ in range(2)]; buT = [Buf(), Buf()]
    rt = [A.alloc([32], F32) for _ in range(2)]; brt_ = [Buf(), Buf()]
    kvrow = [A.alloc([352], BF16) for _ in range(2)]; bkvrow = [Buf(), Buf()]
    ropA = [A.alloc([32], F32) for _ in range(2)]; ropB = [A.alloc([32], F32) for _ in range(2)]; brop = [Buf(), Buf()]
    junk = A.alloc([256], F32); bjunk = Buf()
    sm = [A.alloc([8], F32) for _ in range(2)]; bsm = [Buf(), Buf()]
    qn = [A.alloc([256], BF16) for _ in range(2)]; bqn = [Buf(), Buf()]
    S.dma("pool", winb, win.rearrange("(kc p) n -> p kc n", p=128), w=[bwin])
    for i in range(2):
        kk.memset("pool", kvrow[i], 0.0, w=[bkvrow[i]])
    psT = [psbank(c, 0, 1024, BF16).rearrange("p (a b) -> p a b", a=8),
           psbank(c, 1, 1024, BF16).rearrange("p (a b) -> p a b", a=8)]
    bpsT = [Buf(), Buf()]
    ps_kv = [psbank(c, 2, 288), psbank(c, 3, 288)]; bpskv = [Buf(), Buf()]
    ps_p = psbank(c, 4); bpsp = Buf()
    ps_q = psbank(c, 5, 256); bpsq = Buf()
    psT2 = psbank(c, 6, 640, BF16).rearrange("p (a b) -> p a b", a=5); bpsT2 = Buf()
    psT3 = psbank(c, 7, 256, BF16).rearrange("p (a b) -> p a b", a=2); bpsT3 = Buf()

    tiles = [("halo", 0), ("halo", 1)] + [("kv", i) for i in range(66)]
    NT = len(tiles)

    def stageA(n):
        kind, i = tiles[n]
        src_ap = xhalo[i] if kind == "halo" else xkv[i]
        nmod = 1 if (kind == "kv" and i < 2) else 0
        xi, bxi = xin[n % 3], bxin[n % 3]
        S.dma("sp", xi, src_ap, w=[bxi])
        xb_, bxb_ = xbf[n % 2], bxbf[n % 2]
        kk.cp("act", xb_, xi, r=[bxi], w=[bxb_])
        pT, bpT = psT[n % 2], bpsT[n % 2]
        for kc in range(8):
            kk.tr(pT[:, kc, :], xb_[:, kc * 128:(kc + 1) * 128], r=[bxb_], w=[bpT])
        tT, btT = tmpT[n % 2], btmpT[n % 2]
        u, bu = uT[n % 2], buT[n % 2]
        kk.tt("dve", tT, pT, bcast_mid(colmod[:, 8:16, nmod], 128), ALU.mult, r=[bpT, bconst], w=[btT])
        kk.tt("dve", u, tT, bcast_mid(colmod[:, 0:8, nmod], 128), ALU.add, r=[btT, bconst], w=[bu])

    def stageB(n):
        kind, i = tiles[n]
        u, bu = uT[n % 2], buT[n % 2]
        if kind == "halo":
            for kc in range(8):
                kk.mm(ps_p, u[:, kc, :], winb[:, kc, 0:512], kc == 0, kc == 7, r=[bu, bwin], w=[bpsp])
            ui = 0 if i == 0 else 17
            kk.cp("act", upool[:, ui, :], ps_p, r=[bpsp], w=[bup[ui]])
            return
        is_own = 2 <= i < 18
        il = i - 2
        r_, br_ = rt[n % 2], brt_[n % 2]
        S.dma("sp", r_, ropet[i], w=[br_])
        pkv, bpkv = ps_kv[n % 2], bpskv[n % 2]
        for kc in range(8):
            kk.mm(pkv, u[:, kc, :], winb[:, kc, 768:1056], kc == 0, kc == 7, r=[bu, bwin], w=[bpkv])
        if is_own:
            for kc in range(8):
                kk.mm(ps_p, u[:, kc, :], winb[:, kc, 0:512], kc == 0, kc == 7, r=[bu, bwin], w=[bpsp])
            for kc in range(8):
                kk.mm(ps_q, u[:, kc, :], winb[:, kc, 512:768], kc == 0, kc == 7, r=[bu, bwin], w=[bpsq])
        s_, bs_ = sm[n % 2], bsm[n % 2]
        row, brow_ = kvrow[n % 2], bkvrow[n % 2]
        kk.act(junk, pkv[:, 0:256], AF.Square, r=[bpkv], w=[bjunk, bs_], accum=s_[:, 0:1])
        kk.act(s_[:, 1:2], s_[:, 0:1], AF.Sqrt, r=[bs_], w=[bs_], bias=eps_rms, scale=1.0 / 256)
        kk.recip(s_[:, 2:3], s_[:, 1:2], r=[bs_], w=[bs_])
        kk.ts("dve", row[:, 0:256], pkv[:, 0:256], s_[:, 2:3], None, ALU.mult, None, r=[bpkv, bs_], w=[brow_])
        xr = pkv[:, 256:288].rearrange("p (a h e) -> p a h e", a=2, h=2)
        cosb = r_[:, 0:16].rearrange("p (a e) -> p a e", a=2).unsqueeze(2).to_broadcast([128, 2, 2, 8])
        sinb = r_[:, 16:32].rearrange("p (a e) -> p a e", a=2).unsqueeze(2).to_broadcast([128, 2, 2, 8])
        rA = ropA[n % 2].rearrange("p (a h e) -> p a h e", a=2, h=2)
        rB = ropB[n % 2].rearrange("p (a h e) -> p a h e", a=2, h=2)
        brp = brop[n % 2]
        kk.tt("dve", rA, xr, cosb, ALU.mult, r=[bpkv, br_], w=[brp])
        kk.tt("dve", rB, xr, sinb, ALU.mult, r=[bpkv, br_], w=[brp])
        ro = row[:, 320:352].rearrange("p (a h e) -> p a h e", a=2, h=2)
        kk.tt("pool", ro[:, :, 0, :], rA[:, :, 0, :], rB[:, :, 1, :], ALU.subtract, r=[brp], w=[brow_])
        kk.tt("pool", ro[:, :, 1, :], rA[:, :, 1, :], rB[:, :, 0, :], ALU.add, r=[brp], w=[brow_])
        if is_own:
            kk.cp("act", upool[:, il + 1, :], ps_p, r=[bpsp], w=[bup[il + 1]])
            qn_, bqn_ = qn[n % 2], bqn[n % 2]
            kk.act(junk, ps_q, AF.Square, r=[bpsq], w=[bjunk, bs_], accum=s_[:, 4:5])
            kk.act(s_[:, 5:6], s_[:, 4:5], AF.Sqrt, r=[bs_], w=[bs_], bias=eps_rms, scale=1.0 / 256)
            kk.recip(s_[:, 6:7], s_[:, 5:6], r=[bs_], w=[bs_])
            kk.ts("dve", qn_, ps_q, s_[:, 6:7], 96.0 ** -0.5, ALU.mult, ALU.mult, r=[bpsq, bs_], w=[bqn_])

    def stageC(n):
        kind, i = tiles[n]
        if kind == "halo":
            return
        is_own = 2 <= i < 18
        il = i - 2
        row, brow_ = kvrow[n % 2], bkvrow[n % 2]
        kk.tr(psT2[:, 0, :], row[:, 0:128], r=[brow_], w=[bpsT2])
        kk.tr(psT2[:, 1, :], row[:, 128:256], r=[brow_], w=[bpsT2])
        kk.tr(psT2[0:96, 2, :], row[:, 256:352], r=[brow_], w=[bpsT2])
        kk.cp("act", kvnT[:, :, i * 128:(i + 1) * 128], psT2[:, 0:2, :], r=[bpsT2], w=[bkvnT])
        kk.cp("act", KT[0][64:96, i * 128:(i + 1) * 128], psT2[64:96, 2, :], r=[bpsT2], w=[bKT[0]])
        kk.cp("act", KT[1][64:96, i * 128:(i + 1) * 128], psT2[64:96, 2, :], r=[bpsT2], w=[bKT[1]])
        if is_own:
            qn_, bqn_ = qn[n % 2], bqn[n % 2]
            kk.tr(psT3[:, 0, :], qn_[:, 0:128], r=[bqn_], w=[bpsT3])
            kk.tr(psT3[:, 1, :], qn_[:, 128:256], r=[bqn_], w=[bpsT3])
            kk.cp("dve", qnT[:, :, il * 128:(il + 1) * 128], psT3, r=[bpsT3], w=[bqnT])

    SKB, SKC = 1, 2
    for s_i in range(NT + SKC):
        if s_i < NT:
            stageA(s_i)
        if 0 <= s_i - SKB < NT:
            stageB(s_i - SKB)
        if 0 <= s_i - SKC < NT:
            stageC(s_i - SKC)
    if dbg:
        S.dma("sp", dbgo["d_kvnT"][:, :], kvnT.rearrange("p a b -> p (a b)"), r=[bkvnT], key="dbg")
    S.barrier()
    if stop_after == "B":
        S.emit(); c.st.close(); return nc
    A.reset(mX1)

    qupb = A.alloc([2, 768], BF16); bqup = Buf()
    qnc = A.alloc([2], F32); bqnc = Buf()
    qrow = [A.alloc([8, 96], BF16) for _ in range(2)]; bqrow = [Buf(), Buf()]
    rtq = [A.alloc([512], F32) for _ in range(2)]; brtq = [Buf(), Buf()]
    xs = [A.alloc([8, 32], F32) for _ in range(2)]; bxs = [Buf(), Buf()]
    qA = [A.alloc([256], F32) for _ in range(2)]; qB = [A.alloc([256], F32) for _ in range(2)]; bqAB = [Buf(), Buf()]
    qo = [A.alloc([8, 32], BF16) for _ in range(2)]; bqo = [Buf(), Buf()]
    S.dma("pool", qupb, qup.rearrange("(kc p) n -> p kc n", p=128), w=[bqup])
    S.dma("sp", qnc, qnorm[:, :], w=[bqnc])
    for kc in range(2):
        kk.ts("dve", qupb[:, kc, :], qupb[:, kc, :], qnc[:, kc:kc + 1], None, ALU.mult, None, r=[bqup, bqnc], w=[bqup])
    ps_qa = [psbank(c, 0, 1024, nb=2), psbank(c, 2, 1024, nb=2)]; bpsqa = [Buf(), Buf()]
    ps_qt = [psbank(c, 4, 1024, BF16, parts=96).rearrange("p (a b) -> p a b", a=8),
             psbank(c, 5, 1024, BF16, parts=96).rearrange("p (a b) -> p a b", a=8)]
    bpsqt = [Buf(), Buf()]
    for il in range(16):
        p = il % 2
        S.dma("sp", rtq[p], ropeq[il], w=[brtq[p]])
        for (h0, nh) in ((0, 5), (5, 3)):
            pq = ps_qa[p][:, (0 if h0 == 0 else 512):(0 if h0 == 0 else 512) + nh * 96]
            for kc in range(2):
                kk.mm(pq, qnT[:, kc, il * 128:(il + 1) * 128], qupb[:, kc, h0 * 96:(h0 + nh) * 96], kc == 0, kc == 1,
                      r=[bqnT, bqup], w=[bpsqa[p]])
            qv = pq.rearrange("p (h d) -> p h d", h=nh)
            kk.cp("act", qrow[p][:, h0:h0 + nh, 0:64], qv[:, :, 0:64], r=[bpsqa[p]], w=[bqrow[p]])
            kk.cp("act", xs[p][:, h0:h0 + nh, :], qv[:, :, 64:96], r=[bpsqa[p]], w=[bxs[p]])
        xs2 = xs[p].rearrange("p h d -> p (h d)")
        kk.tt("dve", qA[p], xs2, rtq[p][:, 0:256], ALU.mult, r=[bxs[p], brtq[p]], w=[bqAB[p]])
        kk.tt("dve", qB[p], xs2, rtq[p][:, 256:512], ALU.mult, r=[bxs[p], brtq[p]], w=[bqAB[p]])
        rA = qA[p].rearrange("p (m g e) -> p m g e", g=2, e=8)
        rB = qB[p].rearrange("p (m g e) -> p m g e", g=2, e=8)
        ro = qo[p].rearrange("p h (a g e) -> p (h a) g e", a=2, g=2)
        kk.tt("pool", ro[:, :, 0, :], rA[:, :, 0, :], rB[:, :, 1, :], ALU.subtract, r=[bqAB[p]], w=[bqo[p]])
        kk.tt("pool", ro[:, :, 1, :], rA[:, :, 1, :], rB[:, :, 0, :], ALU.add, r=[bqAB[p]], w=[bqo[p]])
        kk.cp("pool", qrow[p][:, :, 64:96], qo[p], r=[bqo[p]], w=[bqrow[p]])
        for h in range(8):
            kk.tr(ps_qt[p][:, h, :], qrow[p][:, h, :], r=[bqrow[p]], w=[bpsqt[p]])
        kk.cp("dve" if il % 2 else "act", qT[:, :, il * 128:(il + 1) * 128], ps_qt[p], r=[bpsqt[p]], w=[bqT])
    if dbg:
        S.dma("sp", dbgo["d_qT"][:, :], qT.rearrange("p a b -> p (a b)"), r=[bqT], key="dbg")
    S.barrier()
    if stop_after == "C":
        S.emit(); c.st.close(); return nc
    A.reset(mD)

    OnT = A.alloc([8, 2048], BF16, parts=64); bOnT = Buf()
    mE = A.mark()
    kvkb = A.alloc([2, 512], BF16); kvvb = A.alloc([2, 512], BF16); bkvw = Buf()
    kvnc = A.alloc([2], F32); bkvnc = Buf()
    Vb = [A.alloc([66, 65], BF16) for _ in range(2)]; bVb = [Buf(), Buf()]
    PT = [A.alloc([512], BF16) for _ in range(4)]; bPT = [Buf() for _ in range(4)]
    rrow = A.alloc([512], F32, parts=65); brrow = Buf()
    osb = A.alloc([512], F32, parts=64); bosb = Buf()
    S.dma("pool", kvkb, kvupk.rearrange("(kc p) n -> p kc n", p=128), w=[bkvw], key="kvk")
    S.dma("pool", kvvb, kvupv.rearrange("(kc p) n -> p kc n", p=128), w=[bkvw], key="kvv")
    S.dma("sp", kvnc, kvnorm[:, :], w=[bkvnc])
    for kc in range(2):
        kk.ts("dve", kvkb[:, kc, :], kvkb[:, kc, :], kvnc[:, kc:kc + 1], None, ALU.mult, None, r=[bkvw, bkvnc], w=[bkvw])
        kk.ts("dve", kvvb[:, kc, :], kvvb[:, kc, :], kvnc[:, kc:kc + 1], None, ALU.mult, None, r=[bkvw, bkvnc], w=[bkvw])
    for i in range(2):
        kk.memset("pool", Vb[i][:, :, 64:65], 1.0, w=[bVb[i]])
    ps_s = [psbank(c, b) for b in (0, 1, 2, 7)]; bpss = [Buf() for _ in range(4)]
    ps_o = [psbank(c, 3, parts=65), psbank(c, 4, parts=65)]; bpso = [Buf(), Buf()]
    ps_k = [psbank(c, 5), psbank(c, 6)]; bpsk = [Buf(), Buf()]
    ps_b = psbank(c, 5, parts=64); bpsb = bpsk[0]
    nkb = 66
    ei = 0
    def kv_for_head(h):
        KTb, bKTb = KT[h % 2], bKT[h % 2]
        V, bV = Vb[h % 2], bVb[h % 2]
        for ch in range(17):
            c0 = ch * 512
            c1 = min(c0 + 512, 8448)
            pk, bpk = ps_k[ch % 2], bpsk[ch % 2]
            for kc in range(2):
                kk.mm(pk[0:64, 0:c1 - c0], kvkb[:, kc, h * 64:(h + 1) * 64], kvnT[:, kc, c0:c1], kc == 0, kc == 1,
                      r=[bkvw, bkvnT], w=[bpk])
            kk.cp("dve", KTb[0:64, c0:c1], pk[0:64, 0:c1 - c0], r=[bpk], w=[bKTb])
        for g0 in range(0, 66, 8):
            g1 = min(g0 + 8, 66)
            pk, bpk = ps_k[(g0 // 8 + 1) % 2], bpsk[(g0 // 8 + 1) % 2]
            pv = pk.rearrange("p (a b) -> p a b", a=8)
            for t in range(g0, g1):
                for kc in range(2):
                    kk.mm(pv[:, t - g0, :], kvnT[:, kc, t * 128:(t + 1) * 128], kvvb[:, kc, h * 64:(h + 1) * 64],
                          kc == 0, kc == 1, r=[bkvnT, bkvw], w=[bpk])
            kk.cp("dve", V[:, g0:g1, 0:64], pv[:, 0:g1 - g0, :], r=[bpk], w=[bV])

    kv_for_head(0)
    for h in range(8):
        KTb, bKTb = KT[h % 2], bKT[h % 2]
        V, bV = Vb[h % 2], bVb[h % 2]
        for qc in range(4):
            if qc == 1 and h + 1 < 8:
                kv_for_head(h + 1)
            po, bpo = ps_o[ei % 2], bpso[ei % 2]
            ei += 1
            LOOK = 3

            def qk_exp(kb):
                j = kb % 4
                kk.mm(ps_s[j], KTb[0:96, kb * 128:(kb + 1) * 128], qT[0:96, h, qc * 512:(qc + 1) * 512], True, True,
                      r=[bKTb, bqT], w=[bpss[j]])
                kk.act(PT[j], ps_s[j], AF.Exp, r=[bpss[j]], w=[bPT[j]])

            for kb in range(min(LOOK, nkb)):
                qk_exp(kb)
            for kb in range(nkb):
                if kb + LOOK < nkb:
                    qk_exp(kb + LOOK)
                j = kb % 4
                kk.mm(po[0:65, :], V[:, kb, 0:65], PT[j], kb == 0, kb == nkb - 1, r=[bV, bPT[j]], w=[bpo])
            kk.recip(rrow[64:65, :], po[64:65, :], r=[bpo], w=[brrow])
            kk.mm(ps_b, ones_f[64:65, 0:64], rrow[64:65, :], True, True, r=[brrow, bconst], w=[bpsb])
            kk.cp("dve", osb, po[0:64, :], r=[bpo], w=[bosb])
            kk.tt("dve", OnT[:, h, qc * 512:(qc + 1) * 512], ps_b, osb, ALU.mult, r=[bpsb, bosb], w=[bOnT])
    if dbg:
        S.dma("sp", dbgo["d_KT"][:, :], KT[1], r=[bKT[1]], key="dbg")
        S.dma("sp", dbgo["d_OnT"][:, :], OnT.rearrange("p a b -> p (a b)"), r=[bOnT], key="dbg")
    S.barrier()
    if stop_after == "D":
        S.emit(); c.st.close(); return nc

    A2 = Arena(c.arena_t, mM, mX)
    woutp = A2.alloc([4, 1024], BF16); wouta = A2.alloc([8, 1024], BF16, parts=64); bwo = Buf()
    bandb = A2.alloc([28, 128], BF16); bband = Buf()
    poolwb = A2.alloc([4, 128], BF16); bpw = Buf()
    psc = A2.alloc([4], F32); bpsc = Buf()
    dT = A2.alloc([4, 512], BF16); bdT = Buf()
    poolT = A2.alloc([4, 512], BF16); bpoolT = Buf()
    xres = [A2.alloc([1024], F32) for _ in range(2)]; bxres = [Buf(), Buf()]
    tmpE = [A2.alloc([1024], F32) for _ in range(2)]; btmpE = [Buf(), Buf()]
    outE = [A2.alloc([1024], F32) for _ in range(2)]; boutE = [Buf(), Buf()]
    smE = [A2.alloc([16], F32) for _ in range(2)]; bsmE = [Buf(), Buf()]
    S.dma("pool", woutp, wout[0:512, :].rearrange("(g p) n -> p g n", p=128), w=[bwo], key="wo1")
    S.dma("pool", wouta, wout[512:1024, :].rearrange("(h p) n -> p h n", p=64), w=[bwo], key="wo2")
    S.dma("pool", bandb, bands.rearrange("k g s t -> s (k g) t"), w=[bband])
    S.dma("pool", poolwb, poolw.rearrange("g i o -> i g o"), w=[bpw])
    S.dma("sp", psc, pscale[:, :], w=[bpsc])
    ps_d = [psbank(c, b) for b in range(4)]; bpsd = [Buf() for _ in range(4)]
    ps_y = [psbank(c, 4), psbank(c, 5)]; bpsy = [Buf(), Buf()]
    ps_out = psbank(c, 6, 1024, nb=2); bpsout = Buf()
    for qc in range(4):
        for g in range(4):
            for tl in range(4):
                il = qc * 4 + tl
                if il == 0:
                    kinds = (3, 4, 2)
                elif il == 15:
                    kinds = (0, 5, 6)
                else:
                    kinds = (0, 1, 2)
                for o in range(3):
                    kk.mm(ps_d[g][:, tl * 128:(tl + 1) * 128], upool[:, il + o, g * 128:(g + 1) * 128],
                          bandb[:, kinds[o] * 4 + g, :], o == 0, o == 2, r=[bup[il + o], bband], w=[bpsd[g]])
            kk.cp("act", dT[:, g, :], ps_d[g], r=[bpsd[g]], w=[bdT])
            kk.mm(ps_y[g % 2], poolwb[:, g, :], dT[:, g, :], True, True, r=[bpw, bdT], w=[bpsy[g % 2]])
            kk.ts("dve", poolT[:, g, :], ps_y[g % 2], psc[:, g:g + 1], None, ALU.mult, None, r=[bpsy[g % 2], bpsc], w=[bpoolT])
        for tl in range(4):
            il = qc * 4 + tl
            p = il % 2
            S.dma("sp", xres[p], xkv[il + 2], w=[bxres[p]])
            for half in range(2):
                n0 = half * 512
                for g in range(4):
                    kk.mm(ps_out[:, n0:n0 + 512], poolT[:, g, tl * 128:(tl + 1) * 128], woutp[:, g, n0:n0 + 512],
                          g == 0, False, r=[bpoolT, bwo], w=[bpsout])
                for h in range(8):
                    kk.mm(ps_out[:, n0:n0 + 512], OnT[0:64, h, il * 128:(il + 1) * 128], wouta[0:64, h, n0:n0 + 512],
                          False, h == 7, r=[bOnT, bwo], w=[bpsout])
            ln_tile(c, kk, ps_out, bpsout, xres[p], bxres[p], g1b, lngb[:, 0, :], lnbb[:, 0, :], bconst,
                    tmpE[p], btmpE[p], outE[p], boutE[p], smE[p], bsmE[p])
            S.dma("sp", hscr[il], outE[p], r=[boutE[p]], key=f"hs{p}")
            if dbg:
                S.dma("sp", dbgo["d_h0a"][il], outE[p], r=[boutE[p]], key=f"dbgh{p}")
    S.barrier()
    if stop_after == "E":
        S.emit(); c.st.close(); return nc
    A.reset(mP)

    h1T = A.alloc([8, 2048], BF16); bh1T = Buf()
    hidT = A.alloc([22, 1024], BF16); bhid = Buf()
    wdb = A.alloc([22, 1024], BF16); bwd = Buf()
    wgb = [A.alloc([8, 256], BF16) for _ in range(2)]; wub = [A.alloc([8, 256], BF16) for _ in range(2)]
    bwgu = [Buf(), Buf()]
    xinF = [A.alloc([1024], F32) for _ in range(2)]; bxinF = [Buf(), Buf()]
    xbfF = [A.alloc([1024], BF16) for _ in range(2)]; bxbfF = [Buf(), Buf()]
    tmpTF = [A.alloc([8, 128], F32) for _ in range(2)]; btmpTF = [Buf(), Buf()]
    sg = [A.alloc([512], BF16) for _ in range(2)]; bsg = [Buf(), Buf()]
    tmpF = [A.alloc([1024], F32) for _ in range(2)]; btmpF = [Buf(), Buf()]
    outF = [A.alloc([1024], F32) for _ in range(2)]; boutF = [Buf(), Buf()]
    smF = [A.alloc([16], F32) for _ in range(2)]; bsmF = [Buf(), Buf()]
    ob16, bob16 = xbfF, bxbfF
    wd_v = wd.rearrange("(c p) n -> p c n", p=128)
    for q4 in range(2):
        S.dma("pool", wdb[:, q4 * 11:(q4 + 1) * 11, :], wd_v[:, q4 * 11:(q4 + 1) * 11, :], w=[bwd], key=f"wd{q4}")
    psTF = [psbank(c, 0, 1024, BF16).rearrange("p (a b) -> p a b", a=8),
            psbank(c, 1, 1024, BF16).rearrange("p (a b) -> p a b", a=8)]
    bpsTF = [Buf(), Buf()]
    for il in range(16):
        p = il % 2
        S.dma("sp", xinF[p], hscr[il], w=[bxinF[p]])
        kk.cp("act", xbfF[p], xinF[p], r=[bxinF[p]], w=[bxbfF[p]])
        for kc in range(8):
            kk.tr(psTF[p][:, kc, :], xbfF[p][:, kc * 128:(kc + 1) * 128], r=[bxbfF[p]], w=[bpsTF[p]])
        kk.tt("dve", tmpTF[p], psTF[p], bcast_mid(colmod[:, 24:32, 0], 128), ALU.mult, r=[bpsTF[p], bconst], w=[btmpTF[p]])
        kk.tt("dve", h1T[:, :, il * 128:(il + 1) * 128], tmpTF[p], bcast_mid(colmod[:, 16:24, 0], 128), ALU.add,
              r=[btmpTF[p], bconst], w=[bh1T])
    ps_g = [psbank(c, 2), psbank(c, 3)]; bpsg = [Buf(), Buf()]
    ps_u = [psbank(c, 4), psbank(c, 5)]; bpsu = [Buf(), Buf()]
    ps_o2 = psbank(c, 6, 1024, nb=2); bpso2 = Buf()
    wg_v = wg.rearrange("(kc p) n -> p kc n", p=128)
    wu_v = wu.rearrange("(kc p) n -> p kc n", p=128)
    wi = 0
    ci = 0
    for tb in range(2):
        for fb in range(11):
            p = wi % 2
            wi += 1
            S.dma("pool", wgb[p], wg_v[:, :, fb * 256:(fb + 1) * 256], w=[bwgu[p]], key=f"wg{p}")
            S.dma("pool", wub[p], wu_v[:, :, fb * 256:(fb + 1) * 256], w=[bwgu[p]], key=f"wu{p}")
            for fc in range(2):
                ffc = fb * 2 + fc
                for ts_ in range(2):
                    q = ci % 2
                    ci += 1
                    t0 = tb * 1024 + ts_ * 512
                    for kc in range(8):
                        kk.mm(ps_g[q], wgb[p][:, kc, fc * 128:(fc + 1) * 128], h1T[:, kc, t0:t0 + 512], kc == 0, kc == 7,
                              r=[bwgu[p], bh1T], w=[bpsg[q]])
                    for kc in range(8):
                        kk.mm(ps_u[q], wub[p][:, kc, fc * 128:(fc + 1) * 128], h1T[:, kc, t0:t0 + 512], kc == 0, kc == 7,
                              r=[bwgu[p], bh1T], w=[bpsu[q]])
                    kk.act(sg[q], ps_g[q], AF.Silu, r=[bpsg[q]], w=[bsg[q]])
                    kk.tt("dve", hidT[:, ffc, ts_ * 512:(ts_ + 1) * 512], ps_u[q], sg[q], ALU.mult, r=[bpsu[q], bsg[q]], w=[bhid])
        for tl in range(8):
            il = tb * 8 + tl
            p = il % 2
            S.dma("sp", xinF[p], hscr[il], w=[bxinF[p]])
            for half in range(2):
                n0 = half * 512
                for ffc in range(22):
                    kk.mm(ps_o2[:, n0:n0 + 512], hidT[:, ffc, tl * 128:(tl + 1) * 128], wdb[:, ffc, n0:n0 + 512],
                          ffc == 0, ffc == 21, r=[bhid, bwd], w=[bpso2])
            ln_tile(c, kk, ps_o2, bpso2, xinF[p], bxinF[p], g2b, lngb[:, 1, :], lnbb[:, 1, :], bconst,
                    tmpF[p], btmpF[p], outF[p], boutF[p], smF[p], bsmF[p])
            if link is None:
                S.dma("sp", h1[il], outF[p], r=[boutF[p]], key=f"ho{p}")
            else:
                S.dma("sp", link["h1res"][il], outF[p], r=[boutF[p]], w=[link["bh1res"][il]], key=f"ho{p}")
                kk.cp("act", ob16[p], outF[p], r=[boutF[p]], w=[bob16[p]])
                S.dma("sp", link["xsrc"][il // 4][(il % 4) * 128:(il % 4 + 1) * 128, :], ob16[p], r=[bob16[p]],
                      w=[link["bxsrc"][il // 4]], key=f"hx{p}")
    S.barrier()
    if not own:
        return None
    S.emit()
    c.st.close()
    return nc


def mod_phase(c, kk, cs, adaw, adabcol, adabrow, colmod, g1b, g2b, bconst, need=(0, 1, 2, 3, 4, 5)):
    S, A = c.S, c.A
    m0 = A.mark()
    s2 = A.alloc([8, 2], F32); bs2 = Buf()
    s_bc = A.alloc([8, 128], F32); bsbc = Buf()
    wblk = [A.alloc([8, 512], F32) for _ in range(2)]; bwblk = [Buf(), Buf()]
    brow = [A.alloc([1024], F32) for _ in range(2)]; bbrow = [Buf(), Buf()]
    abc = A.alloc([32], F32); babc = Buf()
    S.dma("sp", s2.rearrange("p a b -> p (a b)"), cs[:, :], w=[bs2])
    S.dma("sp", abc, adabcol[:, :], w=[babc])
    S.dma("sp", brow[0], adabrow[0, 2048:3072].partition_broadcast(128), w=[bbrow[0]])
    S.dma("sp", brow[1], adabrow[0, 5120:6144].partition_broadcast(128), w=[bbrow[1]])
    kk.act(s2, s2, AF.Silu, r=[bs2], w=[bs2])
    kk.cp("dve", s_bc, bcast_mid(s2[:, :, 0], 128), r=[bs2], w=[bsbc])
    ps_col = psbank(c, 0, 64).rearrange("p (a b) -> p a b", a=32); bpscol = Buf()
    ps_row = [psbank(c, 1), psbank(c, 2)]; bpsrow = [Buf(), Buf()]
    adaw_v = adaw.rearrange("(kc p) n -> p kc n", p=128)
    colidx = {0: 0, 1: 1, 3: 2, 4: 3}
    gdst = {2: (g1b, brow[0], bbrow[0]), 5: (g2b, brow[1], bbrow[1])}
    for bi in range(12):
        v, half = bi // 2, bi % 2
        if v not in need or (v not in colidx and gdst[v][0] is None):
            continue
        wb, bwb = wblk[bi % 2], bwblk[bi % 2]
        S.dma("sp", wb, adaw_v[:, :, bi * 512:(bi + 1) * 512], w=[bwb])
        if v in colidx:
            for fcl in range(4):
                slot = colidx[v] * 8 + half * 4 + fcl
                for kc in range(8):
                    kk.mm(ps_col[:, slot, :], wb[:, kc, fcl * 128:(fcl + 1) * 128], s2[:, kc, :], kc == 0, kc == 7,
                          r=[bwb, bs2], w=[bpscol])
        else:
            pr, bpr = ps_row[half], bpsrow[half]
            for kc in range(8):
                kk.mm(pr, s_bc[:, kc, :], wb[:, kc, :], kc == 0, kc == 7, r=[bwb, bsbc], w=[bpr])
            gt, brt, bbrt = gdst[v]
            kk.tt("dve", gt[:, half * 512:(half + 1) * 512], pr, brt[:, half * 512:(half + 1) * 512], ALU.add,
                  r=[bpr, bbrt], w=[bconst])
    for v in need:
        if v not in colidx:
            continue
        ci = colidx[v]
        kk.tt("dve", colmod[:, ci * 8:(ci + 1) * 8, :], ps_col[:, ci * 8:(ci + 1) * 8, :], bcast_mid(abc[:, ci * 8:(ci + 1) * 8], 2),
              ALU.add, r=[bpscol, babc], w=[bconst])
        if ci in (1, 3):
            kk.ts("dve", colmod[:, ci * 8:(ci + 1) * 8, :], colmod[:, ci * 8:(ci + 1) * 8, :], 1.0, None, ALU.add, None,
                  r=[bconst], w=[bconst])
    S.barrier()
    A.reset(m0)


CG = 32


def build_l2(dbg=False, stop_after=None, c=None, pre="", link=None):
    own = c is None
    if own:
        c = new_prog()
    nc, S, A = c.nc, c.S, c.A
    A.reset(0)

    def D(name, shape, dt=F32, kind="ExternalInput"):
        return nc.dram_tensor(pre + name, shape, dt, kind=kind).ap()

    hin = D("hin", [64, 128, 1024]) if link is None else None
    cs = D("cs", [128, 16])
    adaw = D("adaw", [1024, 6144]); adabcol = D("adabcol", [128, 32]); adabrow = D("adabrow", [1, 6144])
    win = D("win", [1024, 768]); convw = D("convw", [128, 18]); convb = D("convb", [128, 6])
    zemb = D("zemb", [33, 8192]); fw1 = D("fw1", [33, 64]); fw2 = D("fw2", [64, 64]); fw3 = D("fw3", [64, 64])
    fout = D("fout", [64, 512]); fbs = D("fbs", [64, 4])
    skipc = D("skipc", [128, 2]); deltac = D("deltac", [128, 2]); tvals = D("tvals", [1, 8192])
    f1cat = D("f1cat", [64, 1024]); f1inv = D("f1inv", [128, 512]); gmat = D("gmat", [128, 128 * 2 * 128])
    ident = D("ident", [128, 128])
    yout = D("yout", [256, 8192], BF16, kind="ExternalOutput") if link is None else None
    pscr = D("pscr", [256, 8192], BF16, kind="Internal")
    x0scr = D("x0scr", [256, 8192], BF16, kind="Internal")
    hfscr = D("hfscr", [256, 8192], BF16, kind="Internal")
    hbscr = D("hbscr", [256, 8192], BF16, kind="Internal")
    yscr = D("yscr", [256, 8192], F32, kind="Internal")
    dbgo = {}
    if dbg:
        dbgo["d_p"] = D("d_p", [256, 8192], BF16, kind="ExternalOutput")
        dbgo["d_x0"] = D("d_x0", [256, 8192], BF16, kind="ExternalOutput")
        dbgo["d_hf"] = D("d_hf", [256, 8192], BF16, kind="ExternalOutput")
        dbgo["d_hb"] = D("d_hb", [256, 8192], BF16, kind="ExternalOutput")
        dbgo["d_y"] = D("d_y", [256, 8192], F32, kind="ExternalOutput")

    identb = A.alloc([128], BF16)
    kk = K(S, identb)
    colmod = A.alloc([32, 2], F32)
    bconst = Buf("const")
    S.dma("pool", identb, ident[:, :], w=[kk.bident])
    mP = A.mark()
    mod_phase(c, kk, cs, adaw, adabcol, adabrow, colmod, None, None, bconst, need=(0, 1))

    zT = A.alloc([6, 8194], BF16); bz = [Buf() for _ in range(6)]
    winb = A.alloc([8, 768], BF16); bwin = Buf()
    cw = A.alloc([18], F32); cb = A.alloc([6], F32); bcw = Buf()
    uTb = [A.alloc([8, 512], BF16) for _ in range(2)]; buTb = [Buf(), Buf()]
    xin = [A.alloc([1024], F32) for _ in range(3)]; bxin = [Buf() for _ in range(3)]
    xbf = [A.alloc([1024], BF16) for _ in range(2)]; bxbf = [Buf(), Buf()]
    tmpT = [A.alloc([8, 128], F32) for _ in range(2)]; btmpT = [Buf(), Buf()]
    ct = [A.alloc([2048], F32) for _ in range(3)]; bct = [Buf() for _ in range(3)]
    ob = [A.alloc([2048], BF16) for _ in range(2)]; bob = [Buf(), Buf()]
    S.dma("pool", winb, win.rearrange("(kc p) n -> p kc n", p=128), w=[bwin])
    S.dma("sp", cw, convw[:, :], w=[bcw], key="cw")
    S.dma("sp", cb, convb[:, :], w=[bcw], key="cb")
    for ch in range(6):
        kk.memset("pool", zT[:, ch, 0:1], 0.0, w=[bz[ch]])
        kk.memset("pool", zT[:, ch, 8193:8194], 0.0, w=[bz[ch]])
    psT = [psbank(c, 0, 1024, BF16).rearrange("p (a b) -> p a b", a=8),
           psbank(c, 1, 1024, BF16).rearrange("p (a b) -> p a b", a=8)]
    bpsT = [Buf(), Buf()]
    ps_z = [psbank(c, 2), psbank(c, 3), psbank(c, 4)]; bpsz = [Buf() for _ in range(3)]
    zi = 0

    def p1_prep(tb):
        u, bu = uTb[tb % 2], buTb[tb % 2]
        for tl in range(4):
            i = tb * 4 + tl
            xi, bxi = xin[i % 3], bxin[i % 3]
            if link is None:
                S.dma("sp", xi, hin[i], w=[bxi])
                kk.cp("act", xbf[i % 2], xi, r=[bxi], w=[bxbf[i % 2]])
            else:
                r_, k_, m_ = i // 16, (i % 16) // 4, i % 4
                S.dma("sp", xbf[i % 2], link["xdst"][k_][512 * r_ + 128 * m_:512 * r_ + 128 * m_ + 128, :],
                      r=[link["bxdst"][k_]], w=[bxbf[i % 2]], key=f"xb{i % 2}")
            for kc in range(8):
                kk.tr(psT[i % 2][:, kc, :], xbf[i % 2][:, kc * 128:(kc + 1) * 128], r=[bxbf[i % 2]], w=[bpsT[i % 2]])
            kk.tt("dve", tmpT[i % 2], psT[i % 2], bcast_mid(colmod[:, 8:16, 0], 128), ALU.mult, r=[bpsT[i % 2], bconst], w=[btmpT[i % 2]])
            kk.tt("dve", u[:, :, tl * 128:(tl + 1) * 128], tmpT[i % 2], bcast_mid(colmod[:, 0:8, 0], 128), ALU.add,
                  r=[btmpT[i % 2], bconst], w=[bu])

    def p1_proj(tb):
        nonlocal zi
        u, bu = uTb[tb % 2], buTb[tb % 2]
        for ch in range(6):
            pz, bpz = ps_z[zi % 3], bpsz[zi % 3]
            zi += 1
            for kc in range(8):
                kk.mm(pz, winb[:, kc, ch * 128:(ch + 1) * 128], u[:, kc, :], kc == 0, kc == 7, r=[bwin, bu], w=[bpz])
            kk.cp("act" if ch % 2 else "dve", zT[:, ch, 1 + tb * 512:1 + (tb + 1) * 512], pz, r=[bpz], w=[bz[ch]])

    p1_prep(0)
    for tb in range(16):
        if tb + 1 < 16:
            p1_prep(tb + 1)
        p1_proj(tb)
    ci = 0

    def sconv(ch, t0, n, dst, bdst, tmp, btmp):
        kk.act(tmp, zT[:, ch, 1 + t0:1 + t0 + n], AF.Identity, r=[bz[ch], bcw], w=[btmp],
               bias=cb[:, ch:ch + 1], scale=cw[:, ch * 3 + 1:ch * 3 + 2])
        kk.stt("dve", tmp, zT[:, ch, t0:t0 + n], cw[:, ch * 3:ch * 3 + 1], tmp, ALU.mult, ALU.add, r=[bz[ch], bcw, btmp], w=[btmp])
        kk.stt("dve", dst, zT[:, ch, 2 + t0:2 + t0 + n], cw[:, ch * 3 + 2:ch * 3 + 3], tmp, ALU.mult, ALU.add,
               r=[bz[ch], bcw, btmp], w=[bdst])

    oi = 0
    for q in range(2):
        for sb in range(4):
            t0 = sb * 2048
            o, bo = ob[oi % 2], bob[oi % 2]; oi += 1
            sconv(q, t0, 2048, o, bo, ct[0], bct[0])
            S.dma("sp", x0scr[q * 128:(q + 1) * 128, t0:t0 + 2048], o, r=[bo], key=f"x0s{oi % 2}")
            if dbg:
                S.dma("sp", dbgo["d_x0"][q * 128:(q + 1) * 128, t0:t0 + 2048], o, r=[bo], key=f"dbg{oi % 2}")
            sconv(2 + q, t0, 2048, ct[1], bct[1], ct[1], bct[1])
            sconv(4 + q, t0, 2048, ct[2], bct[2], ct[2], bct[2])
            o, bo = ob[oi % 2], bob[oi % 2]; oi += 1
            kk.tt("pool", o, ct[1], ct[2], ALU.mult, r=[bct[1], bct[2]], w=[bo])
            S.dma("sp", pscr[q * 128:(q + 1) * 128, t0:t0 + 2048], o, r=[bo], key=f"ps{oi % 2}")
            if dbg:
                S.dma("sp", dbgo["d_p"][q * 128:(q + 1) * 128, t0:t0 + 2048], o, r=[bo], key=f"dbg{oi % 2}")
    S.barrier()
    if stop_after == "P1":
        S.emit(); c.st.close(); return nc
    A.reset(mP)

    PI = math.pi
    ze = A.alloc([8192], F32, parts=33); bze = Buf()
    hA = A.alloc([8192], F32, parts=64); bhA = Buf()
    hB = A.alloc([8192], F32, parts=64); bhB = Buf()
    mt = A.alloc([8192], F32, parts=64); bmt = Buf()
    w1 = A.alloc([64], F32, parts=33); w2 = A.alloc([64], F32, parts=64); w3 = A.alloc([64], F32, parts=64)
    fo = A.alloc([512], F32, parts=64); fb = A.alloc([8], F32, parts=64); bfw = Buf()
    skc = A.alloc([2], F32); dlc = A.alloc([2], F32); bsk = Buf()
    tv = [A.alloc([512], F32) for _ in range(2)]; btv = [Buf(), Buf()]
    dec = [A.alloc([512], F32) for _ in range(2)]; bdec = [Buf(), Buf()]
    o32 = [A.alloc([512], F32) for _ in range(2)]; bo32 = [Buf(), Buf()]
    o16 = [A.alloc([512], BF16) for _ in range(4)]; bo16 = [Buf() for _ in range(4)]
    S.dma("sp", ze, zemb[:, :], w=[bze])
    S.dma("sp", w1, fw1[:, :], w=[bfw], key="fw1")
    S.dma("sp", w2, fw2[:, :], w=[bfw], key="fw2")
    S.dma("sp", w3, fw3[:, :], w=[bfw], key="fw3")
    S.dma("sp", fo, fout[:, :], w=[bfw], key="fo")
    S.dma("sp", fb[:, 0:4], fbs[:, :], w=[bfw], key="fbs")
    S.dma("sp", skc, skipc[:, :], w=[bsk], key="skc")
    S.dma("sp", dlc, deltac[:, :], w=[bsk], key="dlc")
    for l in range(3):
        kk.tt("dve", fb[:, 4 + l:5 + l], fb[:, l:l + 1], fb[:, 3:4], ALU.mult, r=[bfw], w=[bfw])
    kk.ts("dve", dlc, dlc, -1.0, None, ALU.mult, None, r=[bsk], w=[bsk])
    ps_f = [psbank(c, b) for b in range(4)]; bpsf = [Buf() for _ in range(4)]
    src, bsrc, K0 = ze, bze, 33
    layers = [(w1, hA, bhA), (w2, hB, bhB), (w3, hA, bhA)]
    for l, (wl, dst, bdst) in enumerate(layers):
        for blk in range(16):
            pf, bpf = ps_f[blk % 4], bpsf[blk % 4]
            kk.mm(pf[0:64, :], wl[0:K0, :], src[0:K0, blk * 512:(blk + 1) * 512], True, True, r=[bfw, bsrc], w=[bpf])
            kk.ts("dve", dst[:, blk * 512:(blk + 1) * 512], pf[0:64, :], fb[:, 3:4], fb[:, 4 + l:5 + l], ALU.mult, ALU.add,
                  r=[bpf, bfw], w=[bdst])
        for sb in range(4):
            xs_ = dst[:, sb * 2048:(sb + 1) * 2048]
            ms_ = mt[:, sb * 2048:(sb + 1) * 2048]
            for rep in range(1):
                kk.ts("dve", ms_, xs_, PI, -2 * PI, ALU.is_gt, ALU.mult, r=[bdst], w=[bmt])
                kk.tt("dve", xs_, xs_, ms_, ALU.add, r=[bdst, bmt], w=[bdst])
                kk.ts("dve", ms_, xs_, -PI, 2 * PI, ALU.is_lt, ALU.mult, r=[bdst], w=[bmt])
                kk.tt("dve", xs_, xs_, ms_, ALU.add, r=[bdst, bmt], w=[bdst])
            kk.act(xs_, xs_, AF.Sin, r=[bdst], w=[bdst])
        src, bsrc, K0 = dst, bdst, 64
    h3, bh3 = src, bsrc
    ps_o = [psbank(c, 4 + b) for b in range(4)]; bpso = [Buf() for _ in range(4)]
    oi = 0
    for blk in range(16):
        S.dma("sp", tv[blk % 2], tvals[0, blk * 512:(blk + 1) * 512].partition_broadcast(128), w=[btv[blk % 2]])
        for q in range(2):
            d_, bd_ = dec[q], bdec[q]
            kk.act(d_, tv[blk % 2], AF.Exp, r=[btv[blk % 2], bsk], w=[bd_], scale=dlc[:, q:q + 1])
            for kind in range(2):
                cc = kind * 2 + q
                po, bpo = ps_o[cc], bpso[cc]
                kk.mm(po, fo[0:64, cc * 128:(cc + 1) * 128], h3[0:64, blk * 512:(blk + 1) * 512], True, True, r=[bfw, bh3], w=[bpo])
                o3, bo3 = o32[kind], bo32[kind]
                kk.tt("dve", o3, po, d_, ALU.mult, r=[bpo, bd_], w=[bo3])
                if blk == 0:
                    if kind == 0:
                        kk.ts("dve", o3[:, 0:1], o3[:, 0:1], skc[:, q:q + 1], None, ALU.add, None, r=[bo3, bsk], w=[bo3])
                    else:
                        kk.memset("dve", o3[:, 0:1], 0.0, w=[bo3])
                o6, bo6 = o16[oi % 4], bo16[oi % 4]
                kk.cp("pool", o6, o3, r=[bo3], w=[bo6])
                dstd = hfscr if kind == 0 else hbscr
                S.dma("sp", dstd[q * 128:(q + 1) * 128, blk * 512:(blk + 1) * 512], o6, r=[bo6], key=f"hs{oi % 4}")
                if dbg:
                    dd = dbgo["d_hf"] if kind == 0 else dbgo["d_hb"]
                    S.dma("sp", dd[q * 128:(q + 1) * 128, blk * 512:(blk + 1) * 512], o6, r=[bo6], key=f"dbgh{oi % 4}")
                oi += 1
    S.barrier()
    if stop_after == "P2":
        S.emit(); c.st.close(); return nc
    A.reset(mP)

    G = A.alloc([128, 2, 128], BF16); bG = Buf()
    f1c = A.alloc([1024], BF16, parts=64); f1i = A.alloc([512], BF16); bf1 = Buf()
    gv = gmat.rearrange("p (j r b) -> p j r b", j=128, r=2)
    for q4 in range(4):
        S.dma("pool", G[:, q4 * 32:(q4 + 1) * 32, :, :], gv[:, q4 * 32:(q4 + 1) * 32, :, :], w=[bG], key=f"G{q4}")
    S.dma("pool", f1c, f1cat[:, :], w=[bf1], key="f1c")
    S.dma("pool", f1i, f1inv[:, :], w=[bf1], key="f1i")
    xg = A.alloc([CG, 128], BF16, parts=64); bxg = Buf()
    hfg = A.alloc([CG, 128], BF16, parts=64); bhfg = Buf()
    hbg = A.alloc([CG, 128], BF16, parts=64); bhbg = Buf()
    A1f = A.alloc([128, CG, 2], BF16); A2f = A.alloc([128, CG, 2], BF16); bAf = Buf()
    A1b = A.alloc([128, CG, 2], BF16); A2b = A.alloc([128, CG, 2], BF16); bAb = Buf()
    Kf = A.alloc([128, CG * 2], F32); bKf = Buf()
    ysb = A2b.rearrange("p a b c -> p (a b c)")[0:64, :].bitcast(F32).rearrange("p (c n) -> p c n", c=CG); bysb = bAb
    tq = [A.alloc([256], F32) for _ in range(4)]; btq = [Buf() for _ in range(4)]
    Yt = A1b.rearrange("p a b c -> p (a b c)").rearrange("p (c r k) -> p c r k", c=CG, r=2)
    Ap1 = A2b
    ps_a = [psbank(c, b) for b in range(3)]; bpsa = [Buf() for _ in range(3)]
    ps_x = [psbank(c, 3 + b) for b in range(3)]; bpsx = [Buf() for _ in range(3)]
    ps_y = [psbank(c, 6 + b, parts=64) for b in range(2)]; bpsy = [Buf() for _ in range(2)]
    NB = 512 // (2 * CG)
    NBI = 512 // CG
    ai = 0
    xi_ = 0
    yi_ = 0

    def s1(xsrc, bxs, cl, rhs, A1, A2, bA):
        nonlocal ai
        pa, bpa = ps_a[ai % 3], bpsa[ai % 3]
        kk.mm(pa, xsrc[0:64, cl, :], rhs, True, True, r=[bxs, bf1], w=[bpa])
        e1, e2 = ("act", "dve") if ai % 2 == 0 else ("dve", "act")
        ai += 1
        kk.cp(e1, A1[:, :, cl, :], pa[:, 0:256].rearrange("p (r k) -> p k r", r=2), r=[bpa], w=[bA])
        kk.cp(e2, A2[:, :, cl, :], pa[:, 256:512].rearrange("p (r k) -> p k r", r=2), r=[bpa], w=[bA])

    for g in range(256 // CG):
        c0 = g * CG
        S.dma("sp", hfg, hfscr[c0:c0 + CG, :].rearrange("c (a b) -> a c b", b=128), w=[bhfg])
        S.dma("sp", hbg, hbscr[c0:c0 + CG, :].rearrange("c (a b) -> a c b", b=128), w=[bhbg])
        S.dma("sp", xg, pscr[c0:c0 + CG, :].rearrange("c (a b) -> a c b", b=128), w=[bxg])
        for cl in range(CG):
            s1(hfg, bhfg, cl, f1c[0:64, 0:512], A1f, A2f, bAf)
            s1(hbg, bhbg, cl, f1c[0:64, 512:1024], A1b, A2b, bAb)
        for k0 in range(0, 128, NB):
            px, bpx = ps_x[xi_ % 3], bpsx[xi_ % 3]; xi_ += 1
            pxv = px.rearrange("p (k n) -> p k n", k=NB)
            for kq in range(NB):
                k2 = k0 + kq
                kk.mm(pxv[:, kq, :], G[:, k2, 0, :], A1f[:, k2, :, :].rearrange("p c r -> p (c r)"), True, False, r=[bG, bAf], w=[bpx])
                kk.mm(pxv[:, kq, :], G[:, k2, 1, :], A2f[:, k2, :, :].rearrange("p c r -> p (c r)"), False, False, r=[bG, bAf], w=[bpx])
                kk.mm(pxv[:, kq, :], G[:, k2, 0, :], A1b[:, k2, :, :].rearrange("p c r -> p (c r)"), False, False, r=[bG, bAb], w=[bpx])
                kk.mm(pxv[:, kq, :], G[:, k2, 1, :], A2b[:, k2, :, :].rearrange("p c r -> p (c r)"), False, True, r=[bG, bAb], w=[bpx])
            kk.cp("act", Kf[:, k0:k0 + NB, :], pxv, r=[bpx], w=[bKf])
        for cl in range(CG):
            s1(xg, bxg, cl, f1c[0:64, 0:512], A1f, A2f, bAf)
        for k0 in range(0, 128, NB):
            px, bpx = ps_x[xi_ % 3], bpsx[xi_ % 3]; xi_ += 1
            pxv = px.rearrange("p (k n) -> p k n", k=NB)
            for kq in range(NB):
                k2 = k0 + kq
                kk.mm(pxv[:, kq, :], G[:, k2, 0, :], A1f[:, k2, :, :].rearrange("p c r -> p (c r)"), True, False, r=[bG, bAf], w=[bpx])
                kk.mm(pxv[:, kq, :], G[:, k2, 1, :], A2f[:, k2, :, :].rearrange("p c r -> p (c r)"), False, True, r=[bG, bAf], w=[bpx])
            Xr = px.rearrange("p (m r) -> p m r", r=2)[:, :, 0]
            Xi = px.rearrange("p (m r) -> p m r", r=2)[:, :, 1]
            Kv = Kf[:, k0:k0 + NB, :].rearrange("p k n -> p (k n)").rearrange("p (m r) -> p m r", r=2)
            kk.tt("dve", tq[0], Xr, Kv[:, :, 0], ALU.mult, r=[bpx, bKf], w=[btq[0]])
            kk.tt("dve", tq[1], Xi, Kv[:, :, 1], ALU.mult, r=[bpx, bKf], w=[btq[1]])
            kk.tt("dve", tq[2], Xr, Kv[:, :, 1], ALU.mult, r=[bpx, bKf], w=[btq[2]])
            kk.tt("dve", tq[3], Xi, Kv[:, :, 0], ALU.mult, r=[bpx, bKf], w=[btq[3]])
            t3 = [t.rearrange("p (k c) -> p k c", k=NB) for t in tq]
            kk.tt("pool", Yt[:, :, 0, k0:k0 + NB].rearrange("p c k -> p k c"), t3[0], t3[1], ALU.subtract, r=[btq[0], btq[1]], w=[bAb])
            kk.tt("pool", Yt[:, :, 1, k0:k0 + NB].rearrange("p c k -> p k c"), t3[2], t3[3], ALU.add, r=[btq[2], btq[3]], w=[bAb])
        for cl in range(CG):
            pa, bpa = ps_a[ai % 3], bpsa[ai % 3]
            kk.mm(pa[:, 0:256], Yt[:, cl, 0, :], f1i[:, 0:256], True, False, r=[bAb, bf1], w=[bpa])
            kk.mm(pa[:, 0:256], Yt[:, cl, 1, :], f1i[:, 256:512], False, True, r=[bAb, bf1], w=[bpa])
            kk.cp("act" if ai % 2 else "dve", A2f[:, :, cl, :], pa[:, 0:256].rearrange("p (r k) -> p k r", r=2), r=[bpa], w=[bAf])
            ai += 1
        for j0 in range(0, 128, NBI):
            py, bpy = ps_y[yi_ % 2], bpsy[yi_ % 2]; yi_ += 1
            pyv = py.rearrange("p (j c) -> p j c", j=NBI)
            for jq in range(NBI):
                j = j0 + jq
                kk.mm(pyv[:, jq, :], G[:, j, 0, 0:64], A2f[:, j, :, 0], True, False, r=[bG, bAf], w=[bpy])
                kk.mm(pyv[:, jq, :], G[:, j, 1, 0:64], A2f[:, j, :, 1], False, True, r=[bG, bAf], w=[bpy])
            kk.act(ysb[:, :, j0:j0 + NBI].rearrange("p c j -> p j c"), pyv, AF.Copy, r=[bpy], w=[bysb], scale=1.0 / 16384)
        S.dma("sp", yscr[c0:c0 + CG, :].rearrange("c (a b) -> a c b", b=128), ysb, r=[bysb], key="ysc")
        if dbg:
            S.dma("sp", dbgo["d_y"][c0:c0 + CG, :].rearrange("c (a b) -> a c b", b=128), ysb, r=[bysb], key="dbgy")
    S.barrier()
    if stop_after == "P3":
        S.emit(); c.st.close(); return nc
    A.reset(mP)

    yl = [A.alloc([2048], F32) for _ in range(2)]; byl = [Buf(), Buf()]
    xl = [A.alloc([2048], BF16) for _ in range(2)]; bxl = [Buf(), Buf()]
    ol = [A.alloc([2048], BF16) for _ in range(2)]; bol = [Buf(), Buf()]
    i = 0
    for q in range(2):
        for sb in range(4):
            p = i % 2; i += 1
            S.dma("sp", yl[p], yscr[q * 128:(q + 1) * 128, sb * 2048:(sb + 1) * 2048], w=[byl[p]])
            S.dma("sp", xl[p], x0scr[q * 128:(q + 1) * 128, sb * 2048:(sb + 1) * 2048], w=[bxl[p]])
            kk.tt("dve", ol[p], yl[p], xl[p], ALU.mult, r=[byl[p], bxl[p]], w=[bol[p]])
            if link is None:
                S.dma("sp", yout[q * 128:(q + 1) * 128, sb * 2048:(sb + 1) * 2048], ol[p], r=[bol[p]], key=f"yo{p}")
            else:
                for hh in range(2):
                    S.dma("sp", link["ysrc"][2 * q + hh][:, sb * 2048:(sb + 1) * 2048], ol[p][hh * 64:(hh + 1) * 64, :],
                          r=[bol[p]], w=[link["bysrc"][2 * q + hh]], key=f"yo{p}{hh}")
    S.barrier()
    if not own:
        return None
    S.emit()
    c.st.close()
    return nc


def build_l3(dbg=False, stop_after=None, c=None, pre="", link=None):
    own = c is None
    if own:
        c = new_prog()
    nc, S, A = c.nc, c.S, c.A
    A.reset(0)

    def D(name, shape, dt=F32, kind="ExternalInput"):
        return nc.dram_tensor(pre + name, shape, dt, kind=kind).ap()

    yT = D("yT", [128, 8 * 2048], BF16) if link is None else None
    hres = D("hres", [16, 128, 1024]) if link is None else link["h1res"]
    cs = D("cs", [128, 16])
    adaw = D("adaw", [1024, 6144]); adabcol = D("adabcol", [128, 32]); adabrow = D("adabrow", [1, 6144])
    lng = D("lng", [2, 1024]); lnb = D("lnb", [2, 1024])
    wout = D("wout", [1024, 1024]); rw = D("rw", [128, 64])
    mg = D("mg", [8, 1024, 3584]); mu = D("mu", [8, 1024, 3584]); md = D("md", [8, 3584, 1024])
    ident = D("ident", [128, 128])
    out = D("out", [16, 128, 1024], kind="ExternalOutput")
    hscr = D("hscr", [16, 128, 1024], kind="Internal")
    dbgo = {}
    if dbg:
        dbgo["d_h1a"] = D("d_h1a", [16, 128, 1024], kind="ExternalOutput")
        dbgo["d_comb"] = D("d_comb", [128, 128], kind="ExternalOutput")
        dbgo["d_acc"] = D("d_acc", [128, 16 * 1024], kind="ExternalOutput")

    identb = A.alloc([128], BF16)
    kk = K(S, identb)
    identf = A.alloc([128], F32)
    colmod = A.alloc([32, 2], F32)
    g1b = A.alloc([1024], F32); g2b = A.alloc([1024], F32)
    lngb = A.alloc([2, 1024], F32); lnbb = A.alloc([2, 1024], F32)
    epsc = A.alloc([4], F32)
    comb = A.alloc([16, 8], F32); bcomb = Buf()
    c.eps_ln = epsc[:, 0:1]
    bconst = Buf("const")
    S.dma("pool", identb, ident[:, :], w=[kk.bident], key="idb")
    S.dma("sp", identf, ident[:, :], w=[kk.bident], key="idf")
    kk.memset("dve", epsc[:, 0:1], LN_EPS, w=[bconst])
    for i in range(2):
        S.dma("sp", lngb[:, i, :], lng[i, :].partition_broadcast(128), w=[bconst], key=f"c{i}a")
        S.dma("sp", lnbb[:, i, :], lnb[i, :].partition_broadcast(128), w=[bconst], key=f"c{i}b")
    mP = A.mark()
    mod_phase(c, kk, cs, adaw, adabcol, adabrow, colmod, g1b, g2b, bconst, need=(2, 3, 4, 5))

    tT = A.alloc([8, 2048], BF16); btT = Buf()
    mP2 = A.mark()
    yTb = A.alloc([8, 2048], BF16); byT = Buf()
    woutb = A.alloc([8, 1024], BF16); bwo = Buf()
    rwf = A.alloc([8, 8], F32); brw = Buf()
    xres = [A.alloc([1024], F32) for _ in range(2)]; bxres = [Buf(), Buf()]
    tmpE = [A.alloc([1024], F32) for _ in range(2)]; btmpE = [Buf(), Buf()]
    outE = [A.alloc([1024], F32) for _ in range(2)]; boutE = [Buf(), Buf()]
    smE = [A.alloc([16], F32) for _ in range(2)]; bsmE = [Buf(), Buf()]
    tmpT = [A.alloc([8, 128], F32) for _ in range(2)]; btmpT = [Buf(), Buf()]
    t32 = [A.alloc([8, 128], F32) for _ in range(2)]; bt32 = [Buf(), Buf()]
    rs = [A.alloc([48], F32) for _ in range(2)]; brs = [Buf(), Buf()]
    if link is None:
        S.dma("sp", yTb.rearrange("p a b -> p (a b)"), yT[:, :], w=[byT])
    else:
        for r_ in range(4):
            for k_ in range(4):
                src = link["ydst"][k_]

                def lazy(eng, src=src, r_=r_):
                    if "off" not in c.dyn:
                        c.dyn["off"] = (eng.partition_id() % 4) * 1024
                    return src[r_ * 64:(r_ + 1) * 64, bass.ds(c.dyn["off"], 1024)]
                S.dma("sp", yTb[(k_ % 2) * 64:(k_ % 2) * 64 + 64, 2 * r_ + k_ // 2, :].bitcast(F32), lazy,
                      r=[link["bydst"][k_]], w=[byT], key=f"yl{(r_ * 4 + k_) % 4}")
    S.dma("pool", woutb, wout.rearrange("(kc p) n -> p kc n", p=128), w=[bwo])
    S.dma("sp", rwf.rearrange("p a b -> p (a b)"), rw[:, :], w=[brw])
    ps_out = psbank(c, 0, 1024, nb=2); bpsout = Buf()
    psTf = [psbank(c, 2, 1024, nb=2).rearrange("p (a b) -> p a b", a=8),
            psbank(c, 4, 1024, nb=2).rearrange("p (a b) -> p a b", a=8)]
    bpsTf = [Buf(), Buf()]
    ps_l = psbank(c, 6, 8); bpsl = Buf()
    def ph2_a(il):
        p = il % 2
        S.dma("sp", xres[p], hres[il], r=([] if link is None else [link["bh1res"][il]]), w=[bxres[p]])
        for half in range(2):
            n0 = half * 512
            for kc in range(8):
                kk.mm(ps_out[:, n0:n0 + 512], yTb[:, kc, il * 128:(il + 1) * 128], woutb[:, kc, n0:n0 + 512], kc == 0, kc == 7,
                      r=[byT, bwo], w=[bpsout])
        ln_tile(c, kk, ps_out, bpsout, xres[p], bxres[p], g1b, lngb[:, 0, :], lnbb[:, 0, :], bconst,
                tmpE[p], btmpE[p], outE[p], boutE[p], smE[p], bsmE[p])
        S.dma("sp", hscr[il], outE[p], r=[boutE[p]], key=f"hs{p}")
        if dbg:
            S.dma("sp", dbgo["d_h1a"][il], outE[p], r=[boutE[p]], key=f"dbgh{p}")

    def ph2_b(il):
        p = il % 2
        for kc in range(8):
            S.op("pe", (lambda o, i_: (lambda e: e.transpose(out=o, in_=i_, identity=identf)))(psTf[p][:, kc, :], outE[p][:, kc * 128:(kc + 1) * 128]),
                 r=[boutE[p], kk.bident], w=[bpsTf[p]])
        for hb_ in range(2):
            kk.tt("dve", tmpT[p][:, hb_ * 4:(hb_ + 1) * 4, :], psTf[p][:, hb_ * 4:(hb_ + 1) * 4, :],
                  bcast_mid(colmod[:, 24 + hb_ * 4:28 + hb_ * 4, 0], 128), ALU.mult, r=[bpsTf[p], bconst], w=[btmpT[p]])
        kk.tt("dve", t32[p], tmpT[p], bcast_mid(colmod[:, 16:24, 0], 128), ALU.add, r=[btmpT[p], bconst], w=[bt32[p]])
        kk.cp("act", tT[:, :, il * 128:(il + 1) * 128], t32[p], r=[bt32[p]], w=[btT])
        for kc in range(8):
            kk.mm(ps_l, t32[p][:, kc, :], rwf[:, kc, :], kc == 0, kc == 7, r=[bt32[p], brw], w=[bpsl])
        r_ = rs[p]; br_ = brs[p]
        lg = r_[:, 0:8]; eq1 = r_[:, 8:16]; l2 = r_[:, 16:24]; eq2 = r_[:, 24:32]
        m1 = r_[:, 32:33]; m2 = r_[:, 33:34]; dg = r_[:, 34:35]; ga = r_[:, 35:36]; gb = r_[:, 36:37]
        kk.cp("dve", lg, ps_l, r=[bpsl], w=[br_])
        S.op("dve", (lambda o, i_: (lambda e: e.tensor_reduce(out=o, in_=i_, axis=mybir.AxisListType.X, op=ALU.max)))(m1, lg), r=[br_], w=[br_])
        kk.ts("dve", eq1, lg, m1, None, ALU.is_equal, None, r=[br_], w=[br_])
        kk.stt("dve", l2, eq1, -1e30, lg, ALU.mult, ALU.add, r=[br_], w=[br_])
        S.op("dve", (lambda o, i_: (lambda e: e.tensor_reduce(out=o, in_=i_, axis=mybir.AxisListType.X, op=ALU.max)))(m2, l2), r=[br_], w=[br_])
        kk.ts("dve", eq2, l2, m2, None, ALU.is_equal, None, r=[br_], w=[br_])
        kk.tt("dve", dg, m1, m2, ALU.subtract, r=[br_], w=[br_])
        kk.act(ga, dg, AF.Sigmoid, r=[br_], w=[br_])
        kk.ts("dve", gb, ga, -1.0, 1.0, ALU.mult, ALU.add, r=[br_], w=[br_])
        kk.ts("dve", comb[:, il, :], eq1, ga, None, ALU.mult, None, r=[br_], w=[bcomb])
        kk.stt("dve", comb[:, il, :], eq2, gb, comb[:, il, :], ALU.mult, ALU.add, r=[br_, bcomb], w=[bcomb])

    ph2_a(0)
    for il in range(16):
        if il + 1 < 16:
            ph2_a(il + 1)
        ph2_b(il)
    if dbg:
        S.dma("sp", dbgo["d_comb"][:, :], comb.rearrange("p a b -> p (a b)"), r=[bcomb], key="dbgc")
    S.barrier()
    if stop_after == "P2":
        S.emit(); c.st.close(); return nc
    A.reset(mP2)

    acc = A.alloc([16, 1024], F32); bacc = [Buf() for _ in range(16)]
    mAcc = A.mark()
    hidT = A.alloc([4, 2048], BF16); bhid = Buf()
    wgq = [A.alloc([8, 512], BF16) for _ in range(2)]; wuq = [A.alloc([8, 512], BF16) for _ in range(2)]
    wdq = [A.alloc([4, 1024], BF16) for _ in range(2)]; bwq = [Buf(), Buf()]; bwdq = [Buf(), Buf()]
    sg = [A.alloc([512], BF16) for _ in range(2)]; bsg = [Buf(), Buf()]
    ps_g = [psbank(c, 0), psbank(c, 1)]; bpsg = [Buf(), Buf()]
    ps_u = [psbank(c, 2), psbank(c, 3)]; bpsu = [Buf(), Buf()]
    ps_d = [psbank(c, 4, 1024, nb=2), psbank(c, 6, 1024, nb=2)]; bpsd = [Buf(), Buf()]
    gi = 0
    ci = 0
    di = 0
    for e in range(8):
        mg_v = mg[e].rearrange("(kc p) n -> p kc n", p=128)
        mu_v = mu[e].rearrange("(kc p) n -> p kc n", p=128)
        md_v = md[e].rearrange("(c p) n -> p c n", p=128)
        for fg in range(7):
            p = gi % 2
            first = (gi == 0)
            gi += 1
            S.dma("pool", wgq[p], mg_v[:, :, fg * 512:(fg + 1) * 512], w=[bwq[p]], key=f"wg{p}")
            S.dma("pool", wuq[p], mu_v[:, :, fg * 512:(fg + 1) * 512], w=[bwq[p]], key=f"wu{p}")
            S.dma("pool", wdq[p], md_v[:, fg * 4:(fg + 1) * 4, :], w=[bwdq[p]], key=f"wd{p}")
            for fc in range(4):
                for ts_ in range(4):
                    q = ci % 2
                    ci += 1
                    t0 = ts_ * 512
                    for kc in range(8):
                        kk.mm(ps_g[q], wgq[p][:, kc, fc * 128:(fc + 1) * 128], tT[:, kc, t0:t0 + 512], kc == 0, kc == 7,
                              r=[bwq[p], btT], w=[bpsg[q]])
                    for kc in range(8):
                        kk.mm(ps_u[q], wuq[p][:, kc, fc * 128:(fc + 1) * 128], tT[:, kc, t0:t0 + 512], kc == 0, kc == 7,
                              r=[bwq[p], btT], w=[bpsu[q]])
                    kk.act(sg[q], ps_g[q], AF.Silu, r=[bpsg[q]], w=[bsg[q]])
                    kk.tt("dve", hidT[:, fc, t0:t0 + 512], ps_u[q], sg[q], ALU.mult, r=[bpsu[q], bsg[q]], w=[bhid])
            for il in range(16):
                d = di % 2
                di += 1
                for half in range(2):
                    n0 = half * 512
                    for fc in range(4):
                        kk.mm(ps_d[d][:, n0:n0 + 512], hidT[:, fc, il * 128:(il + 1) * 128], wdq[p][:, fc, n0:n0 + 512], fc == 0, fc == 3,
                              r=[bhid, bwdq[p]], w=[bpsd[d]])
                for half in range(2):
                    n0 = half * 512
                    if first:
                        kk.ts("dve", acc[:, il, n0:n0 + 512], ps_d[d][:, n0:n0 + 512], comb[:, il, e:e + 1], None, ALU.mult, None,
                              r=[bpsd[d], bcomb], w=[bacc[il]])
                    else:
                        kk.stt("dve", acc[:, il, n0:n0 + 512], ps_d[d][:, n0:n0 + 512], comb[:, il, e:e + 1], acc[:, il, n0:n0 + 512],
                               ALU.mult, ALU.add, r=[bpsd[d], bcomb, bacc[il]], w=[bacc[il]])
    if dbg:
        S.dma("sp", dbgo["d_acc"][:, :], acc.rearrange("p a b -> p (a b)"), r=bacc, key="dbga")
    S.barrier()
    A.reset(mAcc)
    xr2 = [A.alloc([1024], F32) for _ in range(2)]; bxr2 = [Buf(), Buf()]
    tmpF = [A.alloc([1024], F32) for _ in range(2)]; btmpF = [Buf(), Buf()]
    outF = [A.alloc([1024], F32) for _ in range(2)]; boutF = [Buf(), Buf()]
    smF = [A.alloc([16], F32) for _ in range(2)]; bsmF = [Buf(), Buf()]
    for il in range(16):
        p = il % 2
        S.dma("sp", xr2[p], hscr[il], w=[bxr2[p]])
        ln_tile(c, kk, acc[:, il, :], bacc[il], xr2[p], bxr2[p], g2b, lngb[:, 1, :], lnbb[:, 1, :], bconst,
                tmpF[p], btmpF[p], outF[p], boutF[p], smF[p], bsmF[p])
        S.dma("sp", out[il], outF[p], r=[boutF[p]], key=f"ho{p}")
    S.barrier()
    if not own:
        return None
    S.emit()
    c.st.close()
    return nc


def rope_table():
    n = 8192
    rows = n // 64
    r = np.repeat(np.arange(rows, dtype=np.float32), 64)
    col = np.tile(np.arange(64, dtype=np.float32), rows)
    inv = (np.float32(10000.0) ** (-np.arange(0, 16, 2, dtype=np.float32) / np.float32(16))).astype(np.float32)
    ar = r[:, None] * inv
    ac = col[:, None] * inv
    return np.concatenate([np.cos(ar), np.cos(ac), np.sin(ar), np.sin(ac)], axis=1).astype(np.float32)


def band_mats(j):
    n = 8192

    def A(G, o, g):
        m = np.zeros((128, 128), np.float32)
        Gs = G + o
        if Gs < 0 or Gs >= 64:
            return m
        half = POOL_WINDOWS[g] // 2
        for t in range(128):
            T = G * 128 + t
            lo = max(T - half, 0)
            hi = min(T + half, n)
            for Sx in range(lo, hi):
                s = Sx - Gs * 128
                if 0 <= s < 128:
                    m[s, t] += 1.0 / (hi - lo)
            s = T - Gs * 128
            if 0 <= s < 128:
                m[s, t] -= 1.0
        return m
    G0 = 16 * j
    Gm = 1
    out = np.zeros((7, 4, 128, 128), np.float32)
    for g in range(4):
        out[0, g] = A(Gm, -1, g); out[1, g] = A(Gm, 0, g); out[2, g] = A(Gm, 1, g)
        out[3, g] = A(G0, -1, g); out[4, g] = A(G0, 0, g)
        out[5, g] = A(G0 + 15, 0, g); out[6, g] = A(G0 + 15, 1, g)
    return out


def col_form(v, nchunk):
    return np.ascontiguousarray(v.reshape(nchunk, 128).T)


def l1_inputs(inp):
    x = inp["x"]; ctx = inp["ctx"]
    rt = rope_table()
    ident = np.eye(128, dtype=np.float32)
    ada_b0 = inp["ada_b"][0]
    adabcol = np.concatenate([col_form(ada_b0[v * 1024:(v + 1) * 1024], 8) for v in (0, 1, 3, 4)], axis=1)
    kv_up = inp["kv_up"][0].reshape(256, 8, 128)
    kvupk = np.ascontiguousarray(kv_up[:, :, :64].reshape(256, 512))
    kvupv = np.ascontiguousarray(kv_up[:, :, 64:].reshape(256, 512))
    maps = []
    for core in range(NCORES):
        b, j = core // 4, core % 4
        xt = x[b].reshape(64, 128, 1024)
        ct = ctx[b].reshape(2, 128, 1024)
        own = list(range(16 * j, 16 * j + 16))
        others = [t for t in range(64) if t not in own]
        order = own + others
        xkv = np.concatenate([ct, xt[order]], axis=0)
        rtt = rt.reshape(64, 128, 32)[order]
        r0 = np.zeros((2, 128, 32), np.float32); r0[:, :, 0:16] = 1.0
        ropet = np.concatenate([r0, rtt], axis=0)
        ro = rt.reshape(64, 128, 32)[own]
        cosq = np.broadcast_to(ro[:, :, None, 0:16].reshape(16, 128, 1, 2, 1, 8), (16, 128, 8, 2, 2, 8)).reshape(16, 128, 256)
        sinq = np.broadcast_to(ro[:, :, None, 16:32].reshape(16, 128, 1, 2, 1, 8), (16, 128, 8, 2, 2, 8)).reshape(16, 128, 256)
        ropeq = np.ascontiguousarray(np.concatenate([cosq, sinq], axis=2))
        xhalo = np.zeros((2, 128, 1024), np.float32)
        if 16 * j - 1 >= 0:
            xhalo[0] = xt[16 * j - 1]
        if 16 * j + 16 < 64:
            xhalo[1] = xt[16 * j + 16]
        cs = np.stack([col_form(inp["c"][b], 8), col_form(inp["c_ctx"], 8)], axis=2).reshape(128, 16)
        maps.append({
            "xkv": np.ascontiguousarray(xkv), "xhalo": xhalo, "ropeq": ropeq, "ropet": np.ascontiguousarray(ropet),
            "cs": np.ascontiguousarray(cs), "adaw": inp["ada_w"][0], "adabcol": np.ascontiguousarray(adabcol),
            "adabrow": ada_b0.reshape(1, 6144), "lng": inp["ln_g"][0], "lnb": inp["ln_b"][0],
            "win": inp["mix_in_w"][0], "poolw": inp["pool_w"][0], "pscale": col_form(inp["pool_scale"][0], 4),
            "qnorm": col_form(inp["q_norm"][0], 2), "kvnorm": col_form(inp["kv_norm"][0], 2),
            "qup": inp["q_up"][0], "kvupk": kvupk, "kvupv": kvupv, "wout": inp["mix_out_w"][0],
            "wg": inp["ffn_gate"][0], "wu": inp["ffn_up"][0], "wd": inp["ffn_down"][0],
            "bands": band_mats(j), "ident": ident,
        })
    return maps


HY_MIN_DECAY = math.log(1e-2) / 1.5
HY_MAX_DECAY = math.log(1e-2) / 0.3


def fft_consts():
    n1 = np.arange(64)[:, None]; k2 = np.arange(128)[None, :]
    th = 2 * np.pi * n1 * k2 / 128
    fr, fi = np.cos(th), -np.sin(th)
    f1cat = np.concatenate([fr, fi, -fi, fr, fr, -fi, -fi, -fr], axis=1).astype(np.float32)
    k1 = np.arange(128)[:, None]; n2 = np.arange(128)[None, :]
    th = 2 * np.pi * k1 * n2 / 128
    pr, pi_ = np.cos(th), np.sin(th)
    f1inv = np.concatenate([pr, pi_, -pi_, pr], axis=1).astype(np.float32)
    a = np.arange(128)[:, None, None]; j = np.arange(128)[None, :, None]; b = np.arange(128)[None, None, :]
    th = 2 * np.pi * ((a * (j + 128 * b)) % 16384) / 16384
    g = np.stack([np.cos(th), -np.sin(th)], axis=2).astype(np.float32)
    return f1cat, f1inv, np.ascontiguousarray(g.reshape(128, 128 * 2 * 128))


def filt_consts():
    n = 8192
    t = np.linspace(0.0, 1.0, n, dtype=np.float32)
    w_ang = (np.float32(2.0 * math.pi / n) * np.arange(n, dtype=np.float32))[:, None]
    bands = np.linspace(1e-4, 15, 16, dtype=np.float32)[None, :]
    z = np.concatenate([t[:, None], np.cos(bands * w_ang), -np.sin(bands * w_ang)], axis=-1).astype(np.float32)
    deltas = np.abs(np.linspace(HY_MIN_DECAY, HY_MAX_DECAY, 1024, dtype=np.float32))
    return np.ascontiguousarray(z.T), t.reshape(1, n).copy(), deltas


def mod_inputs(inp, layer, b):
    ada_b = inp["ada_b"][layer]
    adabcol = np.concatenate([col_form(ada_b[v * 1024:(v + 1) * 1024], 8) for v in (0, 1, 3, 4)], axis=1)
    cs = np.stack([col_form(inp["c"][b], 8), col_form(inp["c_ctx"], 8)], axis=2).reshape(128, 16)
    return {"cs": np.ascontiguousarray(cs), "adaw": inp["ada_w"][layer], "adabcol": np.ascontiguousarray(adabcol),
            "adabrow": ada_b.reshape(1, 6144)}


def l2_inputs(inp, h1):
    f1cat, f1inv, gmat = fft_consts()
    zembT, tvals, deltas = filt_consts()
    ident = np.eye(128, dtype=np.float32)
    w_in = inp["hy_in_w"][0]; cw = inp["hy_conv_w"][0]; cb = inp["hy_conv_b"][0]
    fout = inp["hy_fout"][0]
    fbs = np.stack([inp["hy_fb1"][0], inp["hy_fb2"][0], inp["hy_fb3"][0], inp["hy_freq"][0]], axis=1)
    maps = []
    for core in range(NCORES):
        b, j = core // 4, core % 4
        cols = np.concatenate([part * 1024 + 256 * j + np.arange(256) for part in range(3)])
        convw = np.zeros((128, 18), np.float32); convb = np.zeros((128, 6), np.float32)
        for ch in range(6):
            cc = cols[ch * 128:(ch + 1) * 128]
            convw[:, ch * 3:(ch + 1) * 3] = cw[:, cc].T
            convb[:, ch] = cb[cc]
        fcols = np.concatenate([kind * 1024 + 256 * j + np.arange(256) for kind in range(2)])
        m = {"win": np.ascontiguousarray(w_in[:, cols]),
             "convw": convw, "convb": convb, "zemb": zembT, "fw1": inp["hy_fw1"][0], "fw2": inp["hy_fw2"][0],
             "fw3": inp["hy_fw3"][0], "fout": np.ascontiguousarray(fout[:, fcols]), "fbs": np.ascontiguousarray(fbs),
             "skipc": col_form(inp["hy_skip"][0][256 * j:256 * j + 256], 2), "deltac": col_form(deltas[256 * j:256 * j + 256], 2),
             "tvals": tvals, "f1cat": f1cat, "f1inv": f1inv, "gmat": gmat, "ident": ident}
        if h1 is not None:
            m["hin"] = np.ascontiguousarray(h1[b].reshape(64, 128, 1024))
        m.update(mod_inputs(inp, 1, b))
        maps.append(m)
    return maps


def l3_inputs(inp, h1, yfull):
    ident = np.eye(128, dtype=np.float32)
    rw = np.ascontiguousarray(inp["router_w"][0].reshape(8, 128, 8).transpose(1, 0, 2).reshape(128, 64))
    maps = []
    for core in range(NCORES):
        b, j = core // 4, core % 4
        m = {"lng": inp["ln_g"][1], "lnb": inp["ln_b"][1], "wout": inp["hy_out_w"][0], "rw": rw,
             "mg": inp["moe_gate"][0], "mu": inp["moe_up"][0], "md": inp["moe_down"][0], "ident": ident}
        if yfull is not None:
            yt = yfull[b][:, 2048 * j:2048 * (j + 1)].reshape(8, 128, 2048).transpose(1, 0, 2).reshape(128, 8 * 2048)
            m["yT"] = np.ascontiguousarray(yt)
            m["hres"] = np.ascontiguousarray(h1[b, 2048 * j:2048 * (j + 1)].reshape(16, 128, 1024))
        m.update(mod_inputs(inp, 1, b))
        maps.append(m)
    return maps


G4 = [[0, 1, 2, 3], [4, 5, 6, 7]]


def build_fused():
    c = new_prog()
    nc, S = c.nc, c.S
    h1res = nc.dram_tensor("x_h1res", [16, 128, 1024], F32).ap()
    bh1res = [Buf() for _ in range(16)]
    xs_t = [nc.dram_tensor(f"x_xs{k}", [2048, 128], F32) for k in range(4)]
    xd_t = [nc.dram_tensor(f"x_xd{k}", [8192, 128], F32) for k in range(4)]
    ys_t = [nc.dram_tensor(f"x_ys{k}", [2048, 128], F32) for k in range(4)]
    yd_t = [nc.dram_tensor(f"x_yd{k}", [8192, 128], F32) for k in range(4)]
    xsrc = [t.ap().bitcast(BF16).rearrange("(t a) b -> t (a b)", a=4) for t in xs_t]
    xdst = [t.ap().bitcast(BF16).rearrange("(t a) b -> t (a b)", a=4) for t in xd_t]
    ysrc = [t.ap().bitcast(BF16).rearrange("(c a) b -> c (a b)", a=32) for t in ys_t]
    ydst = [t.ap().rearrange("(c a) b -> c (a b)", a=32) for t in yd_t]
    bxsrc = [Buf() for _ in range(4)]; bxdst = [Buf() for _ in range(4)]
    bysrc = [Buf() for _ in range(4)]; bydst = [Buf() for _ in range(4)]
    build_l1(c=c, pre="a_", link={"h1res": h1res, "bh1res": bh1res, "xsrc": xsrc, "bxsrc": bxsrc})
    for k in range(4):
        S.coll("AllGather", G4, xs_t[k], xd_t[k], r=[bxsrc[k]], w=[bxdst[k]])
    build_l2(c=c, pre="b_", link={"xdst": xdst, "bxdst": bxdst, "ysrc": ysrc, "bysrc": bysrc})
    for k in range(4):
        S.coll("AllGather", G4, ys_t[k], yd_t[k], r=[bysrc[k]], w=[bydst[k]])
    build_l3(c=c, pre="c_", link={"h1res": h1res, "bh1res": bh1res, "ydst": ydst, "bydst": bydst})
    S.emit()
    c.st.close()
    return nc


def fused_inputs(inp):
    m1 = l1_inputs(inp)
    m2 = l2_inputs(inp, None)
    m3 = l3_inputs(inp, None, None)
    maps = []
    for core in range(NCORES):
        m = {}
        for pre, mm_ in (("a_", m1[core]), ("b_", m2[core]), ("c_", m3[core])):
            for k, v in mm_.items():
                m[pre + k] = v
        maps.append(m)
    return maps


_CACHE = {}


def kernel(**inputs):
    inp = {k: np.asarray(v) for k, v in inputs.items()}
    if "fused" not in _CACHE:
        _CACHE["fused"] = build_fused()
    res = run_bass_kernel_spmd(_CACHE["fused"], fused_inputs(inp), core_ids=list(range(NCORES)))
    out = np.stack([np.asarray(r["c_out"]).reshape(2048, 1024) for r in res.results]).reshape(2, 8192, 1024)
    return out.astype(np.float32)
```

```python
import math
import numpy as np
from contextlib import ExitStack
import concourse.bass as bass
import concourse.mybir as mybir
from concourse.bass_utils import run_bass_kernel_spmd

F32 = mybir.dt.float32
BF16 = mybir.dt.bfloat16
AF = mybir.ActivationFunctionType
ALU = mybir.AluOpType

SEM_CHUNK = 30000
NCORES = 8
ALPHA = 4.0 ** 0.25
LN_EPS = 1e-5
RMS_EPS = 1e-6
POOL_WINDOWS = (2, 4, 8, 16)


class Buf:
    __slots__ = ("name", "w", "r")

    def __init__(self, name=""):
        self.name = name
        self.w = None
        self.r = []


class Sched:
    ENG = ("pe", "act", "dve", "pool", "sp")

    def __init__(self, nc, stack):
        self.nc = nc
        self.stack = stack
        self.streams = {e: [] for e in self.ENG}
        self.seq = {e: 0 for e in self.ENG}
        self.esems = {e: [] for e in self.ENG}
        self.waited = {e: {} for e in self.ENG}
        self.dsem = {}
        self.free_d = []
        self.nsem = 0

    def _newsem(self, name):
        s = self.stack.enter_context(self.nc.semaphore(name))
        self.nsem += 1
        return s

    def _etoken(self, eng):
        n = self.seq[eng]
        ci = n // SEM_CHUNK
        while len(self.esems[eng]) <= ci:
            self.esems[eng].append(self._newsem(f"e_{eng}_{len(self.esems[eng])}"))
        self.seq[eng] = n + 1
        return (self.esems[eng][ci], n % SEM_CHUNK + 1, eng)

    def _need_wait(self, eng, tok):
        if tok is None:
            return False
        sem, val, src = tok
        if src == eng and eng == "pe":
            return False
        cur = self.waited[eng].get(id(sem), 0)
        if cur >= val:
            return False
        self.waited[eng][id(sem)] = val
        return True

    def _deps(self, eng, r, w):
        toks = []
        for b in r:
            if b.w is not None:
                toks.append(b.w)
        for b in w:
            if b.w is not None:
                toks.append(b.w)
            toks.extend(b.r)
        return [t for t in toks if self._need_wait(eng, t)]

    def _commit(self, tok, r, w):
        for b in r:
            b.r.append(tok)
            if len(b.r) > 96:
                b.r = b.r[-96:]
        for b in w:
            b.w = tok
            b.r = []

    def op(self, eng, fn, r=(), w=()):
        waits = self._deps(eng, r, w)
        tok = self._etoken(eng)
        self.streams[eng].append(("op", fn, waits, tok))
        self._commit(tok, r, w)
        return tok

    def dma(self, eng, out, in_, r=(), w=(), key=None):
        waits = self._deps(eng, r, w)
        kb = key if key is not None else (w[0] if len(w) else r[0])
        ent = self._dsem_for(kb)
        ent[1] += 16
        tok = (ent[0], ent[1], "dma")
        self.streams[eng].append(("dma", (out, in_), waits, tok))
        self._commit(tok, r, w)
        return tok

    def _dsem_for(self, kb):
        if kb not in self.dsem:
            if self.free_d:
                self.dsem[kb] = self.free_d.pop()
            else:
                self.dsem[kb] = [self._newsem(f"d{self.nsem}"), 0]
        return self.dsem[kb]

    def coll(self, kind, groups, src_t, dst_t, r=(), w=()):
        waits = self._deps("pool", r, w)
        ent = self._dsem_for(("cc", len(self.streams["pool"])))
        ent[1] += 1
        tok = (ent[0], ent[1], "dma")
        self.streams["pool"].append(("cc", (kind, groups, src_t, dst_t), waits, tok))
        self._commit(tok, r, w)
        return tok

    def wait(self, eng, tok):
        if self._need_wait(eng, tok):
            self.streams[eng].append(("wait", None, [tok], None))

    def barrier(self):
        toks = []
        for e in self.ENG:
            n = self.seq[e]
            if n > 0:
                ci = (n - 1) // SEM_CHUNK
                toks.append((self.esems[e][ci], (n - 1) % SEM_CHUNK + 1, e))
        for kb, ent in self.dsem.items():
            if ent[1] > 0:
                toks.append((ent[0], ent[1], "dma"))
        for e in self.ENG:
            for t in toks:
                if t[2] == e:
                    continue
                self.wait(e, t)
        for kb, ent in self.dsem.items():
            self.free_d.append(ent)
        self.dsem = {}

    def emit(self):
        nc = self.nc
        engmap = {"pe": "tensor", "act": "scalar", "dve": "vector", "pool": "gpsimd", "sp": "sync"}
        with nc.Block() as block:
            for e in self.ENG:
                stream = self.streams[e]

                def body(engine, stream=stream):
                    for kind, payload, waits, tok in stream:
                        for (sem, val, _src) in waits:
                            engine.wait_ge(sem, val)
                        if kind == "op":
                            payload(engine).then_inc(tok[0], 1)
                        elif kind == "dma":
                            out, in_ = payload
                            if callable(in_):
                                in_ = in_(engine)
                            engine.dma_start(out=out, in_=in_).then_inc(tok[0], 16)
                        elif kind == "cc":
                            ckind, groups, src_t, dst_t = payload
                            engine.collective_compute(ckind, ALU.bypass, replica_groups=groups, ins=[src_t.ap().opt()],
                                                      outs=[dst_t.ap().opt()]).then_inc(tok[0])
                getattr(block, engmap[e])(body)


class Arena:
    def __init__(self, t, lo, hi):
        self.t = t
        self.lo = lo
        self.hi = hi
        self.off = lo

    def alloc(self, free_shape, dt, parts=128):
        n = 1
        for s in free_shape:
            n *= s
        esz = 4 if dt == F32 else 2
        words = (n * esz + 3) // 4
        words = (words + 7) // 8 * 8
        assert self.off + words <= self.hi, f"arena overflow {self.off}+{words}>{self.hi}"
        ap = self.t[0:parts, self.off:self.off + words]
        self.off += words
        if dt != F32:
            ap = ap.bitcast(dt)
        ap = ap[:, 0:n]
        if len(free_shape) == 2:
            ap = ap.rearrange("p (a b) -> p a b", a=free_shape[0])
        elif len(free_shape) == 3:
            ap = ap.rearrange("p (a b c) -> p a b c", a=free_shape[0], b=free_shape[1])
        return ap

    def mark(self):
        return self.off

    def reset(self, m):
        self.off = m


class K:
    def __init__(self, S, identb):
        self.S = S
        self.identb = identb
        self.bident = Buf("ident")

    def mm(self, out, lhsT, rhs, start, stop, r, w):
        return self.S.op("pe", lambda e: e.matmul(out=out, lhsT=lhsT, rhs=rhs, start=start, stop=stop), r=r, w=w)

    def tr(self, out, in_, r, w):
        n = in_.shape[0]
        idn = self.identb[0:n, 0:n]
        return self.S.op("pe", lambda e: e.transpose(out=out, in_=in_, identity=idn), r=list(r) + [self.bident], w=w)

    def act(self, out, in_, func, r, w, bias=None, scale=None, accum=None):
        kw = {}
        if bias is not None:
            kw["bias"] = bias
        if scale is not None:
            kw["scale"] = scale
        if accum is not None:
            kw["accum_out"] = accum
        return self.S.op("act", lambda e: e.activation(out=out, in_=in_, func=func, **kw), r=r, w=w)

    def tt(self, eng, out, in0, in1, op, r, w):
        return self.S.op(eng, lambda e: e.tensor_tensor(out=out, in0=in0, in1=in1, op=op), r=r, w=w)

    def ts(self, eng, out, in0, s1, s2, op0, op1, r, w):
        if op1 is None:
            return self.S.op(eng, lambda e: e.tensor_scalar(out=out, in0=in0, scalar1=s1, scalar2=None, op0=op0), r=r, w=w)
        return self.S.op(eng, lambda e: e.tensor_scalar(out=out, in0=in0, scalar1=s1, scalar2=s2, op0=op0, op1=op1), r=r, w=w)

    def stt(self, eng, out, in0, scalar, in1, op0, op1, r, w):
        return self.S.op(eng, lambda e: e.scalar_tensor_tensor(out=out, in0=in0, scalar=scalar, in1=in1, op0=op0, op1=op1), r=r, w=w)

    def cp(self, eng, out, in_, r, w):
        if eng == "act":
            return self.S.op("act", lambda e: e.activation(out=out, in_=in_, func=AF.Copy), r=r, w=w)
        return self.S.op(eng, lambda e: e.tensor_copy(out=out, in_=in_), r=r, w=w)

    def memset(self, eng, out, val, w):
        return self.S.op(eng, lambda e: e.memset(out, val), r=(), w=w)

    def recip(self, out, in_, r, w):
        return self.S.op("dve", lambda e: e.reciprocal(out=out, in_=in_), r=r, w=w)

    def bn_stats(self, out, in_, r, w):
        return self.S.op("dve", lambda e: e.bn_stats(out=out, in_=in_), r=r, w=w)

    def bn_aggr(self, out, in_, r, w):
        return self.S.op("dve", lambda e: e.bn_aggr(out=out, in_=in_), r=r, w=w)


def bcast_mid(ap2, n):
    return ap2.unsqueeze(2).to_broadcast([ap2.shape[0], ap2.shape[1], n])


class Ctx:
    pass


def new_prog():
    nc = bass.Bass("TRN2", target_bir_lowering=False)
    st = ExitStack()
    S = Sched(nc, st)
    arena_t = st.enter_context(nc.sbuf_tensor("arena", [128, 52000], F32))
    psum_t = st.enter_context(nc.psum_tensor("psum", [128, 4096], F32))
    c = Ctx()
    c.nc, c.st, c.S, c.arena_t, c.psum_t = nc, st, S, arena_t, psum_t
    c.A = Arena(arena_t, 0, 52000)
    c.dyn = {}
    return c


def psbank(c, b, n=512, dt=F32, parts=128, nb=1):
    ap = c.psum_t[0:parts, b * 512:(b + nb) * 512]
    if dt != F32:
        ap = ap.bitcast(dt)
    return ap[:, 0:n]


def ln_tile(c, kk, ps_y, bps, res, bres, gate_b, lng_b, lnb_b, bconst, tmp, btmp, out, bout, small, bsmall):
    S = c.S
    for hh in range(2):
        kk.tt("dve", tmp[:, hh * 512:(hh + 1) * 512], ps_y[:, hh * 512:(hh + 1) * 512], gate_b[:, hh * 512:(hh + 1) * 512],
              ALU.mult, r=[bps, bconst], w=[btmp])
    kk.stt("dve", tmp, res, ALPHA, tmp, ALU.mult, ALU.add, r=[bres, btmp], w=[btmp])
    st6 = small[:, 0:12].rearrange("p (a b) -> p a b", a=2)
    for h in range(2):
        kk.bn_stats(st6[:, h, :], tmp[:, h * 512:(h + 1) * 512], r=[btmp], w=[bsmall])
    kk.bn_aggr(small[:, 12:14], st6, r=[bsmall], w=[bsmall])
    kk.act(small[:, 14:15], small[:, 13:14], AF.Sqrt, r=[bsmall], w=[bsmall], bias=c.eps_ln, scale=1.0)
    kk.recip(small[:, 15:16], small[:, 14:15], r=[bsmall], w=[bsmall])
    kk.ts("dve", small[:, 14:15], small[:, 12:13], small[:, 15:16], -1.0, ALU.mult, ALU.mult, r=[bsmall], w=[bsmall])
    kk.act(tmp, tmp, AF.Identity, r=[btmp, bsmall], w=[btmp], bias=small[:, 14:15], scale=small[:, 15:16])
    kk.tt("dve", tmp, tmp, lng_b, ALU.mult, r=[btmp, bconst], w=[btmp])
    kk.tt("pool", out, tmp, lnb_b, ALU.add, r=[btmp, bconst], w=[bout])


def build_l1(dbg=False, stop_after=None, c=None, pre="", link=None):
    own = c is None
    if own:
        c = new_prog()
    nc, S, A = c.nc, c.S, c.A
    A.reset(0)

    def D(name, shape, dt=F32, kind="ExternalInput"):
        return nc.dram_tensor(pre + name, shape, dt, kind=kind).ap()

    xkv = D("xkv", [66, 128, 1024]); xhalo = D("xhalo", [2, 128, 1024]); ropet = D("ropet", [66, 128, 32]); ropeq = D("ropeq", [16, 128, 512])
    cs = D("cs", [128, 16])
    adaw = D("adaw", [1024, 6144]); adabcol = D("adabcol", [128, 32]); adabrow = D("adabrow", [1, 6144])
    lng = D("lng", [2, 1024]); lnb = D("lnb", [2, 1024])
    win = D("win", [1024, 1056]); poolw = D("poolw", [4, 128, 128]); pscale = D("pscale", [128, 4])
    qnorm = D("qnorm", [128, 2]); kvnorm = D("kvnorm", [128, 2])
    qup = D("qup", [256, 768]); kvupk = D("kvupk", [256, 512]); kvupv = D("kvupv", [256, 512])
    wout = D("wout", [1024, 1024]); wg = D("wg", [1024, 2816]); wu = D("wu", [1024, 2816]); wd = D("wd", [2816, 1024])
    bands = D("bands", [7, 4, 128, 128]); ident = D("ident", [128, 128])
    h1 = D("h1", [16, 128, 1024], kind="ExternalOutput") if link is None else None
    hscr = D("hscr", [16, 128, 1024], kind="Internal")
    dbgo = {}
    if dbg:
        dbgo["d_colmod"] = D("d_colmod", [128, 64], kind="ExternalOutput")
        dbgo["d_g1"] = D("d_g1", [128, 1024], kind="ExternalOutput")
        dbgo["d_kvnT"] = D("d_kvnT", [128, 2 * 8448], BF16, kind="ExternalOutput")
        dbgo["d_KT"] = D("d_KT", [96, 8448], BF16, kind="ExternalOutput")
        dbgo["d_qT"] = D("d_qT", [96, 8 * 2048], BF16, kind="ExternalOutput")
        dbgo["d_OnT"] = D("d_OnT", [64, 8 * 2048], BF16, kind="ExternalOutput")
        dbgo["d_h0a"] = D("d_h0a", [16, 128, 1024], kind="ExternalOutput")

    identb = A.alloc([128], BF16)
    kk = K(S, identb)
    ones_f = A.alloc([128], F32)
    colmod = A.alloc([32, 2], F32)
    g1b = A.alloc([1024], F32); g2b = A.alloc([1024], F32)
    lngb = A.alloc([2, 1024], F32); lnbb = A.alloc([2, 1024], F32)
    epsc = A.alloc([4], F32)
    c.eps_ln = epsc[:, 0:1]
    eps_rms = epsc[:, 1:2]
    bconst = Buf("const")
    S.dma("pool", identb, ident[:, :], w=[kk.bident])
    kk.memset("dve", ones_f, 1.0, w=[bconst])
    kk.memset("dve", epsc[:, 0:1], LN_EPS, w=[bconst])
    kk.memset("dve", epsc[:, 1:2], RMS_EPS, w=[bconst])
    for i in range(2):
        S.dma("sp", lngb[:, i, :], lng[i, :].partition_broadcast(128), w=[bconst], key=f"c{i}a")
        S.dma("sp", lnbb[:, i, :], lnb[i, :].partition_broadcast(128), w=[bconst], key=f"c{i}b")
    mP = A.mark()

    s2 = A.alloc([8, 2], F32); bs2 = Buf()
    s_bc = A.alloc([8, 128], F32); bsbc = Buf()
    wblk = [A.alloc([8, 512], F32) for _ in range(2)]; bwblk = [Buf(), Buf()]
    brow = [A.alloc([1024], F32) for _ in range(2)]; bbrow = [Buf(), Buf()]
    abc = A.alloc([32], F32); babc = Buf()
    S.dma("sp", s2.rearrange("p a b -> p (a b)"), cs[:, :], w=[bs2])
    S.dma("sp", abc, adabcol[:, :], w=[babc])
    S.dma("sp", brow[0], adabrow[0, 2048:3072].partition_broadcast(128), w=[bbrow[0]])
    S.dma("sp", brow[1], adabrow[0, 5120:6144].partition_broadcast(128), w=[bbrow[1]])
    kk.act(s2, s2, AF.Silu, r=[bs2], w=[bs2])
    kk.cp("dve", s_bc, bcast_mid(s2[:, :, 0], 128), r=[bs2], w=[bsbc])
    ps_col = psbank(c, 0, 64).rearrange("p (a b) -> p a b", a=32); bpscol = Buf()
    ps_row = [psbank(c, 1), psbank(c, 2)]; bpsrow = [Buf(), Buf()]
    adaw_v = adaw.rearrange("(kc p) n -> p kc n", p=128)
    colidx = {0: 0, 1: 1, 3: 2, 4: 3}
    gdst = {2: (g1b, brow[0], bbrow[0]), 5: (g2b, brow[1], bbrow[1])}
    for bi in range(12):
        v, half = bi // 2, bi % 2
        wb, bwb = wblk[bi % 2], bwblk[bi % 2]
        S.dma("sp", wb, adaw_v[:, :, bi * 512:(bi + 1) * 512], w=[bwb])
        if v in colidx:
            for fcl in range(4):
                slot = colidx[v] * 8 + half * 4 + fcl
                for kc in range(8):
                    kk.mm(ps_col[:, slot, :], wb[:, kc, fcl * 128:(fcl + 1) * 128], s2[:, kc, :], kc == 0, kc == 7,
                          r=[bwb, bs2], w=[bpscol])
        else:
            pr, bpr = ps_row[half], bpsrow[half]
            for kc in range(8):
                kk.mm(pr, s_bc[:, kc, :], wb[:, kc, :], kc == 0, kc == 7, r=[bwb, bsbc], w=[bpr])
            gt, brt, bbrt = gdst[v]
            kk.tt("dve", gt[:, half * 512:(half + 1) * 512], pr, brt[:, half * 512:(half + 1) * 512], ALU.add,
                  r=[bpr, bbrt], w=[bconst])
    kk.tt("dve", colmod, ps_col, bcast_mid(abc, 2), ALU.add, r=[bpscol, babc], w=[bconst])
    for ci in (1, 3):
        kk.ts("dve", colmod[:, ci * 8:(ci + 1) * 8, :], colmod[:, ci * 8:(ci + 1) * 8, :], 1.0, None, ALU.add, None,
              r=[bconst], w=[bconst])
    if dbg:
        S.dma("sp", dbgo["d_colmod"][:, :], colmod.rearrange("p a b -> p (a b)"), r=[bconst], key="dbg")
        S.dma("sp", dbgo["d_g1"][:, :], g1b, r=[bconst], key="dbg")
    S.barrier()
    if stop_after == "A":
        S.emit(); c.st.close(); return nc
    A.reset(mP)

    upool = A.alloc([18, 512], BF16); bup = [Buf() for _ in range(18)]
    mM = A.mark()
    kvnT = A.alloc([2, 8448], BF16); bkvnT = Buf()
    KT = [A.alloc([8448], BF16, parts=96) for _ in range(2)]; bKT = [Buf(), Buf()]
    mX = A.mark()
    qT = A.alloc([8, 2048], BF16, parts=96); bqT = Buf()
    mD = A.mark()
    qnT = A.alloc([2, 2048], BF16); bqnT = Buf()
    mX1 = A.mark()
    winb = A.alloc([8, 1056], BF16); bwin = Buf()
    xin = [A.alloc([1024], F32) for _ in range(3)]; bxin = [Buf() for _ in range(3)]
    xbf = [A.alloc([1024], BF16) for _ in range(2)]; bxbf = [Buf(), Buf()]
    tmpT = [A.alloc([8, 128], F32) for _ in range(2)]; btmpT = [Buf(), Buf()]
    uT = [A.alloc([8, 128], BF16) for _ in range(2)]; buT = [Buf(), Buf()]
    rt = [A.alloc([32], F32) for _ in range(2)]; brt_ = [Buf(), Buf()]
    kvrow = [A.alloc([352], BF16) for _ in range(2)]; bkvrow = [Buf(), Buf()]
    ropA = [A.alloc([32], F32) for _ in range(2)]; ropB = [A.alloc([32], F32) for _ in range(2)]; brop = [Buf(), Buf()]
    junk = A.alloc([256], F32); bjunk = Buf()
    sm = [A.alloc([8], F32) for _ in range(2)]; bsm = [Buf(), Buf()]
    qn = [A.alloc([256], BF16) for _ in range(2)]; bqn = [Buf(), Buf()]
    S.dma("pool", winb, win.rearrange("(kc p) n -> p kc n", p=128), w=[bwin])
    for i in range(2):
        kk.memset("pool", kvrow[i], 0.0, w=[bkvrow[i]])
    psT = [psbank(c, 0, 1024, BF16).rearrange("p (a b) -> p a b", a=8),
           psbank(c, 1, 1024, BF16).rearrange("p (a b) -> p a b", a=8)]
    bpsT = [Buf(), Buf()]
    ps_kv = [psbank(c, 2, 288), psbank(c, 3, 288)]; bpskv = [Buf(), Buf()]
    ps_p = psbank(c, 4); bpsp = Buf()
    ps_q = psbank(c, 5, 256); bpsq = Buf()
    psT2 = psbank(c, 6, 640, BF16).rearrange("p (a b) -> p a b", a=5); bpsT2 = Buf()
    psT3 = psbank(c, 7, 256, BF16).rearrange("p (a b) -> p a b", a=2); bpsT3 = Buf()

    tiles = [("halo", 0), ("halo", 1)] + [("kv", i) for i in range(66)]
    NT = len(tiles)

    def stageA(n):
        kind, i = tiles[n]
        src_ap = xhalo[i] if kind == "halo" else xkv[i]
        nmod = 1 if (kind == "kv" and i < 2) else 0
        xi, bxi = xin[n % 3], bxin[n % 3]
        S.dma("sp", xi, src_ap, w=[bxi])
        xb_, bxb_ = xbf[n % 2], bxbf[n % 2]
        kk.cp("act", xb_, xi, r=[bxi], w=[bxb_])
        pT, bpT = psT[n % 2], bpsT[n % 2]
        for kc in range(8):
            kk.tr(pT[:, kc, :], xb_[:, kc * 128:(kc + 1) * 128], r=[bxb_], w=[bpT])
        tT, btT = tmpT[n % 2], btmpT[n % 2]
        u, bu = uT[n % 2], buT[n % 2]
        kk.tt("dve", tT, pT, bcast_mid(colmod[:, 8:16, nmod], 128), ALU.mult, r=[bpT, bconst], w=[btT])
        kk.tt("dve", u, tT, bcast_mid(colmod[:, 0:8, nmod], 128), ALU.add, r=[btT, bconst], w=[bu])

    def stageB(n):
        kind, i = tiles[n]
        u, bu = uT[n % 2], buT[n % 2]
        if kind == "halo":
            for kc in range(8):
                kk.mm(ps_p, u[:, kc, :], winb[:, kc, 0:512], kc == 0, kc == 7, r=[bu, bwin], w=[bpsp])
            ui = 0 if i == 0 else 17
            kk.cp("act", upool[:, ui, :], ps_p, r=[bpsp], w=[bup[ui]])
            return
        is_own = 2 <= i < 18
        il = i - 2
        r_, br_ = rt[n % 2], brt_[n % 2]
        S.dma("sp", r_, ropet[i], w=[br_])
        pkv, bpkv = ps_kv[n % 2], bpskv[n % 2]
        for kc in range(8):
            kk.mm(pkv, u[:, kc, :], winb[:, kc, 768:1056], kc == 0, kc == 7, r=[bu, bwin], w=[bpkv])
        if is_own:
            for kc in range(8):
                kk.mm(ps_p, u[:, kc, :], winb[:, kc, 0:512], kc == 0, kc == 7, r=[bu, bwin], w=[bpsp])
            for kc in range(8):
                kk.mm(ps_q, u[:, kc, :], winb[:, kc, 512:768], kc == 0, kc == 7, r=[bu, bwin], w=[bpsq])
        s_, bs_ = sm[n % 2], bsm[n % 2]
        row, brow_ = kvrow[n % 2], bkvrow[n % 2]
        kk.act(junk, pkv[:, 0:256], AF.Square, r=[bpkv], w=[bjunk, bs_], accum=s_[:, 0:1])
        kk.act(s_[:, 1:2], s_[:, 0:1], AF.Sqrt, r=[bs_], w=[bs_], bias=eps_rms, scale=1.0 / 256)
        kk.recip(s_[:, 2:3], s_[:, 1:2], r=[bs_], w=[bs_])
        kk.ts("dve", row[:, 0:256], pkv[:, 0:256], s_[:, 2:3], None, ALU.mult, None, r=[bpkv, bs_], w=[brow_])
        xr = pkv[:, 256:288].rearrange("p (a h e) -> p a h e", a=2, h=2)
        cosb = r_[:, 0:16].rearrange("p (a e) -> p a e", a=2).unsqueeze(2).to_broadcast([128, 2, 2, 8])
        sinb = r_[:, 16:32].rearrange("p (a e) -> p a e", a=2).unsqueeze(2).to_broadcast([128, 2, 2, 8])
        rA = ropA[n % 2].rearrange("p (a h e) -> p a h e", a=2, h=2)
        rB = ropB[n % 2].rearrange("p (a h e) -> p a h e", a=2, h=2)
        brp = brop[n % 2]
        kk.tt("dve", rA, xr, cosb, ALU.mult, r=[bpkv, br_], w=[brp])
        kk.tt("dve", rB, xr, sinb, ALU.mult, r=[bpkv, br_], w=[brp])
        ro = row[:, 320:352].rearrange("p (a h e) -> p a h e", a=2, h=2)
        kk.tt("pool", ro[:, :, 0, :], rA[:, :, 0, :], rB[:, :, 1, :], ALU.subtract, r=[brp], w=[brow_])
        kk.tt("pool", ro[:, :, 1, :], rA[:, :, 1, :], rB[:, :, 0, :], ALU.add, r=[brp], w=[brow_])
        if is_own:
            kk.cp("act", upool[:, il + 1, :], ps_p, r=[bpsp], w=[bup[il + 1]])
            qn_, bqn_ = qn[n % 2], bqn[n % 2]
            kk.act(junk, ps_q, AF.Square, r=[bpsq], w=[bjunk, bs_], accum=s_[:, 4:5])
            kk.act(s_[:, 5:6], s_[:, 4:5], AF.Sqrt, r=[bs_], w=[bs_], bias=eps_rms, scale=1.0 / 256)
            kk.recip(s_[:, 6:7], s_[:, 5:6], r=[bs_], w=[bs_])
            kk.ts("dve", qn_, ps_q, s_[:, 6:7], 96.0 ** -0.5, ALU.mult, ALU.mult, r=[bpsq, bs_], w=[bqn_])

    def stageC(n):
        kind, i = tiles[n]
        if kind == "halo":
            return
        is_own = 2 <= i < 18
        il = i - 2
        row, brow_ = kvrow[n % 2], bkvrow[n % 2]
        kk.tr(psT2[:, 0, :], row[:, 0:128], r=[brow_], w=[bpsT2])
        kk.tr(psT2[:, 1, :], row[:, 128:256], r=[brow_], w=[bpsT2])
        kk.tr(psT2[0:96, 2, :], row[:, 256:352], r=[brow_], w=[bpsT2])
        kk.cp("act", kvnT[:, :, i * 128:(i + 1) * 128], psT2[:, 0:2, :], r=[bpsT2], w=[bkvnT])
        kk.cp("act", KT[0][64:96, i * 128:(i + 1) * 128], psT2[64:96, 2, :], r=[bpsT2], w=[bKT[0]])
        kk.cp("act", KT[1][64:96, i * 128:(i + 1) * 128], psT2[64:96, 2, :], r=[bpsT2], w=[bKT[1]])
        if is_own:
            qn_, bqn_ = qn[n % 2], bqn[n % 2]
            kk.tr(psT3[:, 0, :], qn_[:, 0:128], r=[bqn_], w=[bpsT3])
            kk.tr(psT3[:, 1, :], qn_[:, 128:256], r=[bqn_], w=[bpsT3])
            kk.cp("dve", qnT[:, :, il * 128:(il + 1) * 128], psT3, r=[bpsT3], w=[bqnT])

    SKB, SKC = 1, 2
    for s_i in range(NT + SKC):
        if s_i < NT:
            stageA(s_i)
        if 0 <= s_i - SKB < NT:
            stageB(s_i - SKB)
        if 0 <= s_i - SKC < NT:
            stageC(s_i - SKC)
    if dbg:
        S.dma("sp", dbgo["d_kvnT"][:, :], kvnT.rearrange("p a b -> p (a b)"), r=[bkvnT], key="dbg")
    S.barrier()
    if stop_after == "B":
        S.emit(); c.st.close(); return nc
    A.reset(mX1)

    qupb = A.alloc([2, 768], BF16); bqup = Buf()
    qnc = A.alloc([2], F32); bqnc = Buf()
    qrow = [A.alloc([8, 96], BF16) for _ in range(2)]; bqrow = [Buf(), Buf()]
    rtq = [A.alloc([512], F32) for _ in range(2)]; brtq = [Buf(), Buf()]
    xs = [A.alloc([8, 32], F32) for _ in range(2)]; bxs = [Buf(), Buf()]
    qA = [A.alloc([256], F32) for _ in range(2)]; qB = [A.alloc([256], F32) for _ in range(2)]; bqAB = [Buf(), Buf()]
    qo = [A.alloc([8, 32], BF16) for _ in range(2)]; bqo = [Buf(), Buf()]
    S.dma("pool", qupb, qup.rearrange("(kc p) n -> p kc n", p=128), w=[bqup])
    S.dma("sp", qnc, qnorm[:, :], w=[bqnc])
    for kc in range(2):
        kk.ts("dve", qupb[:, kc, :], qupb[:, kc, :], qnc[:, kc:kc + 1], None, ALU.mult, None, r=[bqup, bqnc], w=[bqup])
    ps_qa = [psbank(c, 0, 1024, nb=2), psbank(c, 2, 1024, nb=2)]; bpsqa = [Buf(), Buf()]
    ps_qt = [psbank(c, 4, 1024, BF16, parts=96).rearrange("p (a b) -> p a b", a=8),
             psbank(c, 5, 1024, BF16, parts=96).rearrange("p (a b) -> p a b", a=8)]
    bpsqt = [Buf(), Buf()]
    for il in range(16):
        p = il % 2
        S.dma("sp", rtq[p], ropeq[il], w=[brtq[p]])
        for (h0, nh) in ((0, 5), (5, 3)):
            pq = ps_qa[p][:, (0 if h0 == 0 else 512):(0 if h0 == 0 else 512) + nh * 96]
            for kc in range(2):
                kk.mm(pq, qnT[:, kc, il * 128:(il + 1) * 128], qupb[:, kc, h0 * 96:(h0 + nh) * 96], kc == 0, kc == 1,
                      r=[bqnT, bqup], w=[bpsqa[p]])
            qv = pq.rearrange("p (h d) -> p h d", h=nh)
            kk.cp("act", qrow[p][:, h0:h0 + nh, 0:64], qv[:, :, 0:64], r=[bpsqa[p]], w=[bqrow[p]])
            kk.cp("act", xs[p][:, h0:h0 + nh, :], qv[:, :, 64:96], r=[bpsqa[p]], w=[bxs[p]])
        xs2 = xs[p].rearrange("p h d -> p (h d)")
        kk.tt("dve", qA[p], xs2, rtq[p][:, 0:256], ALU.mult, r=[bxs[p], brtq[p]], w=[bqAB[p]])
        kk.tt("dve", qB[p], xs2, rtq[p][:, 256:512], ALU.mult, r=[bxs[p], brtq[p]], w=[bqAB[p]])
        rA = qA[p].rearrange("p (m g e) -> p m g e", g=2, e=8)
        rB = qB[p].rearrange("p (m g e) -> p m g e", g=2, e=8)
        ro = qo[p].rearrange("p h (a g e) -> p (h a) g e", a=2, g=2)
        kk.tt("pool", ro[:, :, 0, :], rA[:, :, 0, :], rB[:, :, 1, :], ALU.subtract, r=[bqAB[p]], w=[bqo[p]])
        kk.tt("pool", ro[:, :, 1, :], rA[:, :, 1, :], rB[:, :, 0, :], ALU.add, r=[bqAB[p]], w=[bqo[p]])
        kk.cp("pool", qrow[p][:, :, 64:96], qo[p], r=[bqo[p]], w=[bqrow[p]])
        for h in range(8):
            kk.tr(ps_qt[p][:, h, :], qrow[p][:, h, :], r=[bqrow[p]], w=[bpsqt[p]])
        kk.cp("dve" if il % 2 else "act", qT[:, :, il * 128:(il + 1) * 128], ps_qt[p], r=[bpsqt[p]], w=[bqT])
    if dbg:
        S.dma("sp", dbgo["d_qT"][:, :], qT.rearrange("p a b -> p (a b)"), r=[bqT], key="dbg")
    S.barrier()
    if stop_after == "C":
        S.emit(); c.st.close(); return nc
    A.reset(mD)

    OnT = A.alloc([8, 2048], BF16, parts=64); bOnT = Buf()
    mE = A.mark()
    kvkb = A.alloc([2, 512], BF16); kvvb = A.alloc([2, 512], BF16); bkvw = Buf()
    kvnc = A.alloc([2], F32); bkvnc = Buf()
    Vb = [A.alloc([66, 65], BF16) for _ in range(2)]; bVb = [Buf(), Buf()]
    PT = [A.alloc([512], BF16) for _ in range(4)]; bPT = [Buf() for _ in range(4)]
    rrow = A.alloc([512], F32, parts=65); brrow = Buf()
    osb = A.alloc([512], F32, parts=64); bosb = Buf()
    S.dma("pool", kvkb, kvupk.rearrange("(kc p) n -> p kc n", p=128), w=[bkvw], key="kvk")
    S.dma("pool", kvvb, kvupv.rearrange("(kc p) n -> p kc n", p=128), w=[bkvw], key="kvv")
    S.dma("sp", kvnc, kvnorm[:, :], w=[bkvnc])
    for kc in range(2):
        kk.ts("dve", kvkb[:, kc, :], kvkb[:, kc, :], kvnc[:, kc:kc + 1], None, ALU.mult, None, r=[bkvw, bkvnc], w=[bkvw])
        kk.ts("dve", kvvb[:, kc, :], kvvb[:, kc, :], kvnc[:, kc:kc + 1], None, ALU.mult, None, r=[bkvw, bkvnc], w=[bkvw])
    for i in range(2):
        kk.memset("pool", Vb[i][:, :, 64:65], 1.0, w=[bVb[i]])
    ps_s = [psbank(c, b) for b in (0, 1, 2, 7)]; bpss = [Buf() for _ in range(4)]
    ps_o = [psbank(c, 3, parts=65), psbank(c, 4, parts=65)]; bpso = [Buf(), Buf()]
    ps_k = [psbank(c, 5), psbank(c, 6)]; bpsk = [Buf(), Buf()]
    ps_b = psbank(c, 5, parts=64); bpsb = bpsk[0]
    nkb = 66
    ei = 0
    def kv_for_head(h):
        KTb, bKTb = KT[h % 2], bKT[h % 2]
        V, bV = Vb[h % 2], bVb[h % 2]
        for ch in range(17):
            c0 = ch * 512
            c1 = min(c0 + 512, 8448)
            pk, bpk = ps_k[ch % 2], bpsk[ch % 2]
            for kc in range(2):
                kk.mm(pk[0:64, 0:c1 - c0], kvkb[:, kc, h * 64:(h + 1) * 64], kvnT[:, kc, c0:c1], kc == 0, kc == 1,
                      r=[bkvw, bkvnT], w=[bpk])
            kk.cp("dve", KTb[0:64, c0:c1], pk[0:64, 0:c1 - c0], r=[bpk], w=[bKTb])
        for g0 in range(0, 66, 8):
            g1 = min(g0 + 8, 66)
            pk, bpk = ps_k[(g0 // 8 + 1) % 2], bpsk[(g0 // 8 + 1) % 2]
            pv = pk.rearrange("p (a b) -> p a b", a=8)
            for t in range(g0, g1):
                for kc in range(2):
                    kk.mm(pv[:, t - g0, :], kvnT[:, kc, t * 128:(t + 1) * 128], kvvb[:, kc, h * 64:(h + 1) * 64],
                          kc == 0, kc == 1, r=[bkvnT, bkvw], w=[bpk])
            kk.cp("dve", V[:, g0:g1, 0:64], pv[:, 0:g1 - g0, :], r=[bpk], w=[bV])

    kv_for_head(0)
    for h in range(8):
        KTb, bKTb = KT[h % 2], bKT[h % 2]
        V, bV = Vb[h % 2], bVb[h % 2]
        for qc in range(4):
            if qc == 1 and h + 1 < 8:
                kv_for_head(h + 1)
            po, bpo = ps_o[ei % 2], bpso[ei % 2]
            ei += 1
            LOOK = 3

            def qk_exp(kb):
                j = kb % 4
                kk.mm(ps_s[j], KTb[0:96, kb * 128:(kb + 1) * 128], qT[0:96, h, qc * 512:(qc + 1) * 512], True, True,
                      r=[bKTb, bqT], w=[bpss[j]])
                kk.act(PT[j], ps_s[j], AF.Exp, r=[bpss[j]], w=[bPT[j]])

            for kb in range(min(LOOK, nkb)):
                qk_exp(kb)
            for kb in range(nkb):
                if kb + LOOK < nkb:
                    qk_exp(kb + LOOK)
                j = kb % 4
                kk.mm(po[0:65, :], V[:, kb, 0:65], PT[j], kb == 0, kb == nkb - 1, r=[bV, bPT[j]], w=[bpo])
            kk.recip(rrow[64:65, :], po[64:65, :], r=[bpo], w=[brrow])
            kk.mm(ps_b, ones_f[64:65, 0:64], rrow[64:65, :], True, True, r=[brrow, bconst], w=[bpsb])
            kk.cp("dve", osb, po[0:64, :], r=[bpo], w=[bosb])
            kk.tt("dve", OnT[:, h, qc * 512:(qc + 1) * 512], ps_b, osb, ALU.mult, r=[bpsb, bosb], w=[bOnT])
    if dbg:
        S.dma("sp", dbgo["d_KT"][:, :], KT[1], r=[bKT[1]], key="dbg")
        S.dma("sp", dbgo["d_OnT"][:, :], OnT.rearrange("p a b -> p (a b)"), r=[bOnT], key="dbg")
    S.barrier()
    if stop_after == "D":
        S.emit(); c.st.close(); return nc

    A2 = Arena(c.arena_t, mM, mX)
    woutp = A2.alloc([4, 1024], BF16); wouta = A2.alloc([8, 1024], BF16, parts=64); bwo = Buf()
    bandb = A2.alloc([28, 128], BF16); bband = Buf()
    poolwb = A2.alloc([4, 128], BF16); bpw = Buf()
    psc = A2.alloc([4], F32); bpsc = Buf()
    dT = A2.alloc([4, 512], BF16); bdT = Buf()
    poolT = A2.alloc([4, 512], BF16); bpoolT = Buf()
    xres = [A2.alloc([1024], F32) for _ in range(2)]; bxres = [Buf(), Buf()]
    tmpE = [A2.alloc([1024], F32) for _ in range(2)]; btmpE = [Buf(), Buf()]
    outE = [A2.alloc([1024], F32) for _ in range(2)]; boutE = [Buf(), Buf()]
    smE = [A2.alloc([16], F32) for _ in range(2)]; bsmE = [Buf(), Buf()]
    S.dma("pool", woutp, wout[0:512, :].rearrange("(g p) n -> p g n", p=128), w=[bwo], key="wo1")
    S.dma("pool", wouta, wout[512:1024, :].rearrange("(h p) n -> p h n", p=64), w=[bwo], key="wo2")
    S.dma("pool", bandb, bands.rearrange("k g s t -> s (k g) t"), w=[bband])
    S.dma("pool", poolwb, poolw.rearrange("g i o -> i g o"), w=[bpw])
    S.dma("sp", psc, pscale[:, :], w=[bpsc])
    ps_d = [psbank(c, b) for b in range(4)]; bpsd = [Buf() for _ in range(4)]
    ps_y = [psbank(c, 4), psbank(c, 5)]; bpsy = [Buf(), Buf()]
    ps_out = psbank(c, 6, 1024, nb=2); bpsout = Buf()
    for qc in range(4):
        for g in range(4):
            for tl in range(4):
                il = qc * 4 + tl
                if il == 0:
                    kinds = (3, 4, 2)
                elif il == 15:
                    kinds = (0, 5, 6)
                else:
                    kinds = (0, 1, 2)
                for o in range(3):
                    kk.mm(ps_d[g][:, tl * 128:(tl + 1) * 128], upool[:, il + o, g * 128:(g + 1) * 128],
                          bandb[:, kinds[o] * 4 + g, :], o == 0, o == 2, r=[bup[il + o], bband], w=[bpsd[g]])
            kk.cp("act", dT[:, g, :], ps_d[g], r=[bpsd[g]], w=[bdT])
            kk.mm(ps_y[g % 2], poolwb[:, g, :], dT[:, g, :], True, True, r=[bpw, bdT], w=[bpsy[g % 2]])
            kk.ts("dve", poolT[:, g, :], ps_y[g % 2], psc[:, g:g + 1], None, ALU.mult, None, r=[bpsy[g % 2], bpsc], w=[bpoolT])
        for tl in range(4):
            il = qc * 4 + tl
            p = il % 2
            S.dma("sp", xres[p], xkv[il + 2], w=[bxres[p]])
            for half in range(2):
                n0 = half * 512
                for g in range(4):
                    kk.mm(ps_out[:, n0:n0 + 512], poolT[:, g, tl * 128:(tl + 1) * 128], woutp[:, g, n0:n0 + 512],
                          g == 0, False, r=[bpoolT, bwo], w=[bpsout])
                for h in range(8):
                    kk.mm(ps_out[:, n0:n0 + 512], OnT[0:64, h, il * 128:(il + 1) * 128], wouta[0:64, h, n0:n0 + 512],
                          False, h == 7, r=[bOnT, bwo], w=[bpsout])
            ln_tile(c, kk, ps_out, bpsout, xres[p], bxres[p], g1b, lngb[:, 0, :], lnbb[:, 0, :], bconst,
                    tmpE[p], btmpE[p], outE[p], boutE[p], smE[p], bsmE[p])
            S.dma("sp", hscr[il], outE[p], r=[boutE[p]], key=f"hs{p}")
            if dbg:
                S.dma("sp", dbgo["d_h0a"][il], outE[p], r=[boutE[p]], key=f"dbgh{p}")
    S.barrier()
    if stop_after == "E":
        S.emit(); c.st.close(); return nc
    A.reset(mP)

    h1T = A.alloc([8, 2048], BF16); bh1T = Buf()
    hidT = A.alloc([22, 1024], BF16); bhid = Buf()
    wdb = A.alloc([22, 1024], BF16); bwd = Buf()
    wgb = [A.alloc([8, 256], BF16) for _ in range(2)]; wub = [A.alloc([8, 256], BF16) for _ in range(2)]
    bwgu = [Buf(), Buf()]
    xinF = [A.alloc([1024], F32) for _ in range(2)]; bxinF = [Buf(), Buf()]
    xbfF = [A.alloc([1024], BF16) for _ in range(2)]; bxbfF = [Buf(), Buf()]
    tmpTF = [A.alloc([8, 128], F32) for _ in range(2)]; btmpTF = [Buf(), Buf()]
    sg = [A.alloc([512], BF16) for _ in range(2)]; bsg = [Buf(), Buf()]
    tmpF = [A.alloc([1024], F32) for _ in range(2)]; btmpF = [Buf(), Buf()]
    outF = [A.alloc([1024], F32) for _ in range(2)]; boutF = [Buf(), Buf()]
    smF = [A.alloc([16], F32) for _ in range(2)]; bsmF = [Buf(), Buf()]
    ob16, bob16 = xbfF, bxbfF
    wd_v = wd.rearrange("(c p) n -> p c n", p=128)
    for q4 in range(2):
        S.dma("pool", wdb[:, q4 * 11:(q4 + 1) * 11, :], wd_v[:, q4 * 11:(q4 + 1) * 11, :], w=[bwd], key=f"wd{q4}")
    psTF = [psbank(c, 0, 1024, BF16).rearrange("p (a b) -> p a b", a=8),
            psbank(c, 1, 1024, BF16).rearrange("p (a b) -> p a b", a=8)]
    bpsTF = [Buf(), Buf()]
    for il in range(16):
        p = il % 2
        S.dma("sp", xinF[p], hscr[il], w=[bxinF[p]])
        kk.cp("act", xbfF[p], xinF[p], r=[bxinF[p]], w=[bxbfF[p]])
        for kc in range(8):
            kk.tr(psTF[p][:, kc, :], xbfF[p][:, kc * 128:(kc + 1) * 128], r=[bxbfF[p]], w=[bpsTF[p]])
        kk.tt("dve", tmpTF[p], psTF[p], bcast_mid(colmod[:, 24:32, 0], 128), ALU.mult, r=[bpsTF[p], bconst], w=[btmpTF[p]])
        kk.tt("dve", h1T[:, :, il * 128:(il + 1) * 128], tmpTF[p], bcast_mid(colmod[:, 16:24, 0], 128), ALU.add,
              r=[btmpTF[p], bconst], w=[bh1T])
    ps_g = [psbank(c, 2), psbank(c, 3)]; bpsg = [Buf(), Buf()]
    ps_u = [psbank(c, 4), psbank(c, 5)]; bpsu = [Buf(), Buf()]
    ps_o2 = psbank(c, 6, 1024, nb=2); bpso2 = Buf()
    wg_v = wg.rearrange("(kc p) n -> p kc n", p=128)
    wu_v = wu.rearrange("(kc p) n -> p kc n", p=128)
    wi = 0
    ci = 0
    for tb in range(2):
        for fb in range(11):
            p = wi % 2
            wi += 1
            S.dma("pool", wgb[p], wg_v[:, :, fb * 256:(fb + 1) * 256], w=[bwgu[p]], key=f"wg{p}")
            S.dma("pool", wub[p], wu_v[:, :, fb * 256:(fb + 1) * 256], w=[bwgu[p]], key=f"wu{p}")
            for fc in range(2):
                ffc = fb * 2 + fc
                for ts_ in range(2):
                    q = ci % 2
                    ci += 1
                    t0 = tb * 1024 + ts_ * 512
                    for kc in range(8):
                        kk.mm(ps_g[q], wgb[p][:, kc, fc * 128:(fc + 1) * 128], h1T[:, kc, t0:t0 + 512], kc == 0, kc == 7,
                              r=[bwgu[p], bh1T], w=[bpsg[q]])
                    for kc in range(8):
                        kk.mm(ps_u[q], wub[p][:, kc, fc * 128:(fc + 1) * 128], h1T[:, kc, t0:t0 + 512], kc == 0, kc == 7,
                              r=[bwgu[p], bh1T], w=[bpsu[q]])
                    kk.act(sg[q], ps_g[q], AF.Silu, r=[bpsg[q]], w=[bsg[q]])
                    kk.tt("dve", hidT[:, ffc, ts_ * 512:(ts_ + 1) * 512], ps_u[q], sg[q], ALU.mult, r=[bpsu[q], bsg[q]], w=[bhid])
        for tl in range(8):
            il = tb * 8 + tl
            p = il % 2
            S.dma("sp", xinF[p], hscr[il], w=[bxinF[p]])
            for half in range(2):
                n0 = half * 512
                for ffc in range(22):
                    kk.mm(ps_o2[:, n0:n0 + 512], hidT[:, ffc, tl * 128:(tl + 1) * 128], wdb[:, ffc, n0:n0 + 512],
                          ffc == 0, ffc == 21, r=[bhid, bwd], w=[bpso2])
            ln_tile(c, kk, ps_o2, bpso2, xinF[p], bxinF[p], g2b, lngb[:, 1, :], lnbb[:, 1, :], bconst,
                    tmpF[p], btmpF[p], outF[p], boutF[p], smF[p], bsmF[p])
            if link is None:
                S.dma("sp", h1[il], outF[p], r=[boutF[p]], key=f"ho{p}")
            else:
                S.dma("sp", link["h1res"][il], outF[p], r=[boutF[p]], w=[link["bh1res"][il]], key=f"ho{p}")
                kk.cp("act", ob16[p], outF[p], r=[boutF[p]], w=[bob16[p]])
                S.dma("sp", link["xsrc"][il // 4][(il % 4) * 128:(il % 4 + 1) * 128, :], ob16[p], r=[bob16[p]],
                      w=[link["bxsrc"][il // 4]], key=f"hx{p}")
    S.barrier()
    if not own:
        return None
    S.emit()
    c.st.close()
    return nc


def mod_phase(c, kk, cs, adaw, adabcol, adabrow, colmod, g1b, g2b, bconst, need=(0, 1, 2, 3, 4, 5)):
    S, A = c.S, c.A
    m0 = A.mark()
    s2 = A.alloc([8, 2], F32); bs2 = Buf()
    s_bc = A.alloc([8, 128], F32); bsbc = Buf()
    wblk = [A.alloc([8, 512], F32) for _ in range(2)]; bwblk = [Buf(), Buf()]
    brow = [A.alloc([1024], F32) for _ in range(2)]; bbrow = [Buf(), Buf()]
    abc = A.alloc([32], F32); babc = Buf()
    S.dma("sp", s2.rearrange("p a b -> p (a b)"), cs[:, :], w=[bs2])
    S.dma("sp", abc, adabcol[:, :], w=[babc])
    S.dma("sp", brow[0], adabrow[0, 2048:3072].partition_broadcast(128), w=[bbrow[0]])
    S.dma("sp", brow[1], adabrow[0, 5120:6144].partition_broadcast(128), w=[bbrow[1]])
    kk.act(s2, s2, AF.Silu, r=[bs2], w=[bs2])
    kk.cp("dve", s_bc, bcast_mid(s2[:, :, 0], 128), r=[bs2], w=[bsbc])
    ps_col = psbank(c, 0, 64).rearrange("p (a b) -> p a b", a=32); bpscol = Buf()
    ps_row = [psbank(c, 1), psbank(c, 2)]; bpsrow = [Buf(), Buf()]
    adaw_v = adaw.rearrange("(kc p) n -> p kc n", p=128)
    colidx = {0: 0, 1: 1, 3: 2, 4: 3}
    gdst = {2: (g1b, brow[0], bbrow[0]), 5: (g2b, brow[1], bbrow[1])}
    for bi in range(12):
        v, half = bi // 2, bi % 2
        if v not in need or (v not in colidx and gdst[v][0] is None):
            continue
        wb, bwb = wblk[bi % 2], bwblk[bi % 2]
        S.dma("sp", wb, adaw_v[:, :, bi * 512:(bi + 1) * 512], w=[bwb])
        if v in colidx:
            for fcl in range(4):
                slot = colidx[v] * 8 + half * 4 + fcl
                for kc in range(8):
                    kk.mm(ps_col[:, slot, :], wb[:, kc, fcl * 128:(fcl + 1) * 128], s2[:, kc, :], kc == 0, kc == 7,
                          r=[bwb, bs2], w=[bpscol])
        else:
            pr, bpr = ps_row[half], bpsrow[half]
            for kc in range(8):
                kk.mm(pr, s_bc[:, kc, :], wb[:, kc, :], kc == 0, kc == 7, r=[bwb, bsbc], w=[bpr])
            gt, brt, bbrt = gdst[v]
            kk.tt("dve", gt[:, half * 512:(half + 1) * 512], pr, brt[:, half * 512:(half + 1) * 512], ALU.add,
                  r=[bpr, bbrt], w=[bconst])
    for v in need:
        if v not in colidx:
            continue
        ci = colidx[v]
        kk.tt("dve", colmod[:, ci * 8:(ci + 1) * 8, :], ps_col[:, ci * 8:(ci + 1) * 8, :], bcast_mid(abc[:, ci * 8:(ci + 1) * 8], 2),
              ALU.add, r=[bpscol, babc], w=[bconst])
        if ci in (1, 3):
            kk.ts("dve", colmod[:, ci * 8:(ci + 1) * 8, :], colmod[:, ci * 8:(ci + 1) * 8, :], 1.0, None, ALU.add, None,
                  r=[bconst], w=[bconst])
    S.barrier()
    A.reset(m0)


CG = 32


def build_l2(dbg=False, stop_after=None, c=None, pre="", link=None):
    own = c is None
    if own:
        c = new_prog()
    nc, S, A = c.nc, c.S, c.A
    A.reset(0)

    def D(name, shape, dt=F32, kind="ExternalInput"):
        return nc.dram_tensor(pre + name, shape, dt, kind=kind).ap()

    hin = D("hin", [64, 128, 1024]) if link is None else None
    cs = D("cs", [128, 16])
    adaw = D("adaw", [1024, 6144]); adabcol = D("adabcol", [128, 32]); adabrow = D("adabrow", [1, 6144])
    win = D("win", [1024, 768]); convw = D("convw", [128, 18]); convb = D("convb", [128, 6])
    zemb = D("zemb", [33, 8192]); fw1 = D("fw1", [33, 64]); fw2 = D("fw2", [64, 64]); fw3 = D("fw3", [64, 64])
    fout = D("fout", [64, 512]); fbs = D("fbs", [64, 4])
    skipc = D("skipc", [128, 2]); deltac = D("deltac", [128, 2]); tvals = D("tvals", [1, 8192])
    f1cat = D("f1cat", [64, 1024]); f1inv = D("f1inv", [128, 512]); gmat = D("gmat", [128, 128 * 2 * 128])
    ident = D("ident", [128, 128])
    yout = D("yout", [256, 8192], BF16, kind="ExternalOutput") if link is None else None
    pscr = D("pscr", [256, 8192], BF16, kind="Internal")
    x0scr = D("x0scr", [256, 8192], BF16, kind="Internal")
    hfscr = D("hfscr", [256, 8192], BF16, kind="Internal")
    hbscr = D("hbscr", [256, 8192], BF16, kind="Internal")
    yscr = D("yscr", [256, 8192], F32, kind="Internal")
    dbgo = {}
    if dbg:
        dbgo["d_p"] = D("d_p", [256, 8192], BF16, kind="ExternalOutput")
        dbgo["d_x0"] = D("d_x0", [256, 8192], BF16, kind="ExternalOutput")
        dbgo["d_hf"] = D("d_hf", [256, 8192], BF16, kind="ExternalOutput")
        dbgo["d_hb"] = D("d_hb", [256, 8192], BF16, kind="ExternalOutput")
        dbgo["d_y"] = D("d_y", [256, 8192], F32, kind="ExternalOutput")

    identb = A.alloc([128], BF16)
    kk = K(S, identb)
    colmod = A.alloc([32, 2], F32)
    bconst = Buf("const")
    S.dma("pool", identb, ident[:, :], w=[kk.bident])
    mP = A.mark()
    mod_phase(c, kk, cs, adaw, adabcol, adabrow, colmod, None, None, bconst, need=(0, 1))

    zT = A.alloc([6, 8194], BF16); bz = [Buf() for _ in range(6)]
    winb = A.alloc([8, 768], BF16); bwin = Buf()
    cw = A.alloc([18], F32); cb = A.alloc([6], F32); bcw = Buf()
    uTb = [A.alloc([8, 512], BF16) for _ in range(2)]; buTb = [Buf(), Buf()]
    xin = [A.alloc([1024], F32) for _ in range(3)]; bxin = [Buf() for _ in range(3)]
    xbf = [A.alloc([1024], BF16) for _ in range(2)]; bxbf = [Buf(), Buf()]
    tmpT = [A.alloc([8, 128], F32) for _ in range(2)]; btmpT = [Buf(), Buf()]
    ct = [A.alloc([2048], F32) for _ in range(3)]; bct = [Buf() for _ in range(3)]
    ob = [A.alloc([2048], BF16) for _ in range(2)]; bob = [Buf(), Buf()]
    S.dma("pool", winb, win.rearrange("(kc p) n -> p kc n", p=128), w=[bwin])
    S.dma("sp", cw, convw[:, :], w=[bcw], key="cw")
    S.dma("sp", cb, convb[:, :], w=[bcw], key="cb")
    for ch in range(6):
        kk.memset("pool", zT[:, ch, 0:1], 0.0, w=[bz[ch]])
        kk.memset("pool", zT[:, ch, 8193:8194], 0.0, w=[bz[ch]])
    psT = [psbank(c, 0, 1024, BF16).rearrange("p (a b) -> p a b", a=8),
           psbank(c, 1, 1024, BF16).rearrange("p (a b) -> p a b", a=8)]
    bpsT = [Buf(), Buf()]
    ps_z = [psbank(c, 2), psbank(c, 3), psbank(c, 4)]; bpsz = [Buf() for _ in range(3)]
    zi = 0

    def p1_prep(tb):
        u, bu = uTb[tb % 2], buTb[tb % 2]
        for tl in range(4):
            i = tb * 4 + tl
            xi, bxi = xin[i % 3], bxin[i % 3]
            if link is None:
                S.dma("sp", xi, hin[i], w=[bxi])
                kk.cp("act", xbf[i % 2], xi, r=[bxi], w=[bxbf[i % 2]])
            else:
                r_, k_, m_ = i // 16, (i % 16) // 4, i % 4
                S.dma("sp", xbf[i % 2], link["xdst"][k_][512 * r_ + 128 * m_:512 * r_ + 128 * m_ + 128, :],
                      r=[link["bxdst"][k_]], w=[bxbf[i % 2]], key=f"xb{i % 2}")
            for kc in range(8):
                kk.tr(psT[i % 2][:, kc, :], xbf[i % 2][:, kc * 128:(kc + 1) * 128], r=[bxbf[i % 2]], w=[bpsT[i % 2]])
            kk.tt("dve", tmpT[i % 2], psT[i % 2], bcast_mid(colmod[:, 8:16, 0], 128), ALU.mult, r=[bpsT[i % 2], bconst], w=[btmpT[i % 2]])
            kk.tt("dve", u[:, :, tl * 128:(tl + 1) * 128], tmpT[i % 2], bcast_mid(colmod[:, 0:8, 0], 128), ALU.add,
                  r=[btmpT[i % 2], bconst], w=[bu])

    def p1_proj(tb):
        nonlocal zi
        u, bu = uTb[tb % 2], buTb[tb % 2]
        for ch in range(6):
            pz, bpz = ps_z[zi % 3], bpsz[zi % 3]
            zi += 1
            for kc in range(8):
                kk.mm(pz, winb[:, kc, ch * 128:(ch + 1) * 128], u[:, kc, :], kc == 0, kc == 7, r=[bwin, bu], w=[bpz])
            kk.cp("act" if ch % 2 else "dve", zT[:, ch, 1 + tb * 512:1 + (tb + 1) * 512], pz, r=[bpz], w=[bz[ch]])

    p1_prep(0)
    for tb in range(16):
        if tb + 1 < 16:
            p1_prep(tb + 1)
        p1_proj(tb)
    ci = 0

    def sconv(ch, t0, n, dst, bdst, tmp, btmp):
        kk.act(tmp, zT[:, ch, 1 + t0:1 + t0 + n], AF.Identity, r=[bz[ch], bcw], w=[btmp],
               bias=cb[:, ch:ch + 1], scale=cw[:, ch * 3 + 1:ch * 3 + 2])
        kk.stt("dve", tmp, zT[:, ch, t0:t0 + n], cw[:, ch * 3:ch * 3 + 1], tmp, ALU.mult, ALU.add, r=[bz[ch], bcw, btmp], w=[btmp])
        kk.stt("dve", dst, zT[:, ch, 2 + t0:2 + t0 + n], cw[:, ch * 3 + 2:ch * 3 + 3], tmp, ALU.mult, ALU.add,
               r=[bz[ch], bcw, btmp], w=[bdst])

    oi = 0
    for q in range(2):
        for sb in range(4):
            t0 = sb * 2048
            o, bo = ob[oi % 2], bob[oi % 2]; oi += 1
            sconv(q, t0, 2048, o, bo, ct[0], bct[0])
            S.dma("sp", x0scr[q * 128:(q + 1) * 128, t0:t0 + 2048], o, r=[bo], key=f"x0s{oi % 2}")
            if dbg:
                S.dma("sp", dbgo["d_x0"][q * 128:(q + 1) * 128, t0:t0 + 2048], o, r=[bo], key=f"dbg{oi % 2}")
            sconv(2 + q, t0, 2048, ct[1], bct[1], ct[1], bct[1])
            sconv(4 + q, t0, 2048, ct[2], bct[2], ct[2], bct[2])
            o, bo = ob[oi % 2], bob[oi % 2]; oi += 1
            kk.tt("pool", o, ct[1], ct[2], ALU.mult, r=[bct[1], bct[2]], w=[bo])
            S.dma("sp", pscr[q * 128:(q + 1) * 128, t0:t0 + 2048], o, r=[bo], key=f"ps{oi % 2}")
            if dbg:
                S.dma("sp", dbgo["d_p"][q * 128:(q + 1) * 128, t0:t0 + 2048], o, r=[bo], key=f"dbg{oi % 2}")
    S.barrier()
    if stop_after == "P1":
        S.emit(); c.st.close(); return nc
    A.reset(mP)

    PI = math.pi
    ze = A.alloc([8192], F32, parts=33); bze = Buf()
    hA = A.alloc([8192], F32, parts=64); bhA = Buf()
    hB = A.alloc([8192], F32, parts=64); bhB = Buf()
    mt = A.alloc([8192], F32, parts=64); bmt = Buf()
    w1 = A.alloc([64], F32, parts=33); w2 = A.alloc([64], F32, parts=64); w3 = A.alloc([64], F32, parts=64)
    fo = A.alloc([512], F32, parts=64); fb = A.alloc([8], F32, parts=64); bfw = Buf()
    skc = A.alloc([2], F32); dlc = A.alloc([2], F32); bsk = Buf()
    tv = [A.alloc([512], F32) for _ in range(2)]; btv = [Buf(), Buf()]
    dec = [A.alloc([512], F32) for _ in range(2)]; bdec = [Buf(), Buf()]
    o32 = [A.alloc([512], F32) for _ in range(2)]; bo32 = [Buf(), Buf()]
    o16 = [A.alloc([512], BF16) for _ in range(4)]; bo16 = [Buf() for _ in range(4)]
    S.dma("sp", ze, zemb[:, :], w=[bze])
    S.dma("sp", w1, fw1[:, :], w=[bfw], key="fw1")
    S.dma("sp", w2, fw2[:, :], w=[bfw], key="fw2")
    S.dma("sp", w3, fw3[:, :], w=[bfw], key="fw3")
    S.dma("sp", fo, fout[:, :], w=[bfw], key="fo")
    S.dma("sp", fb[:, 0:4], fbs[:, :], w=[bfw], key="fbs")
    S.dma("sp", skc, skipc[:, :], w=[bsk], key="skc")
    S.dma("sp", dlc, deltac[:, :], w=[bsk], key="dlc")
    for l in range(3):
        kk.tt("dve", fb[:, 4 + l:5 + l], fb[:, l:l + 1], fb[:, 3:4], ALU.mult, r=[bfw], w=[bfw])
    kk.ts("dve", dlc, dlc, -1.0, None, ALU.mult, None, r=[bsk], w=[bsk])
    ps_f = [psbank(c, b) for b in range(4)]; bpsf = [Buf() for _ in range(4)]
    src, bsrc, K0 = ze, bze, 33
    layers = [(w1, hA, bhA), (w2, hB, bhB), (w3, hA, bhA)]
    for l, (wl, dst, bdst) in enumerate(layers):
        for blk in range(16):
            pf, bpf = ps_f[blk % 4], bpsf[blk % 4]
            kk.mm(pf[0:64, :], wl[0:K0, :], src[0:K0, blk * 512:(blk + 1) * 512], True, True, r=[bfw, bsrc], w=[bpf])
            kk.ts("dve", dst[:, blk * 512:(blk + 1) * 512], pf[0:64, :], fb[:, 3:4], fb[:, 4 + l:5 + l], ALU.mult, ALU.add,
                  r=[bpf, bfw], w=[bdst])
        for sb in range(4):
            xs_ = dst[:, sb * 2048:(sb + 1) * 2048]
            ms_ = mt[:, sb * 2048:(sb + 1) * 2048]
            for rep in range(1):
                kk.ts("dve", ms_, xs_, PI, -2 * PI, ALU.is_gt, ALU.mult, r=[bdst], w=[bmt])
                kk.tt("dve", xs_, xs_, ms_, ALU.add, r=[bdst, bmt], w=[bdst])
                kk.ts("dve", ms_, xs_, -PI, 2 * PI, ALU.is_lt, ALU.mult, r=[bdst], w=[bmt])
                kk.tt("dve", xs_, xs_, ms_, ALU.add, r=[bdst, bmt], w=[bdst])
            kk.act(xs_, xs_, AF.Sin, r=[bdst], w=[bdst])
        src, bsrc, K0 = dst, bdst, 64
    h3, bh3 = src, bsrc
    ps_o = [psbank(c, 4 + b) for b in range(4)]; bpso = [Buf() for _ in range(4)]
    oi = 0
    for blk in range(16):
        S.dma("sp", tv[blk % 2], tvals[0, blk * 512:(blk + 1) * 512].partition_broadcast(128), w=[btv[blk % 2]])
        for q in range(2):
            d_, bd_ = dec[q], bdec[q]
            kk.act(d_, tv[blk % 2], AF.Exp, r=[btv[blk % 2], bsk], w=[bd_], scale=dlc[:, q:q + 1])
            for kind in range(2):
                cc = kind * 2 + q
                po, bpo = ps_o[cc], bpso[cc]
                kk.mm(po, fo[0:64, cc * 128:(cc + 1) * 128], h3[0:64, blk * 512:(blk + 1) * 512], True, True, r=[bfw, bh3], w=[bpo])
                o3, bo3 = o32[kind], bo32[kind]
                kk.tt("dve", o3, po, d_, ALU.mult, r=[bpo, bd_], w=[bo3])
                if blk == 0:
                    if kind == 0:
                        kk.ts("dve", o3[:, 0:1], o3[:, 0:1], skc[:, q:q + 1], None, ALU.add, None, r=[bo3, bsk], w=[bo3])
                    else:
                        kk.memset("dve", o3[:, 0:1], 0.0, w=[bo3])
                o6, bo6 = o16[oi % 4], bo16[oi % 4]
                kk.cp("act", o6, o3, r=[bo3], w=[bo6])
                dstd = hfscr if kind == 0 else hbscr
                S.dma("sp", dstd[q * 128:(q + 1) * 128, blk * 512:(blk + 1) * 512], o6, r=[bo6], key=f"hs{oi % 4}")
                if dbg:
                    dd = dbgo["d_hf"] if kind == 0 else dbgo["d_hb"]
                    S.dma("sp", dd[q * 128:(q + 1) * 128, blk * 512:(blk + 1) * 512], o6, r=[bo6], key=f"dbgh{oi % 4}")
                oi += 1
    S.barrier()
    if stop_after == "P2":
        S.emit(); c.st.close(); return nc
    A.reset(mP)

    G = A.alloc([128, 2, 128], BF16); bG = Buf()
    f1c = A.alloc([1024], BF16, parts=64); f1i = A.alloc([512], BF16); bf1 = Buf()
    gv = gmat.rearrange("p (j r b) -> p j r b", j=128, r=2)
    for q4 in range(4):
        S.dma("pool", G[:, q4 * 32:(q4 + 1) * 32, :, :], gv[:, q4 * 32:(q4 + 1) * 32, :, :], w=[bG], key=f"G{q4}")
    S.dma("pool", f1c, f1cat[:, :], w=[bf1], key="f1c")
    S.dma("pool", f1i, f1inv[:, :], w=[bf1], key="f1i")
    xg = A.alloc([CG, 128], BF16, parts=64); bxg = Buf()
    hfg = A.alloc([CG, 128], BF16, parts=64); bhfg = Buf()
    hbg = A.alloc([CG, 128], BF16, parts=64); bhbg = Buf()
    A1f = A.alloc([128, CG, 2], BF16); A2f = A.alloc([128, CG, 2], BF16); bAf = Buf()
    A1b = A.alloc([128, CG, 2], BF16); A2b = A.alloc([128, CG, 2], BF16); bAb = Buf()
    Kf = A.alloc([128, CG * 2], F32); bKf = Buf()
    ysb = A2b.rearrange("p a b c -> p (a b c)")[0:64, :].bitcast(F32).rearrange("p (c n) -> p c n", c=CG); bysb = bAb
    tq = [A.alloc([256], F32) for _ in range(4)]; btq = [Buf() for _ in range(4)]
    Yt = A1b.rearrange("p a b c -> p (a b c)").rearrange("p (c r k) -> p c r k", c=CG, r=2)
    Ap1 = A2b
    ps_a = [psbank(c, b) for b in range(3)]; bpsa = [Buf() for _ in range(3)]
    ps_x = [psbank(c, 3 + b) for b in range(3)]; bpsx = [Buf() for _ in range(3)]
    ps_y = [psbank(c, 6 + b, parts=64) for b in range(2)]; bpsy = [Buf() for _ in range(2)]
    NB = 512 // (2 * CG)
    NBI = 512 // CG
    ai = 0
    xi_ = 0
    yi_ = 0

    def s1(xsrc, bxs, cl, rhs, A1, A2, bA):
        nonlocal ai
        pa, bpa = ps_a[ai % 3], bpsa[ai % 3]
        kk.mm(pa, xsrc[0:64, cl, :], rhs, True, True, r=[bxs, bf1], w=[bpa])
        e1, e2 = ("act", "dve") if ai % 2 == 0 else ("dve", "act")
        ai += 1
        kk.cp(e1, A1[:, :, cl, :], pa[:, 0:256].rearrange("p (r k) -> p k r", r=2), r=[bpa], w=[bA])
        kk.cp(e2, A2[:, :, cl, :], pa[:, 256:512].rearrange("p (r k) -> p k r", r=2), r=[bpa], w=[bA])

    for g in range(256 // CG):
        c0 = g * CG
        S.dma("sp", hfg, hfscr[c0:c0 + CG, :].rearrange("c (a b) -> a c b", b=128), w=[bhfg])
        S.dma("sp", hbg, hbscr[c0:c0 + CG, :].rearrange("c (a b) -> a c b", b=128), w=[bhbg])
        S.dma("sp", xg, pscr[c0:c0 + CG, :].rearrange("c (a b) -> a c b", b=128), w=[bxg])
        for cl in range(CG):
            s1(hfg, bhfg, cl, f1c[0:64, 0:512], A1f, A2f, bAf)
            s1(hbg, bhbg, cl, f1c[0:64, 512:1024], A1b, A2b, bAb)
        for k0 in range(0, 128, NB):
            px, bpx = ps_x[xi_ % 3], bpsx[xi_ % 3]; xi_ += 1
            pxv = px.rearrange("p (k n) -> p k n", k=NB)
            for kq in range(NB):
                k2 = k0 + kq
                kk.mm(pxv[:, kq, :], G[:, k2, 0, :], A1f[:, k2, :, :].rearrange("p c r -> p (c r)"), True, False, r=[bG, bAf], w=[bpx])
                kk.mm(pxv[:, kq, :], G[:, k2, 1, :], A2f[:, k2, :, :].rearrange("p c r -> p (c r)"), False, False, r=[bG, bAf], w=[bpx])
                kk.mm(pxv[:, kq, :], G[:, k2, 0, :], A1b[:, k2, :, :].rearrange("p c r -> p (c r)"), False, False, r=[bG, bAb], w=[bpx])
                kk.mm(pxv[:, kq, :], G[:, k2, 1, :], A2b[:, k2, :, :].rearrange("p c r -> p (c r)"), False, True, r=[bG, bAb], w=[bpx])
            kk.cp("act", Kf[:, k0:k0 + NB, :], pxv, r=[bpx], w=[bKf])
        for cl in range(CG):
            s1(xg, bxg, cl, f1c[0:64, 0:512], A1f, A2f, bAf)
        for k0 in range(0, 128, NB):
            px, bpx = ps_x[xi_ % 3], bpsx[xi_ % 3]; xi_ += 1
            pxv = px.rearrange("p (k n) -> p k n", k=NB)
            for kq in range(NB):
                k2 = k0 + kq
                kk.mm(pxv[:, kq, :], G[:, k2, 0, :], A1f[:, k2, :, :].rearrange("p c r -> p (c r)"), True, False, r=[bG, bAf], w=[bpx])
                kk.mm(pxv[:, kq, :], G[:, k2, 1, :], A2f[:, k2, :, :].rearrange("p c r -> p (c r)"), False, True, r=[bG, bAf], w=[bpx])
            Xr = px.rearrange("p (m r) -> p m r", r=2)[:, :, 0]
            Xi = px.rearrange("p (m r) -> p m r", r=2)[:, :, 1]
            Kv = Kf[:, k0:k0 + NB, :].rearrange("p k n -> p (k n)").rearrange("p (m r) -> p m r", r=2)
            kk.tt("dve", tq[0], Xr, Kv[:, :, 0], ALU.mult, r=[bpx, bKf], w=[btq[0]])
            kk.tt("dve", tq[1], Xi, Kv[:, :, 1], ALU.mult, r=[bpx, bKf], w=[btq[1]])
            kk.tt("dve", tq[2], Xr, Kv[:, :, 1], ALU.mult, r=[bpx, bKf], w=[btq[2]])
            kk.tt("dve", tq[3], Xi, Kv[:, :, 0], ALU.mult, r=[bpx, bKf], w=[btq[3]])
            t3 = [t.rearrange("p (k c) -> p k c", k=NB) for t in tq]
            kk.tt("pool", Yt[:, :, 0, k0:k0 + NB].rearrange("p c k -> p k c"), t3[0], t3[1], ALU.subtract, r=[btq[0], btq[1]], w=[bAb])
            kk.tt("pool", Yt[:, :, 1, k0:k0 + NB].rearrange("p c k -> p k c"), t3[2], t3[3], ALU.add, r=[btq[2], btq[3]], w=[bAb])
        for cl in range(CG):
            pa, bpa = ps_a[ai % 3], bpsa[ai % 3]
            kk.mm(pa[:, 0:256], Yt[:, cl, 0, :], f1i[:, 0:256], True, False, r=[bAb, bf1], w=[bpa])
            kk.mm(pa[:, 0:256], Yt[:, cl, 1, :], f1i[:, 256:512], False, True, r=[bAb, bf1], w=[bpa])
            kk.cp("act" if ai % 2 else "dve", A2f[:, :, cl, :], pa[:, 0:256].rearrange("p (r k) -> p k r", r=2), r=[bpa], w=[bAf])
            ai += 1
        for j0 in range(0, 128, NBI):
            py, bpy = ps_y[yi_ % 2], bpsy[yi_ % 2]; yi_ += 1
            pyv = py.rearrange("p (j c) -> p j c", j=NBI)
            for jq in range(NBI):
                j = j0 + jq
                kk.mm(pyv[:, jq, :], G[:, j, 0, 0:64], A2f[:, j, :, 0], True, False, r=[bG, bAf], w=[bpy])
                kk.mm(pyv[:, jq, :], G[:, j, 1, 0:64], A2f[:, j, :, 1], False, True, r=[bG, bAf], w=[bpy])
            kk.act(ysb[:, :, j0:j0 + NBI].rearrange("p c j -> p j c"), pyv, AF.Copy, r=[bpy], w=[bysb], scale=1.0 / 16384)
        S.dma("sp", yscr[c0:c0 + CG, :].rearrange("c (a b) -> a c b", b=128), ysb, r=[bysb], key="ysc")
        if dbg:
            S.dma("sp", dbgo["d_y"][c0:c0 + CG, :].rearrange("c (a b) -> a c b", b=128), ysb, r=[bysb], key="dbgy")
    S.barrier()
    if stop_after == "P3":
        S.emit(); c.st.close(); return nc
    A.reset(mP)

    yl = [A.alloc([2048], F32) for _ in range(2)]; byl = [Buf(), Buf()]
    xl = [A.alloc([2048], BF16) for _ in range(2)]; bxl = [Buf(), Buf()]
    ol = [A.alloc([2048], BF16) for _ in range(2)]; bol = [Buf(), Buf()]
    i = 0
    for q in range(2):
        for sb in range(4):
            p = i % 2; i += 1
            S.dma("sp", yl[p], yscr[q * 128:(q + 1) * 128, sb * 2048:(sb + 1) * 2048], w=[byl[p]])
            S.dma("sp", xl[p], x0scr[q * 128:(q + 1) * 128, sb * 2048:(sb + 1) * 2048], w=[bxl[p]])
            kk.tt("dve", ol[p], yl[p], xl[p], ALU.mult, r=[byl[p], bxl[p]], w=[bol[p]])
            if link is None:
                S.dma("sp", yout[q * 128:(q + 1) * 128, sb * 2048:(sb + 1) * 2048], ol[p], r=[bol[p]], key=f"yo{p}")
            else:
                for hh in range(2):
                    S.dma("sp", link["ysrc"][2 * q + hh][:, sb * 2048:(sb + 1) * 2048], ol[p][hh * 64:(hh + 1) * 64, :],
                          r=[bol[p]], w=[link["bysrc"][2 * q + hh]], key=f"yo{p}{hh}")
    S.barrier()
    if not own:
        return None
    S.emit()
    c.st.close()
    return nc


def build_l3(dbg=False, stop_after=None, c=None, pre="", link=None):
    own = c is None
    if own:
        c = new_prog()
    nc, S, A = c.nc, c.S, c.A
    A.reset(0)

    def D(name, shape, dt=F32, kind="ExternalInput"):
        return nc.dram_tensor(pre + name, shape, dt, kind=kind).ap()

    yT = D("yT", [128, 8 * 2048], BF16) if link is None else None
    hres = D("hres", [16, 128, 1024]) if link is None else link["h1res"]
    cs = D("cs", [128, 16])
    adaw = D("adaw", [1024, 6144]); adabcol = D("adabcol", [128, 32]); adabrow = D("adabrow", [1, 6144])
    lng = D("lng", [2, 1024]); lnb = D("lnb", [2, 1024])
    wout = D("wout", [1024, 1024]); rw = D("rw", [128, 64])
    mg = D("mg", [8, 1024, 3584]); mu = D("mu", [8, 1024, 3584]); md = D("md", [8, 3584, 1024])
    ident = D("ident", [128, 128])
    out = D("out", [16, 128, 1024], kind="ExternalOutput")
    hscr = D("hscr", [16, 128, 1024], kind="Internal")
    dbgo = {}
    if dbg:
        dbgo["d_h1a"] = D("d_h1a", [16, 128, 1024], kind="ExternalOutput")
        dbgo["d_comb"] = D("d_comb", [128, 128], kind="ExternalOutput")
        dbgo["d_acc"] = D("d_acc", [128, 16 * 1024], kind="ExternalOutput")

    identb = A.alloc([128], BF16)
    kk = K(S, identb)
    identf = A.alloc([128], F32)
    colmod = A.alloc([32, 2], F32)
    g1b = A.alloc([1024], F32); g2b = A.alloc([1024], F32)
    lngb = A.alloc([2, 1024], F32); lnbb = A.alloc([2, 1024], F32)
    epsc = A.alloc([4], F32)
    comb = A.alloc([16, 8], F32); bcomb = Buf()
    c.eps_ln = epsc[:, 0:1]
    bconst = Buf("const")
    S.dma("pool", identb, ident[:, :], w=[kk.bident], key="idb")
    S.dma("sp", identf, ident[:, :], w=[kk.bident], key="idf")
    kk.memset("dve", epsc[:, 0:1], LN_EPS, w=[bconst])
    for i in range(2):
        S.dma("sp", lngb[:, i, :], lng[i, :].partition_broadcast(128), w=[bconst], key=f"c{i}a")
        S.dma("sp", lnbb[:, i, :], lnb[i, :].partition_broadcast(128), w=[bconst], key=f"c{i}b")
    mP = A.mark()
    mod_phase(c, kk, cs, adaw, adabcol, adabrow, colmod, g1b, g2b, bconst, need=(2, 3, 4, 5))

    tT = A.alloc([8, 2048], BF16); btT = Buf()
    mP2 = A.mark()
    yTb = A.alloc([8, 2048], BF16); byT = Buf()
    woutb = A.alloc([8, 1024], BF16); bwo = Buf()
    rwf = A.alloc([8, 8], F32); brw = Buf()
    xres = [A.alloc([1024], F32) for _ in range(2)]; bxres = [Buf(), Buf()]
    tmpE = [A.alloc([1024], F32) for _ in range(2)]; btmpE = [Buf(), Buf()]
    outE = [A.alloc([1024], F32) for _ in range(2)]; boutE = [Buf(), Buf()]
    smE = [A.alloc([16], F32) for _ in range(2)]; bsmE = [Buf(), Buf()]
    tmpT = [A.alloc([8, 128], F32) for _ in range(2)]; btmpT = [Buf(), Buf()]
    t32 = [A.alloc([8, 128], F32) for _ in range(2)]; bt32 = [Buf(), Buf()]
    rs = [A.alloc([48], F32) for _ in range(2)]; brs = [Buf(), Buf()]
    if link is None:
        S.dma("sp", yTb.rearrange("p a b -> p (a b)"), yT[:, :], w=[byT])
    else:
        for r_ in range(4):
            for k_ in range(4):
                src = link["ydst"][k_]

                def lazy(eng, src=src, r_=r_):
                    if "off" not in c.dyn:
                        c.dyn["off"] = (eng.partition_id() % 4) * 1024
                    return src[r_ * 64:(r_ + 1) * 64, bass.ds(c.dyn["off"], 1024)]
                S.dma("sp", yTb[(k_ % 2) * 64:(k_ % 2) * 64 + 64, 2 * r_ + k_ // 2, :].bitcast(F32), lazy,
                      r=[link["bydst"][k_]], w=[byT], key=f"yl{(r_ * 4 + k_) % 4}")
    S.dma("pool", woutb, wout.rearrange("(kc p) n -> p kc n", p=128), w=[bwo])
    S.dma("sp", rwf.rearrange("p a b -> p (a b)"), rw[:, :], w=[brw])
    ps_out = psbank(c, 0, 1024, nb=2); bpsout = Buf()
    psTf = [psbank(c, 2, 1024, nb=2).rearrange("p (a b) -> p a b", a=8),
            psbank(c, 4, 1024, nb=2).rearrange("p (a b) -> p a b", a=8)]
    bpsTf = [Buf(), Buf()]
    ps_l = psbank(c, 6, 8); bpsl = Buf()
    def ph2_a(il):
        p = il % 2
        S.dma("sp", xres[p], hres[il], r=([] if link is None else [link["bh1res"][il]]), w=[bxres[p]])
        for half in range(2):
            n0 = half * 512
            for kc in range(8):
                kk.mm(ps_out[:, n0:n0 + 512], yTb[:, kc, il * 128:(il + 1) * 128], woutb[:, kc, n0:n0 + 512], kc == 0, kc == 7,
                      r=[byT, bwo], w=[bpsout])
        ln_tile(c, kk, ps_out, bpsout, xres[p], bxres[p], g1b, lngb[:, 0, :], lnbb[:, 0, :], bconst,
                tmpE[p], btmpE[p], outE[p], boutE[p], smE[p], bsmE[p])
        S.dma("sp", hscr[il], outE[p], r=[boutE[p]], key=f"hs{p}")
        if dbg:
            S.dma("sp", dbgo["d_h1a"][il], outE[p], r=[boutE[p]], key=f"dbgh{p}")

    def ph2_b(il):
        p = il % 2
        for kc in range(8):
            S.op("pe", (lambda o, i_: (lambda e: e.transpose(out=o, in_=i_, identity=identf)))(psTf[p][:, kc, :], outE[p][:, kc * 128:(kc + 1) * 128]),
                 r=[boutE[p], kk.bident], w=[bpsTf[p]])
        for hb_ in range(2):
            kk.tt("dve", tmpT[p][:, hb_ * 4:(hb_ + 1) * 4, :], psTf[p][:, hb_ * 4:(hb_ + 1) * 4, :],
                  bcast_mid(colmod[:, 24 + hb_ * 4:28 + hb_ * 4, 0], 128), ALU.mult, r=[bpsTf[p], bconst], w=[btmpT[p]])
        kk.tt("dve", t32[p], tmpT[p], bcast_mid(colmod[:, 16:24, 0], 128), ALU.add, r=[btmpT[p], bconst], w=[bt32[p]])
        kk.cp("act", tT[:, :, il * 128:(il + 1) * 128], t32[p], r=[bt32[p]], w=[btT])
        for kc in range(8):
            kk.mm(ps_l, t32[p][:, kc, :], rwf[:, kc, :], kc == 0, kc == 7, r=[bt32[p], brw], w=[bpsl])
        r_ = rs[p]; br_ = brs[p]
        lg = r_[:, 0:8]; eq1 = r_[:, 8:16]; l2 = r_[:, 16:24]; eq2 = r_[:, 24:32]
        m1 = r_[:, 32:33]; m2 = r_[:, 33:34]; dg = r_[:, 34:35]; ga = r_[:, 35:36]; gb = r_[:, 36:37]
        kk.cp("dve", lg, ps_l, r=[bpsl], w=[br_])
        S.op("dve", (lambda o, i_: (lambda e: e.tensor_reduce(out=o, in_=i_, axis=mybir.AxisListType.X, op=ALU.max)))(m1, lg), r=[br_], w=[br_])
        kk.ts("dve", eq1, lg, m1, None, ALU.is_equal, None, r=[br_], w=[br_])
        kk.stt("dve", l2, eq1, -1e30, lg, ALU.mult, ALU.add, r=[br_], w=[br_])
        S.op("dve", (lambda o, i_: (lambda e: e.tensor_reduce(out=o, in_=i_, axis=mybir.AxisListType.X, op=ALU.max)))(m2, l2), r=[br_], w=[br_])
        kk.ts("dve", eq2, l2, m2, None, ALU.is_equal, None, r=[br_], w=[br_])
        kk.tt("dve", dg, m1, m2, ALU.subtract, r=[br_], w=[br_])
        kk.act(ga, dg, AF.Sigmoid, r=[br_], w=[br_])
        kk.ts("dve", gb, ga, -1.0, 1.0, ALU.mult, ALU.add, r=[br_], w=[br_])
        kk.ts("dve", comb[:, il, :], eq1, ga, None, ALU.mult, None, r=[br_], w=[bcomb])
        kk.stt("dve", comb[:, il, :], eq2, gb, comb[:, il, :], ALU.mult, ALU.add, r=[br_, bcomb], w=[bcomb])

    ph2_a(0)
    for il in range(16):
        if il + 1 < 16:
            ph2_a(il + 1)
        ph2_b(il)
    if dbg:
        S.dma("sp", dbgo["d_comb"][:, :], comb.rearrange("p a b -> p (a b)"), r=[bcomb], key="dbgc")
    S.barrier()
    if stop_after == "P2":
        S.emit(); c.st.close(); return nc
    A.reset(mP2)

    acc = A.alloc([16, 1024], F32); bacc = [Buf() for _ in range(16)]
    mAcc = A.mark()
    hidT = A.alloc([4, 2048], BF16); bhid = Buf()
    wgq = [A.alloc([8, 512], BF16) for _ in range(2)]; wuq = [A.alloc([8, 512], BF16) for _ in range(2)]
    wdq = [A.alloc([4, 1024], BF16) for _ in range(2)]; bwq = [Buf(), Buf()]; bwdq = [Buf(), Buf()]
    sg = [A.alloc([512], BF16) for _ in range(2)]; bsg = [Buf(), Buf()]
    ps_g = [psbank(c, 0), psbank(c, 1)]; bpsg = [Buf(), Buf()]
    ps_u = [psbank(c, 2), psbank(c, 3)]; bpsu = [Buf(), Buf()]
    ps_d = [psbank(c, 4, 1024, nb=2), psbank(c, 6, 1024, nb=2)]; bpsd = [Buf(), Buf()]
    gi = 0
    ci = 0
    di = 0
    for e in range(8):
        mg_v = mg[e].rearrange("(kc p) n -> p kc n", p=128)
        mu_v = mu[e].rearrange("(kc p) n -> p kc n", p=128)
        md_v = md[e].rearrange("(c p) n -> p c n", p=128)
        for fg in range(7):
            p = gi % 2
            first = (gi == 0)
            gi += 1
            S.dma("pool", wgq[p], mg_v[:, :, fg * 512:(fg + 1) * 512], w=[bwq[p]], key=f"wg{p}")
            S.dma("pool", wuq[p], mu_v[:, :, fg * 512:(fg + 1) * 512], w=[bwq[p]], key=f"wu{p}")
            S.dma("pool", wdq[p], md_v[:, fg * 4:(fg + 1) * 4, :], w=[bwdq[p]], key=f"wd{p}")
            for fc in range(4):
                for ts_ in range(4):
                    q = ci % 2
                    ci += 1
                    t0 = ts_ * 512
                    for kc in range(8):
                        kk.mm(ps_g[q], wgq[p][:, kc, fc * 128:(fc + 1) * 128], tT[:, kc, t0:t0 + 512], kc == 0, kc == 7,
                              r=[bwq[p], btT], w=[bpsg[q]])
                    for kc in range(8):
                        kk.mm(ps_u[q], wuq[p][:, kc, fc * 128:(fc + 1) * 128], tT[:, kc, t0:t0 + 512], kc == 0, kc == 7,
                              r=[bwq[p], btT], w=[bpsu[q]])
                    kk.act(sg[q], ps_g[q], AF.Silu, r=[bpsg[q]], w=[bsg[q]])
                    kk.tt("dve", hidT[:, fc, t0:t0 + 512], ps_u[q], sg[q], ALU.mult, r=[bpsu[q], bsg[q]], w=[bhid])
            for il in range(16):
                d = di % 2
                di += 1
                for half in range(2):
                    n0 = half * 512
                    for fc in range(4):
                        kk.mm(ps_d[d][:, n0:n0 + 512], hidT[:, fc, il * 128:(il + 1) * 128], wdq[p][:, fc, n0:n0 + 512], fc == 0, fc == 3,
                              r=[bhid, bwdq[p]], w=[bpsd[d]])
                for half in range(2):
                    n0 = half * 512
                    if first:
                        kk.ts("dve", acc[:, il, n0:n0 + 512], ps_d[d][:, n0:n0 + 512], comb[:, il, e:e + 1], None, ALU.mult, None,
                              r=[bpsd[d], bcomb], w=[bacc[il]])
                    else:
                        kk.stt("dve", acc[:, il, n0:n0 + 512], ps_d[d][:, n0:n0 + 512], comb[:, il, e:e + 1], acc[:, il, n0:n0 + 512],
                               ALU.mult, ALU.add, r=[bpsd[d], bcomb, bacc[il]], w=[bacc[il]])
    if dbg:
        S.dma("sp", dbgo["d_acc"][:, :], acc.rearrange("p a b -> p (a b)"), r=bacc, key="dbga")
    S.barrier()
    A.reset(mAcc)
    xr2 = [A.alloc([1024], F32) for _ in range(2)]; bxr2 = [Buf(), Buf()]
    tmpF = [A.alloc([1024], F32) for _ in range(2)]; btmpF = [Buf(), Buf()]
    outF = [A.alloc([1024], F32) for _ in range(2)]; boutF = [Buf(), Buf()]
    smF = [A.alloc([16], F32) for _ in range(2)]; bsmF = [Buf(), Buf()]
    for il in range(16):
        p = il % 2
        S.dma("sp", xr2[p], hscr[il], w=[bxr2[p]])
        ln_tile(c, kk, acc[:, il, :], bacc[il], xr2[p], bxr2[p], g2b, lngb[:, 1, :], lnbb[:, 1, :], bconst,
                tmpF[p], btmpF[p], outF[p], boutF[p], smF[p], bsmF[p])
        S.dma("sp", out[il], outF[p], r=[boutF[p]], key=f"ho{p}")
    S.barrier()
    if not own:
        return None
    S.emit()
    c.st.close()
    return nc


def rope_table():
    n = 8192
    rows = n // 64
    r = np.repeat(np.arange(rows, dtype=np.float32), 64)
    col = np.tile(np.arange(64, dtype=np.float32), rows)
    inv = (np.float32(10000.0) ** (-np.arange(0, 16, 2, dtype=np.float32) / np.float32(16))).astype(np.float32)
    ar = r[:, None] * inv
    ac = col[:, None] * inv
    return np.concatenate([np.cos(ar), np.cos(ac), np.sin(ar), np.sin(ac)], axis=1).astype(np.float32)


def band_mats(j):
    n = 8192

    def A(G, o, g):
        m = np.zeros((128, 128), np.float32)
        Gs = G + o
        if Gs < 0 or Gs >= 64:
            return m
        half = POOL_WINDOWS[g] // 2
        for t in range(128):
            T = G * 128 + t
            lo = max(T - half, 0)
            hi = min(T + half, n)
            for Sx in range(lo, hi):
                s = Sx - Gs * 128
                if 0 <= s < 128:
                    m[s, t] += 1.0 / (hi - lo)
            s = T - Gs * 128
            if 0 <= s < 128:
                m[s, t] -= 1.0
        return m
    G0 = 16 * j
    Gm = 1
    out = np.zeros((7, 4, 128, 128), np.float32)
    for g in range(4):
        out[0, g] = A(Gm, -1, g); out[1, g] = A(Gm, 0, g); out[2, g] = A(Gm, 1, g)
        out[3, g] = A(G0, -1, g); out[4, g] = A(G0, 0, g)
        out[5, g] = A(G0 + 15, 0, g); out[6, g] = A(G0 + 15, 1, g)
    return out


def col_form(v, nchunk):
    return np.ascontiguousarray(v.reshape(nchunk, 128).T)


def l1_inputs(inp):
    x = inp["x"]; ctx = inp["ctx"]
    rt = rope_table()
    ident = np.eye(128, dtype=np.float32)
    ada_b0 = inp["ada_b"][0]
    adabcol = np.concatenate([col_form(ada_b0[v * 1024:(v + 1) * 1024], 8) for v in (0, 1, 3, 4)], axis=1)
    kv_up = inp["kv_up"][0].reshape(256, 8, 128)
    kvupk = np.ascontiguousarray(kv_up[:, :, :64].reshape(256, 512))
    kvupv = np.ascontiguousarray(kv_up[:, :, 64:].reshape(256, 512))
    maps = []
    for core in range(NCORES):
        b, j = core // 4, core % 4
        xt = x[b].reshape(64, 128, 1024)
        ct = ctx[b].reshape(2, 128, 1024)
        own = list(range(16 * j, 16 * j + 16))
        others = [t for t in range(64) if t not in own]
        order = own + others
        xkv = np.concatenate([ct, xt[order]], axis=0)
        rtt = rt.reshape(64, 128, 32)[order]
        r0 = np.zeros((2, 128, 32), np.float32); r0[:, :, 0:16] = 1.0
        ropet = np.concatenate([r0, rtt], axis=0)
        ro = rt.reshape(64, 128, 32)[own]
        cosq = np.broadcast_to(ro[:, :, None, 0:16].reshape(16, 128, 1, 2, 1, 8), (16, 128, 8, 2, 2, 8)).reshape(16, 128, 256)
        sinq = np.broadcast_to(ro[:, :, None, 16:32].reshape(16, 128, 1, 2, 1, 8), (16, 128, 8, 2, 2, 8)).reshape(16, 128, 256)
        ropeq = np.ascontiguousarray(np.concatenate([cosq, sinq], axis=2))
        xhalo = np.zeros((2, 128, 1024), np.float32)
        if 16 * j - 1 >= 0:
            xhalo[0] = xt[16 * j - 1]
        if 16 * j + 16 < 64:
            xhalo[1] = xt[16 * j + 16]
        cs = np.stack([col_form(inp["c"][b], 8), col_form(inp["c_ctx"], 8)], axis=2).reshape(128, 16)
        maps.append({
            "xkv": np.ascontiguousarray(xkv), "xhalo": xhalo, "ropeq": ropeq, "ropet": np.ascontiguousarray(ropet),
            "cs": np.ascontiguousarray(cs), "adaw": inp["ada_w"][0], "adabcol": np.ascontiguousarray(adabcol),
            "adabrow": ada_b0.reshape(1, 6144), "lng": inp["ln_g"][0], "lnb": inp["ln_b"][0],
            "win": inp["mix_in_w"][0], "poolw": inp["pool_w"][0], "pscale": col_form(inp["pool_scale"][0], 4),
            "qnorm": col_form(inp["q_norm"][0], 2), "kvnorm": col_form(inp["kv_norm"][0], 2),
            "qup": inp["q_up"][0], "kvupk": kvupk, "kvupv": kvupv, "wout": inp["mix_out_w"][0],
            "wg": inp["ffn_gate"][0], "wu": inp["ffn_up"][0], "wd": inp["ffn_down"][0],
            "bands": band_mats(j), "ident": ident,
        })
    return maps


HY_MIN_DECAY = math.log(1e-2) / 1.5
HY_MAX_DECAY = math.log(1e-2) / 0.3


def fft_consts():
    n1 = np.arange(64)[:, None]; k2 = np.arange(128)[None, :]
    th = 2 * np.pi * n1 * k2 / 128
    fr, fi = np.cos(th), -np.sin(th)
    f1cat = np.concatenate([fr, fi, -fi, fr, fr, -fi, -fi, -fr], axis=1).astype(np.float32)
    k1 = np.arange(128)[:, None]; n2 = np.arange(128)[None, :]
    th = 2 * np.pi * k1 * n2 / 128
    pr, pi_ = np.cos(th), np.sin(th)
    f1inv = np.concatenate([pr, pi_, -pi_, pr], axis=1).astype(np.float32)
    a = np.arange(128)[:, None, None]; j = np.arange(128)[None, :, None]; b = np.arange(128)[None, None, :]
    th = 2 * np.pi * ((a * (j + 128 * b)) % 16384) / 16384
    g = np.stack([np.cos(th), -np.sin(th)], axis=2).astype(np.float32)
    return f1cat, f1inv, np.ascontiguousarray(g.reshape(128, 128 * 2 * 128))


def filt_consts():
    n = 8192
    t = np.linspace(0.0, 1.0, n, dtype=np.float32)
    w_ang = (np.float32(2.0 * math.pi / n) * np.arange(n, dtype=np.float32))[:, None]
    bands = np.linspace(1e-4, 15, 16, dtype=np.float32)[None, :]
    z = np.concatenate([t[:, None], np.cos(bands * w_ang), -np.sin(bands * w_ang)], axis=-1).astype(np.float32)
    deltas = np.abs(np.linspace(HY_MIN_DECAY, HY_MAX_DECAY, 1024, dtype=np.float32))
    return np.ascontiguousarray(z.T), t.reshape(1, n).copy(), deltas


def mod_inputs(inp, layer, b):
    ada_b = inp["ada_b"][layer]
    adabcol = np.concatenate([col_form(ada_b[v * 1024:(v + 1) * 1024], 8) for v in (0, 1, 3, 4)], axis=1)
    cs = np.stack([col_form(inp["c"][b], 8), col_form(inp["c_ctx"], 8)], axis=2).reshape(128, 16)
    return {"cs": np.ascontiguousarray(cs), "adaw": inp["ada_w"][layer], "adabcol": np.ascontiguousarray(adabcol),
            "adabrow": ada_b.reshape(1, 6144)}


def l2_inputs(inp, h1):
    f1cat, f1inv, gmat = fft_consts()
    zembT, tvals, deltas = filt_consts()
    ident = np.eye(128, dtype=np.float32)
    w_in = inp["hy_in_w"][0]; cw = inp["hy_conv_w"][0]; cb = inp["hy_conv_b"][0]
    fout = inp["hy_fout"][0]
    fbs = np.stack([inp["hy_fb1"][0], inp["hy_fb2"][0], inp["hy_fb3"][0], inp["hy_freq"][0]], axis=1)
    maps = []
    for core in range(NCORES):
        b, j = core // 4, core % 4
        cols = np.concatenate([part * 1024 + 256 * j + np.arange(256) for part in range(3)])
        convw = np.zeros((128, 18), np.float32); convb = np.zeros((128, 6), np.float32)
        for ch in range(6):
            cc = cols[ch * 128:(ch + 1) * 128]
            convw[:, ch * 3:(ch + 1) * 3] = cw[:, cc].T
            convb[:, ch] = cb[cc]
        fcols = np.concatenate([kind * 1024 + 256 * j + np.arange(256) for kind in range(2)])
        m = {"win": np.ascontiguousarray(w_in[:, cols]),
             "convw": convw, "convb": convb, "zemb": zembT, "fw1": inp["hy_fw1"][0], "fw2": inp["hy_fw2"][0],
             "fw3": inp["hy_fw3"][0], "fout": np.ascontiguousarray(fout[:, fcols]), "fbs": np.ascontiguousarray(fbs),
             "skipc": col_form(inp["hy_skip"][0][256 * j:256 * j + 256], 2), "deltac": col_form(deltas[256 * j:256 * j + 256], 2),
             "tvals": tvals, "f1cat": f1cat, "f1inv": f1inv, "gmat": gmat, "ident": ident}
        if h1 is not None:
            m["hin"] = np.ascontiguousarray(h1[b].reshape(64, 128, 1024))
        m.update(mod_inputs(inp, 1, b))
        maps.append(m)
    return maps


def l3_inputs(inp, h1, yfull):
    ident = np.eye(128, dtype=np.float32)
    rw = np.ascontiguousarray(inp["router_w"][0].reshape(8, 128, 8).transpose(1, 0, 2).reshape(128, 64))
    maps = []
    for core in range(NCORES):
        b, j = core // 4, core % 4
        m = {"lng": inp["ln_g"][1], "lnb": inp["ln_b"][1], "wout": inp["hy_out_w"][0], "rw": rw,
             "mg": inp["moe_gate"][0], "mu": inp["moe_up"][0], "md": inp["moe_down"][0], "ident": ident}
        if yfull is not None:
            yt = yfull[b][:, 2048 * j:2048 * (j + 1)].reshape(8, 128, 2048).transpose(1, 0, 2).reshape(128, 8 * 2048)
            m["yT"] = np.ascontiguousarray(yt)
            m["hres"] = np.ascontiguousarray(h1[b, 2048 * j:2048 * (j + 1)].reshape(16, 128, 1024))
        m.update(mod_inputs(inp, 1, b))
        maps.append(m)
    return maps


G4 = [[0, 1, 2, 3], [4, 5, 6, 7]]


def build_fused():
    c = new_prog()
    nc, S = c.nc, c.S
    h1res = nc.dram_tensor("x_h1res", [16, 128, 1024], F32).ap()
    bh1res = [Buf() for _ in range(16)]
    xs_t = [nc.dram_tensor(f"x_xs{k}", [2048, 128], F32) for k in range(4)]
    xd_t = [nc.dram_tensor(f"x_xd{k}", [8192, 128], F32) for k in range(4)]
    ys_t = [nc.dram_tensor(f"x_ys{k}", [2048, 128], F32) for k in range(4)]
    yd_t = [nc.dram_tensor(f"x_yd{k}", [8192, 128], F32) for k in range(4)]
    xsrc = [t.ap().bitcast(BF16).rearrange("(t a) b -> t (a b)", a=4) for t in xs_t]
    xdst = [t.ap().bitcast(BF16).rearrange("(t a) b -> t (a b)", a=4) for t in xd_t]
    ysrc = [t.ap().bitcast(BF16).rearrange("(c a) b -> c (a b)", a=32) for t in ys_t]
    ydst = [t.ap().rearrange("(c a) b -> c (a b)", a=32) for t in yd_t]
    bxsrc = [Buf() for _ in range(4)]; bxdst = [Buf() for _ in range(4)]
    bysrc = [Buf() for _ in range(4)]; bydst = [Buf() for _ in range(4)]
    build_l1(c=c, pre="a_", link={"h1res": h1res, "bh1res": bh1res, "xsrc": xsrc, "bxsrc": bxsrc})
    for k in range(4):
        S.coll("AllGather", G4, xs_t[k], xd_t[k], r=[bxsrc[k]], w=[bxdst[k]])
    build_l2(c=c, pre="b_", link={"xdst": xdst, "bxdst": bxdst, "ysrc": ysrc, "bysrc": bysrc})
    for k in range(4):
        S.coll("AllGather", G4, ys_t[k], yd_t[k], r=[bysrc[k]], w=[bydst[k]])
    build_l3(c=c, pre="c_", link={"h1res": h1res, "bh1res": bh1res, "ydst": ydst, "bydst": bydst})
    S.emit()
    c.st.close()
    return nc


def fused_inputs(inp):
    m1 = l1_inputs(inp)
    m2 = l2_inputs(inp, None)
    m3 = l3_inputs(inp, None, None)
    maps = []
    for core in range(NCORES):
        m = {}
        for pre, mm_ in (("a_", m1[core]), ("b_", m2[core]), ("c_", m3[core])):
            for k, v in mm_.items():
                m[pre + k] = v
        maps.append(m)
    return maps


_CACHE = {}


def kernel(**inputs):
    inp = {k: np.asarray(v) for k, v in inputs.items()}
    if "fused" not in _CACHE:
        _CACHE["fused"] = build_fused()
    res = run_bass_kernel_spmd(_CACHE["fused"], fused_inputs(inp), core_ids=list(range(NCORES)))
    out = np.stack([np.asarray(r["c_out"]).reshape(2048, 1024) for r in res.results]).reshape(2, 8192, 1024)
    return out.astype(np.float32)
```
